# Optimizing a Trainium2 kernel written in Bass

```python
import functools
import jax, jax.numpy as jnp
from jax import lax
import numpy as np

D_MODEL = 1024
BATCH = 4
SEQ = 4096
DEPTH = 1
DEC_BATCH = 32
DEC_SEQ = 1
PAST_LEN = 8192
PAGE_SIZE = 128

N_HEADS = 8
HEAD_DIM = 64
ATTN_WIDTH = N_HEADS * HEAD_DIM
MOBA_BLOCK = 256
MOBA_TOPK = 3
ROPE_THETA = 10000.0
Q_CHUNK = 128
POOL_WINDOWS = (2, 4, 8, 16)
POOL_GROUPS = 4
POOL_WIDTH = D_MODEL // 2
POOL_GROUP_WIDTH = POOL_WIDTH // POOL_GROUPS
POOL_STATE = max(POOL_WINDOWS) - 1
IN_WIDTH = 3 * ATTN_WIDTH + POOL_WIDTH + 2 * D_MODEL
N_EXPERT_GROUPS = 4
EXPERTS_PER_GROUP = 4
N_EXPERTS = N_EXPERT_GROUPS * EXPERTS_PER_GROUP
D_EXPERT = D_MODEL // 4
EXPERT_TOPK = 2
LN_EPS = 1e-5
ALPHA = (2 * DEPTH) ** 0.25
BETA = (8 * DEPTH) ** -0.25

kernel_name = 'moba_pool_hmoe_decode_step'


def layer_norm(x, g, b):
    xf = x.astype(jnp.float32)
    mu = jnp.mean(xf, axis=-1, keepdims=True)
    var = jnp.mean(jnp.square(xf - mu), axis=-1, keepdims=True)
    y = (xf - mu) * lax.rsqrt(var + LN_EPS) * g.astype(jnp.float32) + b.astype(jnp.float32)
    return y.astype(x.dtype)


def rope(x, pos):
    half = HEAD_DIM // 2
    inv_freq = 1.0 / (ROPE_THETA ** (jnp.arange(0, HEAD_DIM, 2, dtype=jnp.float32) / HEAD_DIM))
    ang = pos.astype(jnp.float32)[:, None] * inv_freq[None, :]
    cos = jnp.cos(ang)[None, :, None, :]
    sin = jnp.sin(ang)[None, :, None, :]
    xf = x.astype(jnp.float32)
    x1, x2 = xf[..., :half], xf[..., half:]
    return jnp.concatenate([x1 * cos - x2 * sin, x2 * cos + x1 * sin], axis=-1).astype(x.dtype)


def in_projection(x, w_in):
    b, l, _ = x.shape
    h = jnp.einsum('bld,de->ble', x, w_in)
    cuts = [ATTN_WIDTH, 2 * ATTN_WIDTH, 3 * ATTN_WIDTH, 3 * ATTN_WIDTH + POOL_WIDTH,
            3 * ATTN_WIDTH + POOL_WIDTH + D_MODEL]
    q, k, v, u, ga, gb = jnp.split(h, cuts, axis=-1)
    heads = (b, l, N_HEADS, HEAD_DIM)
    return q.reshape(heads), k.reshape(heads), v.reshape(heads), u, ga, gb


def to_blocks(k):
    b, t = k.shape[0], k.shape[1]
    nb = -(-t // MOBA_BLOCK)
    k = jnp.pad(k, ((0, 0), (0, nb * MOBA_BLOCK - t), (0, 0), (0, 0)))
    return k.reshape(b, nb, MOBA_BLOCK, N_HEADS, HEAD_DIM)


def block_means(kb):
    return jnp.mean(kb.astype(jnp.float32), axis=2)


def moba_attention(q, qpos, kb, vb, km):
    b, nq = q.shape[0], q.shape[1]
    nb = kb.shape[1]
    own = qpos // MOBA_BLOCK
    gate = jnp.einsum('bqhd,bnhd->bqhn', q.astype(jnp.float32), km)
    fully_past = jnp.arange(nb)[None, :] < own[:, None]
    gate = jnp.where(fully_past[None, :, None, :], gate, -jnp.inf)
    n_top = min(MOBA_TOPK, nb)
    _, top = lax.top_k(gate, n_top)
    own_b = jnp.broadcast_to(own[None, :, None, None], (b, nq, N_HEADS, 1))
    blk = jnp.concatenate([top.astype(jnp.int32), own_b.astype(jnp.int32)], axis=-1)
    b_ix = jnp.arange(b)[:, None, None, None]
    h_ix = jnp.arange(N_HEADS)[None, None, :, None]
    kg = kb[b_ix, blk, :, h_ix]
    vg = vb[b_ix, blk, :, h_ix]
    is_own = jnp.arange(n_top + 1) == n_top
    blk_ok = is_own[None, None, None, :] | (blk < own[None, :, None, None])
    kpos = blk[..., None] * MOBA_BLOCK + jnp.arange(MOBA_BLOCK)
    ok = blk_ok[..., None] & (kpos <= qpos[None, :, None, None, None])
    s = jnp.einsum('bqhd,bqhnkd->bqhnk', q, kg, preferred_element_type=jnp.float32) * (HEAD_DIM ** -0.5)
    s = jnp.where(ok, s, -jnp.inf)
    p = jax.nn.softmax(s.reshape(b, nq, N_HEADS, -1), axis=-1).reshape(s.shape)
    return jnp.einsum('bqhnk,bqhnkd->bqhd', p.astype(vg.dtype), vg)


def moba_prompt(q, k, v):
    b, s = q.shape[0], q.shape[1]
    kb, vb = to_blocks(k), to_blocks(v)
    km = block_means(kb)
    nc = s // Q_CHUNK
    qc = q.reshape(b, nc, Q_CHUNK, N_HEADS, HEAD_DIM).transpose(1, 0, 2, 3, 4)
    pc = jnp.arange(s, dtype=jnp.int32).reshape(nc, Q_CHUNK)
    out = lax.map(lambda a: moba_attention(a[0], a[1], kb, vb, km), (qc, pc))
    return out.transpose(1, 0, 2, 3, 4).reshape(b, s, N_HEADS, HEAD_DIM)


def moba_sample(q, k, v, cache_k, cache_v, page_table):
    db, l = q.shape[0], q.shape[1]
    past_k = cache_k[page_table].reshape(db, -1, N_HEADS, HEAD_DIM)
    past_v = cache_v[page_table].reshape(db, -1, N_HEADS, HEAD_DIM)
    past_len = past_k.shape[1]
    kb = to_blocks(jnp.concatenate([past_k, k.astype(past_k.dtype)], axis=1))
    vb = to_blocks(jnp.concatenate([past_v, v.astype(past_v.dtype)], axis=1))
    km = block_means(kb)
    qpos = past_len + jnp.arange(l, dtype=jnp.int32)
    return moba_attention(q, qpos, kb, vb, km)


def multiscale_pool(u, prefix, pos, w_pool, pool_scale):
    b, l, c = u.shape
    z = jnp.concatenate([prefix.astype(u.dtype), u], axis=1)
    cs = jnp.pad(jnp.cumsum(z.astype(jnp.float32), axis=1), ((0, 0), (1, 0), (0, 0)))
    means = []
    for g, w in enumerate(POOL_WINDOWS):
        c0, c1 = g * POOL_GROUP_WIDTH, (g + 1) * POOL_GROUP_WIDTH
        win_sum = (cs[:, POOL_STATE + 1:POOL_STATE + 1 + l, c0:c1]
                   - cs[:, POOL_STATE + 1 - w:POOL_STATE + 1 - w + l, c0:c1])
        count = jnp.minimum(w, pos + 1).astype(jnp.float32)[None, :, None]
        means.append(win_sum / count)
    diff = (jnp.concatenate(means, axis=-1) - u.astype(jnp.float32)).astype(u.dtype)
    d = diff.reshape(b, l, POOL_GROUPS, POOL_GROUP_WIDTH)
    mixed = jnp.einsum('blgc,gce->blge', d, w_pool).reshape(b, l, c) * pool_scale
    return mixed, z[:, -POOL_STATE:]


def hier_moe(x, w_group_router, b_group_router, w_expert_router, b_expert_router,
             w_e_gate, w_e_up, w_e_down):
    b, l, d = x.shape
    xt = x.reshape(b * l, d)
    n = xt.shape[0]
    rows = jnp.arange(n)
    g_logits = jnp.einsum('nd,dg->ng', xt, w_group_router).astype(jnp.float32) + b_group_router.astype(jnp.float32)
    g_prob = jax.nn.softmax(g_logits, axis=-1)
    g_sel = jnp.argmax(g_logits, axis=-1)
    g_w = g_prob[rows, g_sel][:, None]
    e_all = jnp.einsum('nd,gde->nge', xt, w_expert_router).astype(jnp.float32) + b_expert_router.astype(jnp.float32)
    e_logits = e_all[rows, g_sel]
    top_v, top_i = lax.top_k(e_logits, EXPERT_TOPK)
    wts = jax.nn.softmax(top_v, axis=-1) * g_w
    eid = g_sel[:, None] * EXPERTS_PER_GROUP + top_i
    combine = jnp.sum(jax.nn.one_hot(eid, N_EXPERTS, dtype=jnp.float32) * wts[..., None], axis=1)
    hg = jnp.einsum('nd,edf->nef', xt, w_e_gate)
    hu = jnp.einsum('nd,edf->nef', xt, w_e_up)
    h = jax.nn.silu(hg) * hu * combine[..., None].astype(x.dtype)
    y = jnp.einsum('nef,efd->nd', h, w_e_down)
    return y.reshape(b, l, d)


def decoder_layer(x, pos, attend, pool_prefix, w_in, w_pool, pool_scale, w_branch_a, w_branch_b,
                  b_gate, w_out, ln1_g, ln1_b, w_group_router, b_group_router, w_expert_router,
                  b_expert_router, w_e_gate, w_e_up, w_e_down, ln2_g, ln2_b):
    b, l, _ = x.shape
    q, k, v, u, ga, gb = in_projection(x, w_in)
    q = rope(q, pos)
    k = rope(k, pos)
    attn = attend(q, k, v).reshape(b, l, ATTN_WIDTH)
    pooled, new_pool = multiscale_pool(u, pool_prefix, pos, w_pool, pool_scale)
    gate_a = jax.nn.sigmoid(ga + b_gate[0])
    gate_b = jax.nn.sigmoid(gb + b_gate[1])
    merged = gate_a * (attn @ w_branch_a) + gate_b * (pooled @ w_branch_b)
    x1 = layer_norm(ALPHA * x + merged @ w_out, ln1_g, ln1_b)
    ffn = hier_moe(x1, w_group_router, b_group_router, w_expert_router, b_expert_router,
                   w_e_gate, w_e_up, w_e_down)
    y = layer_norm(ALPHA * x1 + ffn, ln2_g, ln2_b)
    return y, k, v, new_pool


def setup_inputs(seed: int = 0) -> dict:
    key = jax.random.key(seed)
    ks = jax.random.split(key, 26)
    f32 = jnp.float32
    n_pages = PAST_LEN // PAGE_SIZE
    n_used = DEC_BATCH * n_pages
    n_pool = (5 * n_used + 3) // 4
    perm = jax.random.permutation(ks[0], n_pool)
    page_table = perm[:n_used].reshape(DEC_BATCH, n_pages).astype(jnp.int32)

    def nrm(k, shape, scale):
        return jax.random.normal(k, shape, f32) * scale

    col_scale = jnp.concatenate([jnp.ones((2 * ATTN_WIDTH,), f32), jnp.full((ATTN_WIDTH,), BETA, f32),
                                 jnp.ones((POOL_WIDTH + 2 * D_MODEL,), f32)])
    return {
        'x_prompt': nrm(ks[1], (BATCH, SEQ, D_MODEL), 1.0),
        'x_sample': nrm(ks[2], (DEC_BATCH, DEC_SEQ, D_MODEL), 1.0),
        'cache_k': nrm(ks[3], (DEPTH, n_pool, PAGE_SIZE, N_HEADS, HEAD_DIM), 1.0),
        'cache_v': nrm(ks[4], (DEPTH, n_pool, PAGE_SIZE, N_HEADS, HEAD_DIM), 1.0),
        'state_pool': nrm(ks[5], (DEPTH, DEC_BATCH, POOL_STATE, POOL_WIDTH), 1.0),
        'page_table': page_table,
        'w_in': nrm(ks[6], (DEPTH, D_MODEL, IN_WIDTH), D_MODEL ** -0.5) * col_scale,
        'w_pool': nrm(ks[7], (DEPTH, POOL_GROUPS, POOL_GROUP_WIDTH, POOL_GROUP_WIDTH), POOL_GROUP_WIDTH ** -0.5),
        'pool_scale': 1.0 + nrm(ks[8], (DEPTH, POOL_WIDTH), 0.02),
        'w_branch_a': nrm(ks[9], (DEPTH, ATTN_WIDTH, D_MODEL), ATTN_WIDTH ** -0.5 * BETA),
        'w_branch_b': nrm(ks[10], (DEPTH, POOL_WIDTH, D_MODEL), POOL_WIDTH ** -0.5 * BETA),
        'b_gate': nrm(ks[11], (DEPTH, 2, D_MODEL), 0.02),
        'w_out': nrm(ks[12], (DEPTH, D_MODEL, D_MODEL), D_MODEL ** -0.5 * BETA),
        'ln1_g': 1.0 + nrm(ks[13], (DEPTH, D_MODEL), 0.02),
        'ln1_b': nrm(ks[14], (DEPTH, D_MODEL), 0.02),
        'w_group_router': nrm(ks[15], (DEPTH, D_MODEL, N_EXPERT_GROUPS), D_MODEL ** -0.5),
        'b_group_router': nrm(ks[16], (DEPTH, N_EXPERT_GROUPS), 0.01),
        'w_expert_router': nrm(ks[17], (DEPTH, N_EXPERT_GROUPS, D_MODEL, EXPERTS_PER_GROUP), D_MODEL ** -0.5),
        'b_expert_router': nrm(ks[18], (DEPTH, N_EXPERT_GROUPS, EXPERTS_PER_GROUP), 0.01),
        'w_e_gate': nrm(ks[19], (DEPTH, N_EXPERTS, D_MODEL, D_EXPERT), D_MODEL ** -0.5),
        'w_e_up': nrm(ks[20], (DEPTH, N_EXPERTS, D_MODEL, D_EXPERT), D_MODEL ** -0.5),
        'w_e_down': nrm(ks[21], (DEPTH, N_EXPERTS, D_EXPERT, D_MODEL), D_EXPERT ** -0.5 * BETA),
        'ln2_g': 1.0 + nrm(ks[22], (DEPTH, D_MODEL), 0.02),
        'ln2_b': nrm(ks[23], (DEPTH, D_MODEL), 0.02),
    }


def reference(x_prompt, x_sample, cache_k, cache_v, state_pool, page_table, w_in, w_pool, pool_scale,
              w_branch_a, w_branch_b, b_gate, w_out, ln1_g, ln1_b, w_group_router, b_group_router,
              w_expert_router, b_expert_router, w_e_gate, w_e_up, w_e_down, ln2_g, ln2_b):
    pos_p = jnp.arange(x_prompt.shape[1], dtype=jnp.int32)
    pos_s = PAST_LEN + jnp.arange(x_sample.shape[1], dtype=jnp.int32)
    prefix_p = jnp.zeros((x_prompt.shape[0], POOL_STATE, POOL_WIDTH), x_prompt.dtype)
    h_p, h_s = x_prompt, x_sample
    k_p, v_p, s_p, k_s, v_s, s_s = [], [], [], [], [], []
    for layer in range(DEPTH):
        lw = (w_in[layer], w_pool[layer], pool_scale[layer], w_branch_a[layer], w_branch_b[layer],
              b_gate[layer], w_out[layer], ln1_g[layer], ln1_b[layer], w_group_router[layer],
              b_group_router[layer], w_expert_router[layer], b_expert_router[layer], w_e_gate[layer],
              w_e_up[layer], w_e_down[layer], ln2_g[layer], ln2_b[layer])
        h_p, kl, vl, sl = decoder_layer(h_p, pos_p, moba_prompt, prefix_p, *lw)
        k_p.append(kl)
        v_p.append(vl)
        s_p.append(sl)
        attend_s = functools.partial(moba_sample, cache_k=cache_k[layer], cache_v=cache_v[layer],
                                     page_table=page_table)
        h_s, kl, vl, sl = decoder_layer(h_s, pos_s, attend_s, state_pool[layer], *lw)
        k_s.append(kl)
        v_s.append(vl)
        s_s.append(sl)
    return (h_p, h_s, jnp.stack(k_p), jnp.stack(v_p), jnp.stack(s_p), jnp.stack(k_s), jnp.stack(v_s), jnp.stack(s_s))
```

```python
import numpy as np
import ml_dtypes
import concourse.bass as bass
import concourse.mybir as mybir
from concourse.bass_utils import run_bass_kernel_spmd

F32 = mybir.dt.float32
BF16 = mybir.dt.bfloat16
I32 = mybir.dt.int32
ALU = mybir.AluOpType
AF = mybir.ActivationFunctionType
AX = mybir.AxisListType

D = 1024
SEQ = 4096
NH = 8
HD = 64
G = 512
NG = 8
NSLOT = 4
ALPHA = 2.0 ** 0.25
BIG = 30000.0
NEG = -1.0e30
LN_EPS = 1e-5
NE = 16
DE = 256
STOP = 99


class Buf:
    __slots__ = ("w", "r")

    def __init__(self):
        self.w = None
        self.r = []


class Ctx:
    def __init__(self, nc, stack):
        self.nc = nc
        self.stack = stack
        self.eng = {"pe": nc.tensor, "act": nc.scalar, "dve": nc.vector, "pool": nc.gpsimd, "sp": nc.sync}
        self.sem = {}
        self.cnt = {}
        self.waited = {e: {} for e in self.eng}
        self.nsem = 0
        for e in self.eng:
            self._new_sem(e)
        self.dsem = {}
        self.dcnt = {}
        self.dnext = {}

    def _new_sem(self, e):
        self.nsem += 1
        s = self.stack.enter_context(self.nc.semaphore(f"s_{e}_{self.nsem}"))
        self.sem[e] = s
        self.cnt[e] = 0

    def epoch(self):
        for e in self.eng:
            if self.cnt[e] > 20000:
                self._new_sem(e)

    def _wait(self, e, tok):
        if tok is None:
            return
        sem, val = tok
        key = id(sem)
        if self.waited[e].get(key, 0) >= val:
            return
        self.waited[e][key] = val
        self.eng[e].wait_ge(sem, val)

    def deps(self, e, reads, writes):
        for b in reads:
            self._wait(e, b.w)
        for b in writes:
            self._wait(e, b.w)
            for t in b.r:
                self._wait(e, t)

    def done(self, tok, reads, writes):
        for b in reads:
            b.r.append(tok)
            if len(b.r) > 12:
                b.r = b.r[-12:]
        for b in writes:
            b.w = tok
            b.r = []

    def op(self, e, ins, reads=(), writes=()):
        self.cnt[e] += 1
        ins.then_inc(self.sem[e], 1)
        tok = (self.sem[e], self.cnt[e])
        self.waited[e][id(self.sem[e])] = max(self.waited[e].get(id(self.sem[e]), 0), 0)
        self.done(tok, reads, writes)
        return tok

    NROT = 4

    def dma(self, q, out, in_, reads=(), writes=(), stream="d", **kw):
        e = q
        self.deps(e, reads, writes)
        key = (q, stream)
        if key not in self.dsem:
            sems = []
            for i in range(self.NROT):
                self.nsem += 1
                sems.append(self.stack.enter_context(self.nc.semaphore(f"dma_{q}_{stream}_{self.nsem}")))
            self.dsem[key] = sems
            self.dcnt[key] = [0] * self.NROT
            self.dnext[key] = 0
        i = self.dnext[key]
        self.dnext[key] = (i + 1) % self.NROT
        sem = self.dsem[key][i]
        if self.dcnt[key][i] > 0:
            self._wait(e, (sem, self.dcnt[key][i]))
        self.dcnt[key][i] += 16
        self.eng[e].dma_start(out=out, in_=in_, **kw).then_inc(sem, 16)
        tok = (sem, self.dcnt[key][i])
        self.done(tok, reads, writes)
        return tok

    def dma_done(self, q, ins, reads, writes, stream):
        key = (q, stream)
        if key not in self.dsem:
            sems = []
            for i in range(self.NROT):
                self.nsem += 1
                sems.append(self.stack.enter_context(self.nc.semaphore(f"dma_{q}_{stream}_{self.nsem}")))
            self.dsem[key] = sems
            self.dcnt[key] = [0] * self.NROT
            self.dnext[key] = 0
        i = self.dnext[key]
        self.dnext[key] = (i + 1) % self.NROT
        sem = self.dsem[key][i]
        self.dcnt[key][i] += 16
        ins.then_inc(sem, 16)
        tok = (sem, self.dcnt[key][i])
        self.done(tok, reads, writes)
        return tok

    def drain(self, e):
        for key, sems in self.dsem.items():
            for i, sem in enumerate(sems):
                if self.dcnt[key][i] > 0:
                    self._wait(e, (sem, self.dcnt[key][i]))


def _emit(cx, e, reads, writes, fn):
    cx.deps(e, reads, writes)
    ins = fn()
    return cx.op(e, ins, reads, writes)


def build_program():
    from contextlib import ExitStack
    nc = bass.Bass("TRN2", target_bir_lowering=False)
    st = ExitStack()
    with st:
        _build(nc, st)
    return nc


def _build(nc, st):
    from contextlib import ExitStack
    cx = Ctx(nc, st)

    def din(name, shape, dt=F32):
        return nc.dram_tensor(name, list(shape), dt, kind="ExternalInput").ap()

    def dout(name, shape, dt=F32):
        return nc.dram_tensor(name, list(shape), dt, kind="ExternalOutput").ap()

    def dscr(name, shape, dt=BF16):
        return nc.dram_tensor(name, list(shape), dt, kind="Internal").ap()

    def sb(name, shape, dt=F32):
        return st.enter_context(nc.sbuf_tensor("sb_" + name, list(shape), dt))

    xseq = din("xseq", [SEQ, D])
    xown = din("xown", [NSLOT * G, D])
    xhalo = din("xhalo", [NSLOT * 16, D])
    w_in = din("w_in", [D, 4096])
    w_rot = din("w_rot", [D, 1024])
    w_pool = din("w_pool", [4 * 128, 128])
    w_a = din("w_a", [512, D])
    w_b = din("w_b", [512, D])
    w_out = din("w_out", [D, D])
    w_eg = din("w_eg", [NE * D, DE])
    w_eu = din("w_eu", [NE * D, DE])
    w_ed = din("w_ed", [NE * DE, D])
    w_r = din("w_r", [128, 8 * 20])
    b_r = din("b_r", [128, 20])
    fpar = din("fpar", [128, 52])
    cosk = din("cosk", [128, SEQ])
    sink = din("sink", [128, SEQ])
    cosq = din("cosq", [128, NSLOT * G])
    sinq = din("sinq", [128, NSLOT * G])
    negm = din("negm", [NSLOT * G, 128])
    valm = din("valm", [NSLOT * G, 128])
    ownm = din("ownm", [NSLOT * G, 128])
    sel16_in = din("sel16", [16, 16 * 128])
    cmask = din("cmask", [2, 128, 8 * G], BF16)
    blkind = din("blkind", [16, SEQ], BF16)
    invc = din("invc", [NSLOT, 128, 4 * G])
    ident_in = din("ident", [128, 128])
    xs4 = din("xs4", [4, D])
    ck = din("ck", [2560 * 8, 8192])
    cv = din("cv", [2560 * 8, 8192])
    pt_in = din("pt", [128, 2], I32)
    st4 = din("st4", [4, 15 * 512])
    cs8 = din("cs8", [4, 512])
    sn8 = din("sn8", [4, 512])
    selT_in = din("selT", [4, 256])
    pairsel_in = din("pairsel", [128, 8])

    y_out = dout("y_out", [NSLOT * G, D])
    k_out = dout("k_out", [SEQ, 512])
    v_out = dout("v_out", [SEQ, 512])
    pool_out = dout("pool_out", [16, 512])
    ys_out = dout("ys_out", [4, D])
    ks_out = dout("ks_out", [4, 512])
    vs_out = dout("vs_out", [4, 512])
    ps_out = dout("ps_out", [4, 15 * 512])

    s_win = dscr("s_win", [D, 4096])
    s_wrot = dscr("s_wrot", [D, 1024])
    s_wpool = dscr("s_wpool", [512, 128])
    s_wa = dscr("s_wa", [512, D])
    s_wb = dscr("s_wb", [512, D])
    s_wout = dscr("s_wout", [D, D])
    s_eg = dscr("s_eg", [NE * D, DE])
    s_eu = dscr("s_eu", [NE * D, DE])
    s_ed = dscr("s_ed", [NE * DE, D])
    s_kT = dscr("s_kT", [NH, HD, SEQ])
    s_v = dscr("s_v", [NH, 128, 32, 66])

    conv = Buf()

    def convert(dst, src, rows, cols):
        tot = rows * cols
        L = 2048 if tot % 2048 == 0 else cols
        R = tot // L
        if cols != L:
            if cols > L:
                s2 = src.rearrange("r (a b) -> (r a) b", b=L)
                d2 = dst.rearrange("r (a b) -> (r a) b", b=L)
            else:
                s2 = src.rearrange("(r a) b -> r (a b)", a=L // cols)
                d2 = dst.rearrange("(r a) b -> r (a b)", a=L // cols)
        else:
            s2, d2 = src, dst
        step = 512
        for r0 in range(0, R, step):
            r1 = min(R, r0 + step)
            cx.dma("pool", d2[r0:r1, :], s2[r0:r1, :], writes=[conv], stream="conv")

    convert(s_win, w_in, D, 4096)
    convert(s_wrot, w_rot, D, 1024)
    convert(s_wpool, w_pool, 512, 128)
    convert(s_wa, w_a, 512, D)
    convert(s_wb, w_b, 512, D)
    convert(s_wout, w_out, D, D)
    convert(s_eg, w_eg, NE * D, DE)
    convert(s_eu, w_eu, NE * D, DE)
    convert(s_ed, w_ed, NE * DE, D)

    if STOP == 0:
        cx.drain("sp")
        return
    ident = sb("ident", [128, 128])
    identb = sb("identb", [128, 128], BF16)
    ones_ln = sb("ones_ln", [128, 128])
    fp = sb("fp", [128, 52])
    wr_sb = sb("wr_sb", [128, 8 * 20])
    br_sb = sb("br_sb", [128, 20])
    cB = Buf()
    cx.dma("sp", ident[:], ident_in[:, :], writes=[cB])
    cx.dma("sp", fp[:], fpar[:, :], writes=[cB])
    cx.dma("sp", wr_sb[:], w_r[:, :], writes=[cB])
    cx.dma("sp", br_sb[:], b_r[:, :], writes=[cB])
    _emit(cx, "dve", [cB], [cB], lambda: nc.vector.tensor_copy(out=identb[:], in_=ident[:]))
    _emit(cx, "dve", [], [cB], lambda: nc.vector.memset(ones_ln[:], 1.0 / D))

    ps = [st.enter_context(nc.psum_tensor(f"ps{i}", [128, 512], F32)) for i in range(8)]
    pb = [Buf() for _ in range(8)]

    def mm_group(out_ap, pairs, reads, wbuf):
        cx.deps("pe", reads, [wbuf])
        n = len(pairs)
        ins = None
        for i, (l, r) in enumerate(pairs):
            ins = nc.tensor.matmul(out_ap, l, r, start=(i == 0), stop=(i == n - 1))
        return cx.op("pe", ins, reads, [wbuf])

    def transpose(out_ap, in_ap, idt, reads, wbuf):
        cx.deps("pe", reads, [wbuf])
        ins = nc.tensor.transpose(out_ap, in_ap, idt)
        return cx.op("pe", ins, reads, [wbuf])

    def load_xT(x_dram, row0, ntok, xT_f, xT_b, xf_buf, xb_buf, xt_tiles, xt_bufs, psA, psB):
        nt = ntok // 128
        for t in range(nt):
            xt = xt_tiles[t % 2]
            xtb = xt_bufs[t % 2]
            cx.dma("sp", xt[:], x_dram[row0 + t * 128: row0 + (t + 1) * 128, :], writes=[xtb], stream="x")
            for half in range(2):
                pi = psA if half == 0 else psB
                for j in range(4):
                    kc = half * 4 + j
                    transpose(ps[pi][:, j * 128:(j + 1) * 128], xt[:, kc * 128:(kc + 1) * 128], ident[:],
                              [xtb, cB], pb[pi])
                src = ps[pi][:, :].rearrange("p (j t) -> p j t", j=4)
                if xT_f is not None:
                    dstf = xT_f[:, half * 4:(half + 1) * 4, t * 128:(t + 1) * 128]
                    _emit(cx, "act", [pb[pi]], [xf_buf], lambda: nc.scalar.copy(out=dstf, in_=src))
                dstb = xT_b[:, half * 4:(half + 1) * 4, t * 128:(t + 1) * 128]
                _emit(cx, "dve", [pb[pi]], [xb_buf], lambda: nc.vector.tensor_copy(out=dstb, in_=src))

    def barrier():
        for e in cx.eng:
            for e2 in cx.eng:
                if e2 != e and cx.cnt[e2] > 0:
                    cx._wait(e, (cx.sem[e2], cx.cnt[e2]))
            cx.drain(e)

    def Vv(reads, writes, fn):
        return _emit(cx, "dve", reads, writes, fn)

    def Aa(reads, writes, fn):
        return _emit(cx, "act", reads, writes, fn)

    def Pp(reads, writes, fn):
        return _emit(cx, "pool", reads, writes, fn)

    def load_wblk(tile, tb, src, c0, ncols=512, nk=8):
        cx.dma("sp", tile[:, 0:nk, 0:ncols], src[:, c0:c0 + ncols].rearrange("(kc p) j -> p kc j", p=128),
               reads=[conv], writes=[tb], stream="w")

    def load_xT(x_dram, row0, ntok, xT_f, xT_b, xf_buf, xb_buf, xt_tiles, xt_bufs, psA, psB):
        nt = ntok // 128
        for t in range(nt):
            xt = xt_tiles[t % 2]
            xtb = xt_bufs[t % 2]
            cx.dma("sp", xt[:], x_dram[row0 + t * 128: row0 + (t + 1) * 128, :], writes=[xtb], stream="x")
            for half in range(2):
                pi = psA if half == 0 else psB
                for j in range(4):
                    kc = half * 4 + j
                    transpose(ps[pi][:, j * 128:(j + 1) * 128], xt[:, kc * 128:(kc + 1) * 128], ident[:],
                              [xtb, cB], pb[pi])
                src = ps[pi][:, :].rearrange("p (j t) -> p j t", j=4)
                dstb = xT_b[:, half * 4:(half + 1) * 4, t * 128:(t + 1) * 128]
                if xT_f is not None:
                    dstf = xT_f[:, half * 4:(half + 1) * 4, t * 128:(t + 1) * 128]
                    Aa([pb[pi]], [xf_buf], lambda: nc.scalar.copy(out=dstf, in_=src))
                    Vv([xf_buf], [xb_buf], lambda: nc.vector.tensor_copy(out=dstb, in_=dstf))
                else:
                    Vv([pb[pi]], [xb_buf], lambda: nc.vector.tensor_copy(out=dstb, in_=src))

    xt_tiles = [sb(f"xt{i}", [128, D]) for i in range(2)]
    xt_bufs = [Buf(), Buf()]
    xT_b = sb("xT_b", [128, 8, G], BF16)
    xb_buf = Buf()
    kmT = sb("kmT", [128, 4, 16])
    kmB = Buf()
    kmbd = sb("kmbd", [128, 4, 32])
    ones1 = sb("ones1", [1, 64])
    sel16 = sb("sel16", [16, 16 * 128])
    cx.dma("sp", sel16[:], sel16_in[:, :], writes=[cB])
    Vv([], [cB], lambda: nc.vector.memset(ones1[:], 1.0))

    xsT_f = sb("xsT_f", [128, 8, 4])
    xsfb = Buf()
    xsT_b = sb("xsT_b", [128, 8, 4], BF16)
    xsbb = Buf()
    attnT_s = sb("attnT_s", [64, NH, 4], BF16)
    atsb = Buf()
    pooledT_s = sb("pooledT_s", [128, 4, 4], BF16)
    plsb = Buf()
    with ExitStack() as sS:
        def sbs(name, shape, dt=F32):
            return sS.enter_context(nc.sbuf_tensor("s_" + name, list(shape), dt))
        xs_t = sbs("xs_t", [4, D])
        xsb = Buf()
        wS = [sbs(f"wS{i}", [128, 8, 512], BF16) for i in range(2)]
        wSb = [Buf(), Buf()]
        tok = [sbs(f"tok{i}", [4, 512]) for i in range(6)]
        tokb = [Buf() for _ in range(6)]
        cst = sbs("cst", [4, 2, 512])
        cstb = Buf()
        tmp4 = sbs("tmp4", [4, 512])
        tmp4b = Buf()
        st_t = sbs("st_t", [4, 15, 512])
        stb_ = Buf()
        ssum = sbs("ssum", [4, 512])
        ssb = Buf()
        diff4 = sbs("diff4", [4, 512])
        d4b = Buf()
        diffT_s = sbs("diffT_s", [128, 4, 4], BF16)
        dTsb = Buf()
        wpool_s = sbs("wpool_s", [128, 4, 128], BF16)
        wpsb = Buf()
        pt_sb = sbs("pt_sb", [128, 2], I32)
        idx8 = sbs("idx8", [128, 2, 8], I32)
        ptb = Buf()
        selT = sbs("selT", [4, 256])
        pairsel = sbs("pairsel", [128, 8])
        scb = Buf()
        q_bc = sbs("q_bc", [128, 512])
        qbb = Buf()
        KV = [sbs(f"KV{i}", [128, 8192]) for i in range(2)]
        KVb = [Buf(), Buf()]
        s_all = sbs("s_all", [128, 128, NH])
        sab = Buf()
        Pm = sbs("Pm", [128, 128, NH])
        Pmb = Buf()
        gpage = sbs("gpage", [128, NH])
        gpb = Buf()
        gpT = sbs("gpT", [NH, 128])
        gpTb = Buf()
        gblk = sbs("gblk", [NH, 64])
        gbb = Buf()
        top8s = sbs("top8s", [NH, 8])
        selb_ = sbs("selb", [NH, 64])
        selp = sbs("selp", [NH, 128])
        spb = Buf()
        maskp = sbs("maskp", [128, NH])
        mpb = Buf()
        den = sbs("den", [128, NH])
        denb = Buf()
        Oacc = sbs("Oacc", [128, 512])
        Oab = Buf()
        red = sbs("red", [128, 512])
        redb = Buf()
        snew = sbs("snew", [4, 3, NH])
        snb = Buf()
        Osum = sbs("Osum", [4, 512])
        Osb = Buf()
        attn_tok = sbs("attn_tok", [4, 512])
        atkb = Buf()

        cx.dma("sp", xs_t[:], xs4[:, :], writes=[xsb], stream="c")
        cx.dma("sp", cst[:, 0, :], cs8[:, :], writes=[cstb], stream="c")
        cx.dma("sp", cst[:, 1, :], sn8[:, :], writes=[cstb], stream="c")
        cx.dma("sp", st_t[:], st4[:, :].rearrange("p (r c) -> p r c", r=15), writes=[stb_], stream="c")
        cx.dma("sp", pt_sb[:], pt_in[:, :], writes=[ptb], stream="c")
        cx.dma("sp", selT[:], selT_in[:, :], writes=[scb], stream="c")
        cx.dma("sp", pairsel[:], pairsel_in[:, :], writes=[scb], stream="c")
        cx.dma("sp", wpool_s[:], s_wpool[:, :].rearrange("(g c) e -> c g e", g=4), reads=[conv], writes=[wpsb],
               stream="w")
        for kc in range(8):
            transpose(ps[0][:, kc * 4:(kc + 1) * 4], xs_t[0:4, kc * 128:(kc + 1) * 128], ident[0:4, 0:4],
                      [xsb, cB], pb[0])
        Aa([pb[0]], [xsfb], lambda: nc.scalar.copy(out=xsT_f[:, :, :],
                                                  in_=ps[0][:, 0:32].rearrange("p (k t) -> p k t", k=8)))
        Vv([xsfb], [xsbb], lambda: nc.vector.tensor_copy(out=xsT_b[:, :, :], in_=xsT_f[:, :, :]))
        for i, (srcw, c0) in enumerate(((s_win, 0), (s_wrot, 0), (s_win, 512), (s_wrot, 512), (s_win, 1024),
                                        (s_win, 1536))):
            load_wblk(wS[i % 2], wSb[i % 2], srcw, c0)
            pi = 2 + i % 2
            mm_group(ps[pi][0:4, :], [(xsT_b[:, kc, :], wS[i % 2][:, kc, :]) for kc in range(8)],
                     [wSb[i % 2], xsbb], pb[pi])
            Aa([pb[pi]], [tokb[i]], lambda: nc.scalar.copy(out=tok[i][:], in_=ps[pi][0:4, :]))
        for (a_, r_) in ((0, 1), (2, 3)):
            Vv([tokb[a_], cstb], [tokb[a_]], lambda: nc.vector.tensor_tensor(out=tok[a_][:], in0=tok[a_][:],
                                                                            in1=cst[:, 0, :], op=ALU.mult))
            Vv([tokb[r_], cstb], [tokb[r_]], lambda: nc.vector.tensor_tensor(out=tok[r_][:], in0=tok[r_][:],
                                                                            in1=cst[:, 1, :], op=ALU.mult))
            Vv([tokb[r_]], [tokb[a_]], lambda: nc.vector.tensor_tensor(out=tok[a_][:], in0=tok[a_][:], in1=tok[r_][:],
                                                                      op=ALU.add))
        q_tok, k_tok, v_tok, u_tok = tok[0], tok[2], tok[4], tok[5]
        qtb, ktb, vtb, utb = tokb[0], tokb[2], tokb[4], tokb[5]
        cx.dma("sp", ks_out[:, :], k_tok[:], reads=[ktb], stream="so")
        cx.dma("sp", vs_out[:, :], v_tok[:], reads=[vtb], stream="so")
        cx.dma("sp", ps_out[:, 14 * 512:15 * 512], u_tok[:], reads=[utb], stream="so")
        cx.dma("sp", ps_out[:, 0:14 * 512], st4[:, 512:15 * 512], stream="so")
        for g4, w_ in enumerate((2, 4, 8, 16)):
            c0 = g4 * 128
            Vv([stb_], [ssb], lambda: nc.vector.tensor_reduce(
                out=ssum[:, c0:c0 + 128], in_=st_t[:, 15 - (w_ - 1):15, c0:c0 + 128].rearrange("p r c -> p c r"),
                axis=AX.X, op=ALU.add))
            Vv([ssb, utb], [tmp4b], lambda: nc.vector.tensor_tensor(out=tmp4[:, c0:c0 + 128], in0=ssum[:, c0:c0 + 128],
                                                                   in1=u_tok[:, c0:c0 + 128], op=ALU.add))
            Vv([tmp4b, utb], [d4b], lambda: nc.vector.scalar_tensor_tensor(
                out=diff4[:, c0:c0 + 128], in0=tmp4[:, c0:c0 + 128], scalar=1.0 / w_, in1=u_tok[:, c0:c0 + 128],
                op0=ALU.mult, op1=ALU.subtract))
        for g4 in range(4):
            transpose(ps[1][:, g4 * 4:(g4 + 1) * 4], diff4[0:4, g4 * 128:(g4 + 1) * 128], ident[0:4, 0:4],
                      [d4b, cB], pb[1])
        Vv([pb[1]], [dTsb], lambda: nc.vector.tensor_copy(out=diffT_s[:, :, :],
                                                         in_=ps[1][:, 0:16].rearrange("p (g t) -> p g t", g=4)))
        for g4 in range(4):
            mm_group(ps[2][:, 0:4], [(wpool_s[:, g4, :], diffT_s[:, g4, :])], [wpsb, dTsb], pb[2])
            Vv([pb[2], cB], [plsb], lambda: nc.vector.tensor_scalar(out=pooledT_s[:, g4, :], in0=ps[2][:, 0:4],
                                                                   scalar1=fp[:, 48 + g4:49 + g4], scalar2=None,
                                                                   op0=ALU.mult))
        for c in range(8):
            Vv([ptb], [ptb], lambda: nc.vector.tensor_scalar(out=idx8[:, :, c], in0=pt_sb[:, :], scalar1=8.0,
                                                            scalar2=float(c), op0=ALU.mult, op1=ALU.add))
        nbuf = 0
        for t in range(2):
            mm_group(ps[3][:, :], [(selT[0:4, t * 128:(t + 1) * 128], q_tok[0:4, :])], [scb, qtb], pb[3])
            Aa([pb[3]], [qbb], lambda: nc.scalar.copy(out=q_bc[:], in_=ps[3][:, :]))
            for c in range(8):
                kb_ = nbuf % 2
                nbuf += 1
                cx.deps("pool", [ptb], [KVb[kb_]])
                ins = nc.gpsimd.indirect_dma_start(out=KV[kb_][:, :], out_offset=None, in_=ck[:, :],
                                                   in_offset=bass.IndirectOffsetOnAxis(ap=idx8[:, t, c:c + 1], axis=0))
                cx.dma_done("pool", ins, [ptb], [KVb[kb_]], "g")
                Pp([qbb], [KVb[kb_]], lambda: nc.gpsimd.tensor_tensor(
                    out=KV[kb_][:, :].rearrange("p (r e) -> p r e", r=16),
                    in0=KV[kb_][:, :].rearrange("p (r e) -> p r e", r=16),
                    in1=q_bc[:, :].rearrange("p (o e) -> p o e", o=1).broadcast_to([128, 16, 512]), op=ALU.mult))
                Vv([KVb[kb_]], [sab], lambda: nc.vector.tensor_reduce(
                    out=s_all[:, c * 16:(c + 1) * 16, :].rearrange("p r h -> p (r h)"),
                    in_=KV[kb_][:, :].rearrange("p (a d) -> p a d", d=HD), axis=AX.X, op=ALU.add))
            Vv([sab], [gpb], lambda: nc.vector.tensor_reduce(out=gpage[:, :], in_=s_all[:, :, :].rearrange("p r h -> p h r"),
                                                            axis=AX.X, op=ALU.add))
            transpose(ps[4][0:NH, 0:128], gpage[:, :], ident[:], [gpb, cB], pb[4])
            Aa([pb[4]], [gpTb], lambda: nc.scalar.copy(out=gpT[:, :], in_=ps[4][0:NH, 0:128]))
            gv = gpT[:, :].rearrange("h (n two) -> h n two", two=2)
            Vv([gpTb], [gbb], lambda: nc.vector.tensor_tensor(out=gblk[:, :], in0=gv[:, :, 0], in1=gv[:, :, 1], op=ALU.add))
            for s2 in range(2):
                Vv([gbb], [gbb], lambda: nc.vector.max(out=top8s[:, :], in_=gblk[:, s2 * 32:(s2 + 1) * 32]))
                Vv([gbb], [gbb], lambda: nc.vector.tensor_scalar(out=selb_[:, s2 * 32:(s2 + 1) * 32],
                                                                in0=gblk[:, s2 * 32:(s2 + 1) * 32],
                                                                scalar1=top8s[:, 2:3], scalar2=None, op0=ALU.is_ge))
            sv = selp[:, :].rearrange("h (n two) -> h n two", two=2)
            Vv([gbb], [spb], lambda: nc.vector.tensor_copy(out=sv[:, :, 0], in_=selb_[:, :]))
            Vv([gbb], [spb], lambda: nc.vector.tensor_copy(out=sv[:, :, 1], in_=selb_[:, :]))
            transpose(ps[4][:, 128:128 + NH], selp[:, :], ident[0:NH, 0:NH], [spb, cB], pb[4])
            Aa([pb[4]], [mpb], lambda: nc.scalar.copy(out=maskp[:, :], in_=ps[4][:, 128:128 + NH]))
            Aa([sab], [Pmb], lambda: nc.scalar.activation(out=Pm[:, :, :], in_=s_all[:, :, :], func=AF.Exp, scale=0.125))
            Vv([mpb], [Pmb], lambda: nc.vector.tensor_tensor(
                out=Pm[:, :, :], in0=Pm[:, :, :],
                in1=maskp[:, :].rearrange("p (o h) -> p o h", o=1).broadcast_to([128, 128, NH]), op=ALU.mult))
            Vv([Pmb], [denb], lambda: nc.vector.tensor_reduce(out=den[:, :], in_=Pm[:, :, :].rearrange("p r h -> p h r"),
                                                             axis=AX.X, op=ALU.add))
            Vv([], [Oab], lambda: nc.vector.memset(Oacc[:], 0.0))
            for c in range(8):
                kb_ = nbuf % 2
                nbuf += 1
                cx.deps("pool", [ptb], [KVb[kb_]])
                ins = nc.gpsimd.indirect_dma_start(out=KV[kb_][:, :], out_offset=None, in_=cv[:, :],
                                                   in_offset=bass.IndirectOffsetOnAxis(ap=idx8[:, t, c:c + 1], axis=0))
                cx.dma_done("pool", ins, [ptb], [KVb[kb_]], "g")
                Pp([Pmb], [KVb[kb_]], lambda: nc.gpsimd.tensor_tensor(
                    out=KV[kb_][:, :].rearrange("p (r h d) -> p r h d", r=16, h=NH),
                    in0=KV[kb_][:, :].rearrange("p (r h d) -> p r h d", r=16, h=NH),
                    in1=Pm[:, c * 16:(c + 1) * 16, :].rearrange("p r (h o) -> p r h o", o=1).broadcast_to([128, 16, NH, HD]),
                    op=ALU.mult))
                Vv([KVb[kb_]], [redb], lambda: nc.vector.tensor_reduce(
                    out=red[:, :], in_=KV[kb_][:, :].rearrange("p (r e) -> p e r", r=16), axis=AX.X, op=ALU.add))
                Vv([redb], [Oab], lambda: nc.vector.tensor_tensor(out=Oacc[:], in0=Oacc[:], in1=red[:], op=ALU.add))
            cx.deps("pe", [scb, Oab], [pb[5]] if t == 0 else [])
            ins = nc.tensor.matmul(ps[5][0:4, :], pairsel[:, t * 4:(t + 1) * 4], Oacc[:, :], start=(t == 0), stop=(t == 1))
            cx.op("pe", ins, [scb, Oab], [pb[5]] if t == 1 else [])
            cx.deps("pe", [scb, denb], [pb[6]] if t == 0 else [])
            ins = nc.tensor.matmul(ps[6][0:4, 0:NH], pairsel[:, t * 4:(t + 1) * 4], den[:, :], start=(t == 0), stop=(t == 1))
            cx.op("pe", ins, [scb, denb], [pb[6]] if t == 1 else [])
        Vv([qtb, ktb], [tmp4b], lambda: nc.vector.tensor_tensor(out=tmp4[:], in0=q_tok[:], in1=k_tok[:], op=ALU.mult))
        Vv([tmp4b], [snb], lambda: nc.vector.tensor_reduce(out=snew[:, 0, :], in_=tmp4[:, :].rearrange("p (h d) -> p h d", h=NH),
                                                          axis=AX.X, op=ALU.add))
        Aa([snb], [snb], lambda: nc.scalar.activation(out=snew[:, 1, :], in_=snew[:, 0, :], func=AF.Exp, scale=0.125))
        Vv([snb, vtb], [tmp4b], lambda: nc.vector.tensor_tensor(
            out=tmp4[:, :].rearrange("p (h d) -> p h d", h=NH), in0=v_tok[:, :].rearrange("p (h d) -> p h d", h=NH),
            in1=snew[:, 1, :].rearrange("p (h o) -> p h o", o=1).broadcast_to([4, NH, HD]), op=ALU.mult))
        Vv([pb[5], tmp4b], [Osb], lambda: nc.vector.tensor_tensor(out=Osum[:], in0=ps[5][0:4, :], in1=tmp4[:], op=ALU.add))
        Vv([pb[6], snb], [snb], lambda: nc.vector.tensor_tensor(out=snew[:, 2, :], in0=ps[6][0:4, 0:NH], in1=snew[:, 1, :],
                                                               op=ALU.add))
        Vv([snb], [snb], lambda: nc.vector.reciprocal(out=snew[:, 2, :], in_=snew[:, 2, :]))
        Vv([Osb, snb], [atkb], lambda: nc.vector.tensor_tensor(
            out=attn_tok[:, :].rearrange("p (h d) -> p h d", h=NH), in0=Osum[:, :].rearrange("p (h d) -> p h d", h=NH),
            in1=snew[:, 2, :].rearrange("p (h o) -> p h o", o=1).broadcast_to([4, NH, HD]), op=ALU.mult))
        for h in range(NH):
            transpose(ps[7][0:64, h * 4:(h + 1) * 4], attn_tok[0:4, h * 64:(h + 1) * 64], ident[0:4, 0:4],
                      [atkb, cB], pb[7])
        Aa([pb[7]], [atsb], lambda: nc.scalar.copy(out=attnT_s[:, :, :],
                                                  in_=ps[7][0:64, 0:32].rearrange("p (h t) -> p h t", h=NH)))
        barrier()
    if STOP == 2:
        cx.drain("sp")
        return

    with ExitStack() as s1:
        def sb1(name, shape, dt=F32):
            return s1.enter_context(nc.sbuf_tensor("p1_" + name, list(shape), dt))
        wk = sb1("wk", [128, 8, 512], BF16)
        wkr = sb1("wkr", [128, 8, 512], BF16)
        wv = sb1("wv", [128, 8, 512], BF16)
        wB = Buf()
        for (t_, c0, srcw) in ((wk, 512, s_win), (wkr, 512, s_wrot), (wv, 1024, s_win)):
            cx.dma("sp", t_[:], srcw[:, c0:c0 + 512].rearrange("(kc p) j -> p kc j", p=128), reads=[conv],
                   writes=[wB], stream="w")
        cs_t = sb1("cs_t", [128, G])
        sn_t = sb1("sn_t", [128, G])
        csB = Buf()
        kT_f = sb1("kT_f", [128, G])
        kT_fb = Buf()
        kT_h = sb1("kT_h", [128, G], BF16)
        kT_hb = Buf()
        t1 = sb1("t1", [128, G])
        t1b = Buf()
        ktok = sb1("ktok", [128, 512])
        ktokb = Buf()
        vtok = sb1("vtok", [128, 512])
        vtokb = Buf()
        vaug = sb1("vaug", [128, NH, 4, 66], BF16)
        vaugb = Buf()
        Pp([], [vaugb], lambda: nc.gpsimd.memset(vaug[:], 1.0))

        for g in range(NG):
            load_xT(xseq, g * G, G, None, xT_b, None, xb_buf, xt_tiles, xt_bufs, 0, 1)
            cx.dma("sp", cs_t[:], cosk[:, g * G:(g + 1) * G], writes=[csB], stream="c")
            cx.dma("sp", sn_t[:], sink[:, g * G:(g + 1) * G], writes=[csB], stream="c")
            for c in range(4):
                mm_group(ps[2][:, :], [(wk[:, kc, c * 128:(c + 1) * 128], xT_b[:, kc, :]) for kc in range(8)],
                         [wB, xb_buf], pb[2])
                mm_group(ps[3][:, :], [(wkr[:, kc, c * 128:(c + 1) * 128], xT_b[:, kc, :]) for kc in range(8)],
                         [wB, xb_buf], pb[3])
                Vv([pb[2], csB], [t1b],
                   lambda: nc.vector.tensor_tensor(out=t1[:], in0=ps[2][:, :], in1=cs_t[:], op=ALU.mult))
                Vv([pb[3], csB], [kT_fb],
                   lambda: nc.vector.tensor_tensor(out=kT_f[:], in0=ps[3][:, :], in1=sn_t[:], op=ALU.mult))
                Vv([t1b], [kT_fb],
                   lambda: nc.vector.tensor_tensor(out=kT_f[:], in0=kT_f[:], in1=t1[:], op=ALU.add))
                Aa([kT_fb], [kT_hb], lambda: nc.scalar.copy(out=kT_h[:], in_=kT_f[:]))
                cx.dma("sp", s_kT[2 * c:2 * c + 2, :, g * G:(g + 1) * G].rearrange("h d t -> (h d) t"), kT_h[:],
                       reads=[kT_hb], stream="ks")
                Vv([kT_fb], [kmB],
                   lambda: nc.vector.tensor_reduce(out=kmT[:, c, 2 * g:2 * g + 2],
                                                   in_=kT_f[:, :].rearrange("p (b t) -> p b t", b=2),
                                                   axis=AX.X, op=ALU.add))
                for t in range(4):
                    transpose(ps[4][:, t * 128:(t + 1) * 128], kT_f[:, t * 128:(t + 1) * 128], ident[:],
                              [kT_fb, cB], pb[4])
                Aa([pb[4]], [ktokb], lambda: nc.scalar.copy(out=ktok[:, :], in_=ps[4][:, :]))
                cx.dma("sp", k_out[g * G:(g + 1) * G, c * 128:(c + 1) * 128].rearrange("(t p) j -> p t j", p=128),
                       ktok[:, :].rearrange("p (t j) -> p t j", t=4), reads=[ktokb], stream="ko")
            for t in range(4):
                mm_group(ps[5][:, :], [(xT_b[:, kc, t * 128:(t + 1) * 128], wv[:, kc, :]) for kc in range(8)],
                         [wB, xb_buf], pb[5])
                Aa([pb[5]], [vtokb], lambda: nc.scalar.copy(out=vtok[:], in_=ps[5][:, :]))
                Vv([vtokb], [vaugb],
                   lambda: nc.vector.tensor_copy(out=vaug[:, :, t, 0:64],
                                                 in_=vtok[:, :].rearrange("p (h d) -> p h d", h=NH)))
                cx.dma("sp", v_out[g * G + t * 128: g * G + (t + 1) * 128, :], vtok[:], reads=[vtokb], stream="vo")
            for h in range(NH):
                cx.dma("sp", s_v[h, :, g * 4:(g + 1) * 4, :], vaug[:, h, :, :], reads=[vaugb], stream="vs")
        Vv([], [kmB], lambda: nc.vector.memset(kmbd[:], 0.0))
        Vv([kmB], [kmB], lambda: nc.vector.tensor_scalar(out=kmbd[0:64, :, 0:16], in0=kmT[0:64, :, :],
                                                         scalar1=1.0 / 256, scalar2=None, op0=ALU.mult))
        Vv([kmB], [kmB], lambda: nc.vector.tensor_scalar(out=kmbd[64:128, :, 16:32], in0=kmT[64:128, :, :],
                                                         scalar1=1.0 / 256, scalar2=None, op0=ALU.mult))
        barrier()
    if STOP == 1:
        cx.drain("sp")
        return

    def layer_norm(zT, zb, gcol, bcol, outf, outfb, outb, outbb, tmp, tmpb, mean_sb, rstd_sb, stb, T):
        for oc in range(8):
            Aa([zb], [tmpb], lambda: nc.scalar.activation(out=tmp[:, 0:T], in_=zT[:, oc, 0:T], func=AF.Square))
            cx.deps("pe", [zb, cB], [pb[6]] if oc == 0 else [])
            ins = nc.tensor.matmul(ps[6][:, 0:T], ones_ln[:], zT[:, oc, 0:T], start=(oc == 0), stop=(oc == 7))
            cx.op("pe", ins, [zb, cB], [pb[6]] if oc == 7 else [])
            cx.deps("pe", [tmpb], [pb[7]] if oc == 0 else [])
            ins = nc.tensor.matmul(ps[7][:, 0:T], ones_ln[:], tmp[:, 0:T], start=(oc == 0), stop=(oc == 7))
            cx.op("pe", ins, [tmpb], [pb[7]] if oc == 7 else [])
        Aa([pb[6]], [stb], lambda: nc.scalar.copy(out=mean_sb[:, 0:T], in_=ps[6][:, 0:T]))
        Vv([stb], [tmpb], lambda: nc.vector.tensor_tensor(out=tmp[:, 0:T], in0=mean_sb[:, 0:T], in1=mean_sb[:, 0:T],
                                                         op=ALU.mult))
        Vv([pb[7], tmpb], [tmpb], lambda: nc.vector.tensor_tensor(out=tmp[:, 0:T], in0=ps[7][:, 0:T], in1=tmp[:, 0:T],
                                                                 op=ALU.subtract))
        Aa([tmpb], [tmpb], lambda: nc.scalar.activation(out=tmp[:, 0:T], in_=tmp[:, 0:T], func=AF.Sqrt, bias=eps_t[:, 0:1]))
        Vv([tmpb], [stb], lambda: nc.vector.reciprocal(out=rstd_sb[:, 0:T], in_=tmp[:, 0:T]))
        for oc in range(8):
            Vv([zb, stb], [tmpb], lambda: nc.vector.tensor_tensor(out=tmp[:, 0:T], in0=zT[:, oc, 0:T], in1=mean_sb[:, 0:T],
                                                                 op=ALU.subtract))
            Vv([stb], [tmpb], lambda: nc.vector.tensor_tensor(out=tmp[:, 0:T], in0=tmp[:, 0:T], in1=rstd_sb[:, 0:T],
                                                             op=ALU.mult))
            Vv([tmpb, cB], [outfb], lambda: nc.vector.tensor_scalar(out=outf[:, oc, 0:T], in0=tmp[:, 0:T],
                                                                   scalar1=fp[:, gcol + oc:gcol + oc + 1],
                                                                   scalar2=fp[:, bcol + oc:bcol + oc + 1],
                                                                   op0=ALU.mult, op1=ALU.add))
            if outb is not None:
                Aa([outfb], [outbb], lambda: nc.scalar.copy(out=outb[:, oc, 0:T], in_=outf[:, oc, 0:T]))

    eps_t = sb("eps_t", [128, 1])
    Vv([], [cB], lambda: nc.vector.memset(eps_t[:], LN_EPS))
    x1T_f = sb("x1T_f", [128, 8, G])
    x1fb = Buf()
    x1T_b = sb("x1T_b", [128, 8, G], BF16)
    x1bb = Buf()
    tmp = sb("ln_tmp", [128, G])
    tmpb = Buf()
    mean_sb = sb("mean_sb", [128, G])
    rstd_sb = sb("rstd_sb", [128, G])
    stb = Buf()

    def moe_and_out(sbb, row0, T, y_dst):
        nt = min(128, T)
        ntile = T // nt
        Lg = sbb("Lg", [128, 20])
        rs = sbb("rs", [128, 64])
        esel = sbb("esel", [128, 8])
        top8e = sbb("top8e", [128, 8])
        comb = sbb("comb", [128, 16])
        rb = Buf()
        combT = sbb("combT", [16, G])
        cTb = Buf()
        bce = sbb("bce", [128, G])
        bceb = Buf()
        wg = [sbb(f"wg{i}", [128, 8, DE], BF16) for i in range(2)]
        wu = [sbb(f"wu{i}", [128, 8, DE], BF16) for i in range(2)]
        wgb = [Buf(), Buf()]
        wub = [Buf(), Buf()]
        silt = [sbb(f"silt{i}", [128, G]) for i in range(2)]
        siltb = [Buf(), Buf()]
        hT = sbb("hT", [128, 32, G], BF16)
        hTb = Buf()
        wdblk = sbb("wdblk", [128, 32, 512], BF16)
        wdb = Buf()
        otile = sbb("otile", [128, D])
        otb = Buf()

        Vv([], [rb], lambda: nc.vector.memset(esel[:], NEG))
        for t in range(ntile):
            ts_ = slice(t * nt, (t + 1) * nt)
            P_ = slice(0, nt)
            cx.deps("pe", [x1fb, cB], [pb[0]])
            ins = None
            for kc in range(8):
                ins = nc.tensor.matmul(ps[0][P_, 0:20], x1T_f[:, kc, ts_], wr_sb[:, kc * 20:(kc + 1) * 20],
                                       start=(kc == 0), stop=(kc == 7))
            cx.op("pe", ins, [x1fb, cB], [pb[0]])
            Vv([pb[0], cB], [rb], lambda: nc.vector.tensor_tensor(out=Lg[P_, :], in0=ps[0][P_, 0:20], in1=br_sb[P_, :],
                                                                 op=ALU.add))
            Vv([rb], [rb], lambda: nc.vector.tensor_reduce(out=rs[P_, 0:1], in_=Lg[P_, 0:4], axis=AX.X, op=ALU.max))
            Vv([rb], [rb], lambda: nc.vector.tensor_scalar(out=rs[P_, 4:8], in0=Lg[P_, 0:4], scalar1=rs[P_, 0:1],
                                                          scalar2=None, op0=ALU.is_ge))
            Vv([rb], [rb], lambda: nc.vector.tensor_scalar(out=rs[P_, 1:2], in0=rs[P_, 0:1], scalar1=-1.0,
                                                          scalar2=None, op0=ALU.mult))
            Aa([rb], [rb], lambda: nc.scalar.activation(out=rs[P_, 8:12], in_=Lg[P_, 0:4], func=AF.Exp,
                                                       bias=rs[P_, 1:2], accum_out=rs[P_, 2:3]))
            Vv([rb], [rb], lambda: nc.vector.reciprocal(out=rs[P_, 3:4], in_=rs[P_, 2:3]))
            Vv([rb], [rb], lambda: nc.vector.tensor_scalar(out=esel[P_, 0:4], in0=Lg[P_, 4:8], scalar1=rs[P_, 4:5],
                                                          scalar2=None, op0=ALU.mult))
            for g4 in range(1, 4):
                Vv([rb], [rb], lambda: nc.vector.scalar_tensor_tensor(out=esel[P_, 0:4], in0=Lg[P_, 4 + 4 * g4:8 + 4 * g4],
                                                                     scalar=rs[P_, 4 + g4:5 + g4], in1=esel[P_, 0:4],
                                                                     op0=ALU.mult, op1=ALU.add))
            Vv([rb], [rb], lambda: nc.vector.max(out=top8e[P_, :], in_=esel[P_, :]))
            Vv([rb], [rb], lambda: nc.vector.tensor_tensor(out=rs[P_, 12:13], in0=top8e[P_, 0:1], in1=top8e[P_, 1:2],
                                                          op=ALU.subtract))
            Aa([rb], [rb], lambda: nc.scalar.activation(out=rs[P_, 13:14], in_=rs[P_, 12:13], func=AF.Sigmoid))
            Vv([rb], [rb], lambda: nc.vector.tensor_tensor(out=rs[P_, 14:15], in0=rs[P_, 13:14], in1=rs[P_, 3:4],
                                                          op=ALU.mult))
            Vv([rb], [rb], lambda: nc.vector.tensor_tensor(out=rs[P_, 15:16], in0=rs[P_, 3:4], in1=rs[P_, 14:15],
                                                          op=ALU.subtract))
            Vv([rb], [rb], lambda: nc.vector.tensor_tensor(out=rs[P_, 16:17], in0=rs[P_, 14:15], in1=rs[P_, 15:16],
                                                          op=ALU.subtract))
            Vv([rb], [rb], lambda: nc.vector.tensor_scalar(out=rs[P_, 20:24], in0=esel[P_, 0:4], scalar1=top8e[P_, 0:1],
                                                          scalar2=None, op0=ALU.is_ge))
            Vv([rb], [rb], lambda: nc.vector.tensor_scalar(out=rs[P_, 24:28], in0=esel[P_, 0:4], scalar1=top8e[P_, 1:2],
                                                          scalar2=None, op0=ALU.is_ge))
            Vv([rb], [rb], lambda: nc.vector.tensor_scalar(out=rs[P_, 28:32], in0=rs[P_, 24:28], scalar1=rs[P_, 15:16],
                                                          scalar2=None, op0=ALU.mult))
            Vv([rb], [rb], lambda: nc.vector.scalar_tensor_tensor(out=rs[P_, 28:32], in0=rs[P_, 20:24],
                                                                 scalar=rs[P_, 16:17], in1=rs[P_, 28:32],
                                                                 op0=ALU.mult, op1=ALU.add))
            for g4 in range(4):
                Vv([rb], [rb], lambda: nc.vector.tensor_scalar(out=comb[P_, 4 * g4:4 * g4 + 4], in0=rs[P_, 28:32],
                                                              scalar1=rs[P_, 4 + g4:5 + g4], scalar2=None,
                                                              op0=ALU.mult))
            transpose(ps[1][0:16, ts_], comb[P_, :], ident[P_, P_], [rb, cB], pb[1])
        Aa([pb[1]], [cTb], lambda: nc.scalar.copy(out=combT[:, 0:T], in_=ps[1][0:16, 0:T]))

        for e in range(NE):
            i2 = e % 2
            cx.dma("sp", wg[i2][:], s_eg[e * D:(e + 1) * D, :].rearrange("(kc p) f -> p kc f", p=128), reads=[conv],
                   writes=[wgb[i2]], stream="we")
            cx.dma("sp", wu[i2][:], s_eu[e * D:(e + 1) * D, :].rearrange("(kc p) f -> p kc f", p=128), reads=[conv],
                   writes=[wub[i2]], stream="we")
            mm_group(ps[2][:, 0:T], [(sel16[0:16, e * 128:(e + 1) * 128], combT[0:16, 0:T])], [cTb, cB], pb[2])
            Aa([pb[2]], [bceb], lambda: nc.scalar.copy(out=bce[:, 0:T], in_=ps[2][:, 0:T]))
            for fc in range(2):
                j = e * 2 + fc
                pg, pu, sj = 3 + j % 2, 5 + j % 2, j % 2
                fs = slice(fc * 128, (fc + 1) * 128)
                mm_group(ps[pg][:, 0:T], [(wg[i2][:, kc, fs], x1T_b[:, kc, 0:T]) for kc in range(8)],
                         [wgb[i2], x1bb], pb[pg])
                mm_group(ps[pu][:, 0:T], [(wu[i2][:, kc, fs], x1T_b[:, kc, 0:T]) for kc in range(8)],
                         [wub[i2], x1bb], pb[pu])
                Aa([pb[pg]], [siltb[sj]], lambda: nc.scalar.activation(out=silt[sj][:, 0:T], in_=ps[pg][:, 0:T],
                                                                      func=AF.Silu))
                Pp([bceb], [siltb[sj]], lambda: nc.gpsimd.tensor_tensor(out=silt[sj][:, 0:T], in0=silt[sj][:, 0:T],
                                                                       in1=bce[:, 0:T], op=ALU.mult))
                Vv([pb[pu], siltb[sj]], [hTb], lambda: nc.vector.tensor_tensor(out=hT[:, j, 0:T], in0=ps[pu][:, 0:T],
                                                                              in1=silt[sj][:, 0:T], op=ALU.mult))
        for half in range(2):
            cx.dma("sp", wdblk[:], s_ed[:, half * 512:(half + 1) * 512].rearrange("(j p) o -> p j o", p=128),
                   reads=[conv], writes=[wdb], stream="we")
            for o4 in range(4):
                oc = half * 4 + o4
                pi = oc % 2
                mm_group(ps[pi][:, 0:T], [(wdblk[:, j, o4 * 128:(o4 + 1) * 128], hT[:, j, 0:T]) for j in range(32)],
                         [wdb, hTb], pb[pi])
                Vv([pb[pi], x1fb], [x1fb],
                   lambda: nc.vector.scalar_tensor_tensor(out=x1T_f[:, oc, 0:T], in0=x1T_f[:, oc, 0:T], scalar=ALPHA,
                                                          in1=ps[pi][:, 0:T], op0=ALU.mult, op1=ALU.add))
        layer_norm(x1T_f, x1fb, 16, 24, x1T_f, x1fb, None, None, tmp, tmpb, mean_sb, rstd_sb, stb, T)
        for t in range(ntile):
            ts_ = slice(t * nt, (t + 1) * nt)
            for half in range(2):
                pi = 3 + half
                for j in range(4):
                    oc = half * 4 + j
                    transpose(ps[pi][0:nt, j * 128:(j + 1) * 128], x1T_f[:, oc, ts_], ident[:], [x1fb, cB], pb[pi])
                if half == 0:
                    Aa([pb[pi]], [otb], lambda: nc.scalar.copy(out=otile[0:nt, 0:512], in_=ps[pi][0:nt, :]))
                else:
                    Vv([pb[pi]], [otb], lambda: nc.vector.tensor_copy(out=otile[0:nt, 512:1024], in_=ps[pi][0:nt, :]))
            cx.dma("sp", y_dst[row0 + t * nt: row0 + (t + 1) * nt, :], otile[0:nt, :], reads=[otb], stream="yo")

    def merge_ln1(sba, T, xT_f, xfb, xTb, xb_buf, attnT, atb, pooledT, plb):
        wblk = [sba(f"m_wblk{i}", [128, 8, 512], BF16) for i in range(2)]
        wblkb = [Buf(), Buf()]
        wa_t = sba("wa_t", [64, NH, 512], BF16)
        wab = Buf()
        wb_t = sba("wb_t", [128, 4, 512], BF16)
        wbb = Buf()
        sga = sba("m_sga", [128, T])
        sgab = Buf()
        sgb = sba("m_sgb", [128, T])
        sgbb = Buf()
        t1 = sba("m_t1", [128, T])
        t1b = Buf()
        t2 = sba("m_t2", [128, T])
        t2b = Buf()
        mergedT = sba("mergedT", [128, 8, T], BF16)
        mgb = Buf()
        for half in range(2):
            load_wblk(wblk[0], wblkb[0], s_win, 2048 + half * 512)
            load_wblk(wblk[1], wblkb[1], s_win, 3072 + half * 512)
            cx.dma("sp", wa_t[:], s_wa[:, half * 512:(half + 1) * 512].rearrange("(h d) o -> d h o", h=NH),
                   reads=[conv], writes=[wab], stream="w")
            cx.dma("sp", wb_t[:], s_wb[:, half * 512:(half + 1) * 512].rearrange("(g c) o -> c g o", g=4),
                   reads=[conv], writes=[wbb], stream="w")
            for o4 in range(4):
                oc = half * 4 + o4
                cs_ = slice(o4 * 128, (o4 + 1) * 128)
                mm_group(ps[2][:, 0:T], [(wblk[0][:, kc, cs_], xTb[:, kc, 0:T]) for kc in range(8)],
                         [wblkb[0], xb_buf], pb[2])
                mm_group(ps[3][:, 0:T], [(wblk[1][:, kc, cs_], xTb[:, kc, 0:T]) for kc in range(8)],
                         [wblkb[1], xb_buf], pb[3])
                Aa([pb[2], cB], [sgab], lambda: nc.scalar.activation(out=sga[:], in_=ps[2][:, 0:T], func=AF.Sigmoid,
                                                                    bias=fp[:, 32 + oc:33 + oc]))
                Aa([pb[3], cB], [sgbb], lambda: nc.scalar.activation(out=sgb[:], in_=ps[3][:, 0:T], func=AF.Sigmoid,
                                                                    bias=fp[:, 40 + oc:41 + oc]))
                mm_group(ps[0][:, 0:T], [(wa_t[0:64, h, cs_], attnT[0:64, h, 0:T]) for h in range(NH)],
                         [wab, atb], pb[0])
                mm_group(ps[1][:, 0:T], [(wb_t[:, g4, cs_], pooledT[:, g4, 0:T]) for g4 in range(4)],
                         [wbb, plb], pb[1])
                Vv([pb[0], sgab], [t1b], lambda: nc.vector.tensor_tensor(out=t1[:], in0=ps[0][:, 0:T], in1=sga[:],
                                                                        op=ALU.mult))
                Vv([pb[1], sgbb], [t2b], lambda: nc.vector.tensor_tensor(out=t2[:], in0=ps[1][:, 0:T], in1=sgb[:],
                                                                        op=ALU.mult))
                Vv([t1b, t2b], [mgb], lambda: nc.vector.tensor_tensor(out=mergedT[:, oc, :], in0=t1[:], in1=t2[:],
                                                                     op=ALU.add))
        for half in range(2):
            load_wblk(wblk[half], wblkb[half], s_wout, half * 512)
            for o4 in range(4):
                oc = half * 4 + o4
                pi = 2 + (oc % 2)
                mm_group(ps[pi][:, 0:T], [(wblk[half][:, kc, o4 * 128:(o4 + 1) * 128], mergedT[:, kc, :])
                                          for kc in range(8)], [wblkb[half], mgb], pb[pi])
                Vv([pb[pi], xfb], [xfb],
                   lambda: nc.vector.scalar_tensor_tensor(out=xT_f[:, oc, 0:T], in0=xT_f[:, oc, 0:T], scalar=ALPHA,
                                                          in1=ps[pi][:, 0:T], op0=ALU.mult, op1=ALU.add))
        layer_norm(xT_f, xfb, 0, 8, x1T_f, x1fb, x1T_b, x1bb, tmp, tmpb, mean_sb, rstd_sb, stb, T)

    for slot in range(NSLOT):
        cx.epoch()
        nkt = 8 * (slot + 1)
        with ExitStack() as sA:
            def sba(name, shape, dt=F32):
                return sA.enter_context(nc.sbuf_tensor(f"a{slot}_" + name, list(shape), dt))
            xT_f = sba("xT_f", [128, 8, G])
            xfb = Buf()
            attnT = sba("attnT", [64, NH, G], BF16)
            atb = Buf()
            pooledT = sba("pooledT", [128, 4, G], BF16)
            plb = Buf()
            sA1 = ExitStack()

            def sba1(name, shape, dt=F32):
                return sA1.enter_context(nc.sbuf_tensor(f"a1{slot}_" + name, list(shape), dt))
            wblk = [sba1(f"wblk{i}", [128, 8, 512], BF16) for i in range(2)]
            wblkb = [Buf(), Buf()]
            cq_t = sba1("cq_t", [128, G])
            sq_t = sba1("sq_t", [128, G])
            cqb = Buf()
            qT_f = sba1("qT_f", [128, 4, G])
            qfb = Buf()
            qaug = sba1("qaug", [80, NH, G], BF16)
            qab = Buf()
            t1 = sba1("t1", [128, G])
            t1b = Buf()
            kaug0 = sba1("kaug0", [80, SEQ], BF16)
            kaug = [kaug0, kaug0]
            kab0 = Buf()
            kab = [kab0, kab0]
            vh = [sba1(f"vh{i}", [128, 32, 66], BF16) for i in range(2)]
            vhb = [Buf(), Buf()]
            pT = [sba1(f"pT{i}", [128, G], BF16) for i in range(3)]
            pTb = [Buf(), Buf(), Buf()]
            cmk = sba1("cmk", [128, 8, G], BF16)
            cmkb = Buf()
            rden = sba1("rden", [65, G])
            rdb = Buf()
            rden0 = sba1("rden0", [1, G])
            rd0b = Buf()
            bcs = sba1("bcs", [64, G])
            bcsb = Buf()
            gm = sba1("gm", [128, 128])
            gmb = Buf()
            msk = sba1("msk", [128, 3, 128])
            mskb = Buf()
            top8 = sba1("top8", [128, NH, 8])
            top8b = Buf()
            sel = sba1("sel", [128, 128])
            selb = Buf()
            biasw = sba1("biasw", [128, NH, 32])
            bwb = Buf()
            uT = sba1("uT", [128, 4, 16 + G])
            uTb = Buf()
            xh = sba1("xh", [16, D])
            xhb = Buf()
            xhT = sba1("xhT", [128, 8, 16], BF16)
            xhTb = Buf()
            pa = sba1("pa", [128, 16 + G])
            pab = Buf()
            pbt = sba1("pbt", [128, 16 + G])
            pbb = Buf()
            invt = sba1("invt", [128, G])
            invb = Buf()
            diffT = sba1("diffT", [128, G], BF16)
            dfb = Buf()
            wpool_t = sba1("wpool_t", [128, 4, 128], BF16)
            wpb = Buf()
            sga = sba1("sga", [128, G])
            sgab = Buf()

            row0 = slot * G
            load_xT(xown, row0, G, xT_f, xT_b, xfb, xb_buf, xt_tiles, xt_bufs, 0, 1)
            cx.dma("sp", cq_t[:], cosq[:, row0:row0 + G], writes=[cqb], stream="c")
            cx.dma("sp", sq_t[:], sinq[:, row0:row0 + G], writes=[cqb], stream="c")
            cx.dma("sp", cmk[:], cmask[slot % 2, :, :].rearrange("p (j t) -> p j t", j=8), writes=[cmkb], stream="c")
            Vv([], [bwb], lambda: nc.vector.memset(biasw[:], 0.0))
            Vv([], [pab], lambda: nc.vector.memset(pa[:], 0.0))
            Vv([], [pbb], lambda: nc.vector.memset(pbt[:], 0.0))

            load_wblk(wblk[0], wblkb[0], s_win, 0)
            load_wblk(wblk[1], wblkb[1], s_wrot, 0)
            for c in range(4):
                mm_group(ps[2][:, :], [(wblk[0][:, kc, c * 128:(c + 1) * 128], xT_b[:, kc, :]) for kc in range(8)],
                         [wblkb[0], xb_buf], pb[2])
                mm_group(ps[3][:, :], [(wblk[1][:, kc, c * 128:(c + 1) * 128], xT_b[:, kc, :]) for kc in range(8)],
                         [wblkb[1], xb_buf], pb[3])
                Vv([pb[2], cqb], [t1b],
                   lambda: nc.vector.tensor_tensor(out=t1[:], in0=ps[2][:, :], in1=cq_t[:], op=ALU.mult))
                Vv([pb[3], cqb], [qfb],
                   lambda: nc.vector.tensor_tensor(out=qT_f[:, c, :], in0=ps[3][:, :], in1=sq_t[:], op=ALU.mult))
                Vv([t1b], [qfb],
                   lambda: nc.vector.tensor_tensor(out=qT_f[:, c, :], in0=qT_f[:, c, :], in1=t1[:], op=ALU.add))
                Aa([qfb], [qab], lambda: nc.scalar.copy(out=qaug[0:64, 2 * c, :], in_=qT_f[0:64, c, :]))
                Aa([qfb], [qab], lambda: nc.scalar.copy(out=qaug[0:64, 2 * c + 1, :], in_=qT_f[64:128, c, :]))

            for t in range(4):
                tr0 = row0 + t * 128
                cx.dma("sp", msk[:, 0, :], negm[tr0:tr0 + 128, :], writes=[mskb], stream="m")
                cx.dma("sp", msk[:, 1, :], valm[tr0:tr0 + 128, :], writes=[mskb], stream="m")
                cx.dma("sp", msk[:, 2, :], ownm[tr0:tr0 + 128, :], writes=[mskb], stream="m")
                cx.deps("pe", [qfb, kmB], [pb[4]])
                ins = None
                for c in range(4):
                    ins = nc.tensor.matmul(ps[4][:, c * 32:(c + 1) * 32], qT_f[:, c, t * 128:(t + 1) * 128],
                                           kmbd[:, c, :], start=True, stop=True)
                cx.op("pe", ins, [qfb, kmB], [pb[4]])
                Vv([pb[4], mskb], [gmb],
                   lambda: nc.vector.tensor_tensor(out=gm[:], in0=ps[4][:, 0:128], in1=msk[:, 0, :], op=ALU.add))
                for h in range(NH):
                    Vv([gmb], [top8b], lambda: nc.vector.max(out=top8[:, h, :], in_=gm[:, h * 16:(h + 1) * 16]))
                for h in range(NH):
                    Vv([gmb, top8b], [selb],
                       lambda: nc.vector.tensor_scalar(out=sel[:, h * 16:(h + 1) * 16], in0=gm[:, h * 16:(h + 1) * 16],
                                                       scalar1=top8[:, h, 2:3], scalar2=None, op0=ALU.is_ge))
                Vv([selb, mskb], [selb],
                   lambda: nc.vector.tensor_tensor(out=sel[:], in0=sel[:], in1=msk[:, 1, :], op=ALU.mult))
                Vv([selb, mskb], [selb],
                   lambda: nc.vector.tensor_tensor(out=sel[:], in0=sel[:], in1=msk[:, 2, :], op=ALU.add))
                Vv([selb], [bwb],
                   lambda: nc.vector.tensor_scalar(out=biasw[:, :, 0:16],
                                                   in0=sel[:, :].rearrange("p (h n) -> p h n", h=NH),
                                                   scalar1=BIG, scalar2=-BIG, op0=ALU.mult, op1=ALU.add))
                for b2 in range(2):
                    transpose(ps[5][:, b2 * 128:(b2 + 1) * 128],
                              biasw[:, b2 * 4:(b2 + 1) * 4, :].rearrange("p h n -> p (h n)"), ident[:],
                              [bwb, cB], pb[5])
                for h in range(NH):
                    p0 = (h % 4) * 32
                    c0 = (h // 4) * 128
                    Aa([pb[5]], [qab], lambda: nc.scalar.copy(out=qaug[64:80, h, t * 128:(t + 1) * 128],
                                                             in_=ps[5][p0:p0 + 16, c0:c0 + 128]))

            nkeys = nkt * 128
            for h in range(NH):
                kb_ = h % 2
                cx.dma("sp", kaug[kb_][0:64, 0:nkeys], s_kT[h, :, 0:nkeys], writes=[kab[kb_]], stream="ka")
                cx.dma("sp", kaug[kb_][64:80, 0:nkeys], blkind[:, 0:nkeys], writes=[kab[kb_]], stream="ka")
                cx.dma("sp", vh[kb_][:, 0:nkt, :], s_v[h, :, 0:nkt, :], writes=[vhb[kb_]], stream="va")
                for kt in range(nkt):
                    sbk = 6 + (kt % 2)
                    pj = kt % 3
                    mm_group(ps[sbk][:, :], [(kaug[kb_][0:80, kt * 128:(kt + 1) * 128], qaug[0:80, h, :])],
                             [kab[kb_], qab], pb[sbk])
                    Aa([pb[sbk]], [pTb[pj]],
                       lambda: nc.scalar.activation(out=pT[pj][:], in_=ps[sbk][:, :], func=AF.Exp, scale=0.125))
                    if kt >= nkt - 8:
                        jj = kt - (nkt - 8)
                        Pp([cmkb], [pTb[pj]],
                           lambda: nc.gpsimd.tensor_tensor(out=pT[pj][:], in0=pT[pj][:], in1=cmk[:, jj, :], op=ALU.mult))
                    cx.deps("pe", [vhb[kb_], pTb[pj]], [pb[4]] if kt == 0 else [])
                    ins = nc.tensor.matmul(ps[4][0:65, :], vh[kb_][:, kt, 0:65], pT[pj][:], start=(kt == 0),
                                           stop=(kt == nkt - 1))
                    cx.op("pe", ins, [vhb[kb_], pTb[pj]], [pb[4]] if kt == nkt - 1 else [])
                Vv([pb[4]], [rdb], lambda: nc.vector.reciprocal(out=rden[64:65, :], in_=ps[4][64:65, :]))
                Aa([rdb], [rd0b], lambda: nc.scalar.copy(out=rden0[0:1, :], in_=rden[64:65, :]))
                mm_group(ps[5][0:64, :], [(ones1[0:1, 0:64], rden0[0:1, :])], [rd0b, cB], pb[5])
                Aa([pb[5]], [bcsb], lambda: nc.scalar.copy(out=bcs[:], in_=ps[5][0:64, :]))
                Vv([pb[4], bcsb], [atb],
                   lambda: nc.vector.tensor_tensor(out=attnT[0:64, h, :], in0=ps[4][0:64, :], in1=bcs[:], op=ALU.mult))

            cx.dma("sp", xh[:], xhalo[slot * 16:(slot + 1) * 16, :], writes=[xhb], stream="c")
            for kc in range(8):
                transpose(ps[0][:, kc * 16:(kc + 1) * 16], xh[:, kc * 128:(kc + 1) * 128], ident[0:16, 0:16],
                          [xhb, cB], pb[0])
            Vv([pb[0]], [xhTb], lambda: nc.vector.tensor_copy(out=xhT[:, :, :],
                                                             in_=ps[0][:, 0:128].rearrange("p (k t) -> p k t", k=8)))
            load_wblk(wblk[0], wblkb[0], s_win, 1536)
            cx.dma("sp", wpool_t[:], s_wpool[:, :].rearrange("(g c) e -> c g e", g=4), reads=[conv], writes=[wpb],
                   stream="w")
            for g4 in range(4):
                mm_group(ps[2][:, :], [(wblk[0][:, kc, g4 * 128:(g4 + 1) * 128], xT_b[:, kc, :]) for kc in range(8)],
                         [wblkb[0], xb_buf], pb[2])
                mm_group(ps[3][:, 0:16], [(wblk[0][:, kc, g4 * 128:(g4 + 1) * 128], xhT[:, kc, :]) for kc in range(8)],
                         [wblkb[0], xhTb], pb[3])
                Aa([pb[2]], [uTb], lambda: nc.scalar.copy(out=uT[:, g4, 16:16 + G], in_=ps[2][:, :]))
                Aa([pb[3]], [uTb], lambda: nc.scalar.copy(out=uT[:, g4, 0:16], in_=ps[3][:, 0:16]))
                cur, curb = uT[:, g4, :], uTb
                L = 16 + G
                for k in range(g4 + 1):
                    sh = 1 << k
                    nxt, nxtb = (pa, pab) if k % 2 == 0 else (pbt, pbb)
                    Vv([curb], [nxtb], lambda: nc.vector.tensor_tensor(out=nxt[:, sh:L], in0=cur[:, sh:L],
                                                                      in1=cur[:, 0:L - sh], op=ALU.add))
                    cur, curb = nxt[:, :], nxtb
                cx.dma("sp", invt[:], invc[slot, :, g4 * G:(g4 + 1) * G], writes=[invb], stream="c")
                Vv([curb, invb], [t1b], lambda: nc.vector.tensor_tensor(out=t1[:], in0=cur[:, 16:L], in1=invt[:],
                                                                       op=ALU.mult))
                Vv([t1b, uTb], [dfb], lambda: nc.vector.tensor_tensor(out=diffT[:], in0=t1[:], in1=uT[:, g4, 16:L],
                                                                     op=ALU.subtract))
                mm_group(ps[5][:, :], [(wpool_t[:, g4, :], diffT[:])], [wpb, dfb], pb[5])
                Vv([pb[5], cB], [plb], lambda: nc.vector.tensor_scalar(out=pooledT[:, g4, :], in0=ps[5][:, :],
                                                                      scalar1=fp[:, 48 + g4:49 + g4], scalar2=None,
                                                                      op0=ALU.mult))
            if slot == NSLOT - 1:
                for g4 in range(4):
                    transpose(ps[0][:, g4 * 128:(g4 + 1) * 128], uT[:, g4, 16 + G - 128:16 + G], ident[:],
                              [uTb, cB], pb[0])
                Aa([pb[0]], [sgab], lambda: nc.scalar.copy(out=sga[:], in_=ps[0][:, :]))
                cx.dma("sp", pool_out[:, :], sga[112:128, :], reads=[sgab], stream="po")

            barrier()
            sA1.close()
            merge_ln1(sba, G, xT_f, xfb, xT_b, xb_buf, attnT, atb, pooledT, plb)
            barrier()

        with ExitStack() as sB:
            def sbb(name, shape, dt=F32):
                return sB.enter_context(nc.sbuf_tensor(f"b{slot}_" + name, list(shape), dt))
            moe_and_out(sbb, slot * G, G, y_out)
            barrier()

    with ExitStack() as sC:
        def sbc(name, shape, dt=F32):
            return sC.enter_context(nc.sbuf_tensor("c_" + name, list(shape), dt))
        merge_ln1(sbc, 4, xsT_f, xsfb, xsT_b, xsbb, attnT_s, atsb, pooledT_s, plsb)
        barrier()
    with ExitStack() as sD:
        def sbd(name, shape, dt=F32):
            return sD.enter_context(nc.sbuf_tensor("d_" + name, list(shape), dt))
        moe_and_out(sbd, 0, 4, ys_out)
        barrier()
    cx.drain("sp")


_PROG = None


def _rope_tables(pos):
    half = HD // 2
    inv_freq = 1.0 / (10000.0 ** (np.arange(0, HD, 2, dtype=np.float32) / HD))
    ang = pos.astype(np.float32)[None, :] * inv_freq[:, None].astype(np.float32)
    cos = np.cos(ang).astype(np.float32)
    sin = np.sin(ang).astype(np.float32)
    cos64 = np.concatenate([cos, cos], 0)
    sin64 = np.concatenate([-sin, sin], 0)
    return np.ascontiguousarray(np.concatenate([cos64, cos64], 0)), np.ascontiguousarray(np.concatenate([sin64, sin64], 0))


def kernel(**inp):
    global _PROG
    x_prompt = np.asarray(inp["x_prompt"], np.float32)
    w_in = np.asarray(inp["w_in"], np.float32)[0]
    wqk = w_in[:, 0:1024].reshape(D, 16, 2, 32)
    w_rot = np.ascontiguousarray(wqk[:, :, ::-1, :].reshape(D, 1024))
    w_r = np.concatenate([np.asarray(inp["w_group_router"], np.float32)[0],
                          np.asarray(inp["w_expert_router"], np.float32)[0].transpose(1, 0, 2).reshape(D, 16)], 1)
    w_r = np.ascontiguousarray(w_r.reshape(8, 128, 20).transpose(1, 0, 2).reshape(128, 160))
    b_r = np.concatenate([np.asarray(inp["b_group_router"], np.float32)[0],
                          np.asarray(inp["b_expert_router"], np.float32)[0].reshape(16)])
    b_r = np.ascontiguousarray(np.broadcast_to(b_r[None, :], (128, 20)))

    def fm(v):
        return np.asarray(v, np.float32).reshape(8, 128).T

    fpar = np.concatenate([fm(inp["ln1_g"][0]), fm(inp["ln1_b"][0]), fm(inp["ln2_g"][0]), fm(inp["ln2_b"][0]),
                           fm(inp["b_gate"][0, 0]), fm(inp["b_gate"][0, 1]),
                           np.asarray(inp["pool_scale"], np.float32)[0].reshape(4, 128).T], 1)
    fpar = np.ascontiguousarray(fpar)
    cosk, sink = _rope_tables(np.arange(SEQ))
    blk = (np.arange(SEQ)[None, :] // 256 == np.arange(16)[:, None]).astype(ml_dtypes.bfloat16)
    ident = np.eye(128, dtype=np.float32)
    sel16 = np.ascontiguousarray(np.repeat(np.eye(16, dtype=np.float32)[:, :, None], 128, axis=2).reshape(16, 16 * 128))
    kk = np.arange(128)[:, None]
    qq = np.arange(G)[None, :]
    mt = []
    for t in range(4):
        kp = 128 * t + kk
        mt.append(1.0 - ((kp // 256 == qq // 256) & (kp > qq)).astype(np.float32))
    ones = np.ones((128, G), np.float32)
    zeros = np.zeros((128, G), np.float32)
    lower = np.concatenate(mt + [zeros] * 4, 1)
    upper = np.concatenate([ones] * 4 + mt, 1)

    x_sample = np.asarray(inp["x_sample"], np.float32)[:, 0, :]
    ck = np.asarray(inp["cache_k"], np.float32)[0].reshape(2560 * 8, 8192)
    cv = np.asarray(inp["cache_v"], np.float32)[0].reshape(2560 * 8, 8192)
    page_table = np.asarray(inp["page_table"], np.int32)
    state_pool = np.asarray(inp["state_pool"], np.float32)[0]
    c8, s8 = _rope_tables(np.array([8192]))
    cs8 = np.ascontiguousarray(np.tile(c8[0:64, 0][None, :], (4, NH)))
    sn8 = np.ascontiguousarray(np.tile(s8[0:64, 0][None, :], (4, NH)))
    pp = np.arange(128)
    selT = np.zeros((4, 256), np.float32)
    pairsel = np.zeros((128, 8), np.float32)
    for t in range(2):
        for p_ in range(128):
            selT[2 * t + p_ // 64, t * 128 + p_] = 1.0
            pairsel[p_, t * 4 + 2 * t + p_ // 64] = 1.0
    in_maps = []
    for c in range(8):
        s, p = c // 2, c % 2
        groups = [0, 3, 4, 7] if p == 0 else [1, 2, 5, 6]
        xs = x_prompt[s]
        xo = np.concatenate([xs[g * G:(g + 1) * G] for g in groups], 0)
        xh = np.zeros((NSLOT * 16, D), np.float32)
        for i, g in enumerate(groups):
            if g > 0:
                xh[i * 16:(i + 1) * 16] = xs[g * G - 16:g * G]
        posq = np.concatenate([np.arange(g * G, (g + 1) * G) for g in groups])
        cq, sq = _rope_tables(posq)
        own = posq // 256
        nb = np.arange(16)[None, :]
        valm = np.ascontiguousarray(np.tile((nb < own[:, None]).astype(np.float32), (1, NH)))
        ownm = np.ascontiguousarray(np.tile((nb == own[:, None]).astype(np.float32), (1, NH)))
        negm = np.ascontiguousarray(np.tile(np.where(nb < own[:, None], 0.0, NEG).astype(np.float32), (1, NH)))
        cm = np.stack([lower if (g % 2 == 0) else upper for g in groups[:2]], 0).astype(ml_dtypes.bfloat16)
        invc = np.zeros((NSLOT, 128, 4 * G), np.float32)
        for i, g in enumerate(groups):
            pos = np.arange(g * G, (g + 1) * G)
            for gi, w in enumerate((2, 4, 8, 16)):
                invc[i, :, gi * G:(gi + 1) * G] = (1.0 / np.minimum(w, pos + 1))[None, :]
        in_maps.append({
            "xseq": np.ascontiguousarray(xs), "xown": np.ascontiguousarray(xo), "xhalo": xh,
            "w_in": w_in, "w_rot": w_rot,
            "w_pool": np.asarray(inp["w_pool"], np.float32)[0].reshape(512, 128),
            "w_a": np.asarray(inp["w_branch_a"], np.float32)[0], "w_b": np.asarray(inp["w_branch_b"], np.float32)[0],
            "w_out": np.asarray(inp["w_out"], np.float32)[0],
            "w_eg": np.asarray(inp["w_e_gate"], np.float32)[0].reshape(NE * D, DE),
            "w_eu": np.asarray(inp["w_e_up"], np.float32)[0].reshape(NE * D, DE),
            "w_ed": np.asarray(inp["w_e_down"], np.float32)[0].reshape(NE * DE, D),
            "w_r": w_r, "b_r": b_r, "fpar": fpar, "cosk": cosk, "sink": sink, "cosq": cq, "sinq": sq,
            "negm": negm, "valm": valm, "ownm": ownm, "cmask": np.ascontiguousarray(cm.reshape(2, 128, 8 * G)),
            "blkind": blk, "invc": invc, "ident": ident, "sel16": sel16,
            "xs4": np.ascontiguousarray(x_sample[4 * c:4 * c + 4]), "ck": ck, "cv": cv,
            "pt": np.ascontiguousarray(page_table[4 * c:4 * c + 4].reshape(2, 128).T),
            "st4": np.ascontiguousarray(state_pool[4 * c:4 * c + 4].reshape(4, 15 * 512)),
            "cs8": cs8, "sn8": sn8, "selT": selT, "pairsel": pairsel,
        })
    if _PROG is None:
        _PROG = build_program()
    res = run_bass_kernel_spmd(_PROG, in_maps, core_ids=list(range(8)))
    R = res.results
    y_p = np.zeros((4, SEQ, D), np.float32)
    k_p = np.zeros((1, 4, SEQ, NH, HD), np.float32)
    v_p = np.zeros((1, 4, SEQ, NH, HD), np.float32)
    pool_p = np.zeros((1, 4, 15, 512), np.float32)
    for c in range(8):
        s, p = c // 2, c % 2
        groups = [0, 3, 4, 7] if p == 0 else [1, 2, 5, 6]
        for i, g in enumerate(groups):
            y_p[s, g * G:(g + 1) * G] = R[c]["y_out"][i * G:(i + 1) * G]
        if p == 0:
            k_p[0, s] = R[c]["k_out"].reshape(SEQ, NH, HD)
            v_p[0, s] = R[c]["v_out"].reshape(SEQ, NH, HD)
            pool_p[0, s] = R[c]["pool_out"][1:16]
    y_s = np.zeros((32, 1, D), np.float32)
    k_s = np.zeros((1, 32, 1, NH, HD), np.float32)
    v_s = np.zeros((1, 32, 1, NH, HD), np.float32)
    pool_s = np.zeros((1, 32, 15, 512), np.float32)
    for c in range(8):
        y_s[4 * c:4 * c + 4, 0] = R[c]["ys_out"]
        k_s[0, 4 * c:4 * c + 4, 0] = R[c]["ks_out"].reshape(4, NH, HD)
        v_s[0, 4 * c:4 * c + 4, 0] = R[c]["vs_out"].reshape(4, NH, HD)
        pool_s[0, 4 * c:4 * c + 4] = R[c]["ps_out"].reshape(4, 15, 512)
    return (y_p, y_s, k_p, v_p, pool_p, k_s, v_s, pool_s)
```

```python
import numpy as np
import ml_dtypes
import concourse.bass as bass
import concourse.mybir as mybir
from concourse.bass_utils import run_bass_kernel_spmd

F32 = mybir.dt.float32
BF16 = mybir.dt.bfloat16
I32 = mybir.dt.int32
ALU = mybir.AluOpType
AF = mybir.ActivationFunctionType
AX = mybir.AxisListType

D = 1024
SEQ = 4096
NH = 8
HD = 64
G = 512
NG = 8
NSLOT = 4
ALPHA = 2.0 ** 0.25
BIG = 30000.0
NEG = -1.0e30
LN_EPS = 1e-5
NE = 16
DE = 256
STOP = 99


class Buf:
    __slots__ = ("w", "r")

    def __init__(self):
        self.w = None
        self.r = []


class Ctx:
    def __init__(self, nc, stack):
        self.nc = nc
        self.stack = stack
        self.eng = {"pe": nc.tensor, "act": nc.scalar, "dve": nc.vector, "pool": nc.gpsimd, "sp": nc.sync}
        self.sem = {}
        self.cnt = {}
        self.waited = {e: {} for e in self.eng}
        self.nsem = 0
        for e in self.eng:
            self._new_sem(e)
        self.dsem = {}
        self.dcnt = {}
        self.dnext = {}

    def _new_sem(self, e):
        self.nsem += 1
        s = self.stack.enter_context(self.nc.semaphore(f"s_{e}_{self.nsem}"))
        self.sem[e] = s
        self.cnt[e] = 0

    def epoch(self):
        for e in self.eng:
            if self.cnt[e] > 20000:
                self._new_sem(e)

    def _wait(self, e, tok):
        if tok is None:
            return
        sem, val = tok
        key = id(sem)
        if self.waited[e].get(key, 0) >= val:
            return
        self.waited[e][key] = val
        self.eng[e].wait_ge(sem, val)

    def deps(self, e, reads, writes):
        for b in reads:
            self._wait(e, b.w)
        for b in writes:
            self._wait(e, b.w)
            for t in b.r:
                self._wait(e, t)

    def done(self, tok, reads, writes):
        for b in reads:
            b.r.append(tok)
            if len(b.r) > 12:
                b.r = b.r[-12:]
        for b in writes:
            b.w = tok
            b.r = []

    def op(self, e, ins, reads=(), writes=()):
        self.cnt[e] += 1
        ins.then_inc(self.sem[e], 1)
        tok = (self.sem[e], self.cnt[e])
        self.waited[e][id(self.sem[e])] = max(self.waited[e].get(id(self.sem[e]), 0), 0)
        self.done(tok, reads, writes)
        return tok

    NROT = 4

    def dma(self, q, out, in_, reads=(), writes=(), stream="d", **kw):
        e = q
        self.deps(e, reads, writes)
        key = (q, stream)
        if key not in self.dsem:
            sems = []
            for i in range(self.NROT):
                self.nsem += 1
                sems.append(self.stack.enter_context(self.nc.semaphore(f"dma_{q}_{stream}_{self.nsem}")))
            self.dsem[key] = sems
            self.dcnt[key] = [0] * self.NROT
            self.dnext[key] = 0
        i = self.dnext[key]
        self.dnext[key] = (i + 1) % self.NROT
        sem = self.dsem[key][i]
        if self.dcnt[key][i] > 0:
            self._wait(e, (sem, self.dcnt[key][i]))
        self.dcnt[key][i] += 16
        self.eng[e].dma_start(out=out, in_=in_, **kw).then_inc(sem, 16)
        tok = (sem, self.dcnt[key][i])
        self.done(tok, reads, writes)
        return tok

    def dma_done(self, q, ins, reads, writes, stream):
        key = (q, stream)
        if key not in self.dsem:
            sems = []
            for i in range(self.NROT):
                self.nsem += 1
                sems.append(self.stack.enter_context(self.nc.semaphore(f"dma_{q}_{stream}_{self.nsem}")))
            self.dsem[key] = sems
            self.dcnt[key] = [0] * self.NROT
            self.dnext[key] = 0
        i = self.dnext[key]
        self.dnext[key] = (i + 1) % self.NROT
        sem = self.dsem[key][i]
        self.dcnt[key][i] += 16
        ins.then_inc(sem, 16)
        tok = (sem, self.dcnt[key][i])
        self.done(tok, reads, writes)
        return tok

    def drain(self, e):
        for key, sems in self.dsem.items():
            for i, sem in enumerate(sems):
                if self.dcnt[key][i] > 0:
                    self._wait(e, (sem, self.dcnt[key][i]))


def _emit(cx, e, reads, writes, fn):
    cx.deps(e, reads, writes)
    ins = fn()
    return cx.op(e, ins, reads, writes)


def build_program():
    from contextlib import ExitStack
    nc = bass.Bass("TRN2", target_bir_lowering=False)
    st = ExitStack()
    with st:
        _build(nc, st)
    return nc


def _build(nc, st):
    from contextlib import ExitStack
    cx = Ctx(nc, st)

    def din(name, shape, dt=F32):
        return nc.dram_tensor(name, list(shape), dt, kind="ExternalInput").ap()

    def dout(name, shape, dt=F32):
        return nc.dram_tensor(name, list(shape), dt, kind="ExternalOutput").ap()

    def dscr(name, shape, dt=BF16):
        return nc.dram_tensor(name, list(shape), dt, kind="Internal").ap()

    def sb(name, shape, dt=F32):
        return st.enter_context(nc.sbuf_tensor("sb_" + name, list(shape), dt))

    xseq = din("xseq", [SEQ, D])
    xown = din("xown", [NSLOT * G, D])
    xhalo = din("xhalo", [NSLOT * 16, D])
    w_in = din("w_in", [D, 4096])
    w_rot = din("w_rot", [D, 1024])
    w_pool = din("w_pool", [4 * 128, 128])
    w_a = din("w_a", [512, D])
    w_b = din("w_b", [512, D])
    w_out = din("w_out", [D, D])
    w_eg = din("w_eg", [NE * D, DE])
    w_eu = din("w_eu", [NE * D, DE])
    w_ed = din("w_ed", [NE * DE, D])
    w_r = din("w_r", [128, 8 * 20])
    b_r = din("b_r", [128, 20])
    fpar = din("fpar", [128, 52])
    cosk = din("cosk", [128, SEQ])
    sink = din("sink", [128, SEQ])
    cosq = din("cosq", [128, NSLOT * G])
    sinq = din("sinq", [128, NSLOT * G])
    negm = din("negm", [NSLOT * G, 128])
    valm = din("valm", [NSLOT * G, 128])
    ownm = din("ownm", [NSLOT * G, 128])
    sel16_in = din("sel16", [16, 16 * 128])
    cmask = din("cmask", [2, 128, 8 * G], BF16)
    blkind = din("blkind", [16, SEQ], BF16)
    invc = din("invc", [NSLOT, 128, 4 * G])
    ident_in = din("ident", [128, 128])
    xs4 = din("xs4", [4, D])
    ck = din("ck", [2560 * 8, 8192])
    cv = din("cv", [2560 * 8, 8192])
    pt_in = din("pt", [128, 2], I32)
    st4 = din("st4", [4, 15 * 512])
    cs8 = din("cs8", [4, 512])
    sn8 = din("sn8", [4, 512])
    selT_in = din("selT", [4, 256])
    pairsel_in = din("pairsel", [128, 8])

    y_out = dout("y_out", [NSLOT * G, D])
    k_out = dout("k_out", [SEQ, 512])
    v_out = dout("v_out", [SEQ, 512])
    pool_out = dout("pool_out", [16, 512])
    ys_out = dout("ys_out", [4, D])
    ks_out = dout("ks_out", [4, 512])
    vs_out = dout("vs_out", [4, 512])
    ps_out = dout("ps_out", [4, 15 * 512])

    s_win = dscr("s_win", [D, 4096])
    s_wrot = dscr("s_wrot", [D, 1024])
    s_wpool = dscr("s_wpool", [512, 128])
    s_wa = dscr("s_wa", [512, D])
    s_wb = dscr("s_wb", [512, D])
    s_wout = dscr("s_wout", [D, D])
    s_eg = dscr("s_eg", [NE * D, DE])
    s_eu = dscr("s_eu", [NE * D, DE])
    s_ed = dscr("s_ed", [NE * DE, D])
    s_kT = dscr("s_kT", [NH, HD, SEQ])
    s_v = dscr("s_v", [NH, 128, 32, 66])

    convs = {}

    def convert(dst, src, rows, cols, name):
        conv = Buf()
        convs[name] = conv
        tot = rows * cols
        L = 2048 if tot % 2048 == 0 else cols
        R = tot // L
        if cols != L:
            if cols > L:
                s2 = src.rearrange("r (a b) -> (r a) b", b=L)
                d2 = dst.rearrange("r (a b) -> (r a) b", b=L)
            else:
                s2 = src.rearrange("(r a) b -> r (a b)", a=L // cols)
                d2 = dst.rearrange("(r a) b -> r (a b)", a=L // cols)
        else:
            s2, d2 = src, dst
        step = 512
        for r0 in range(0, R, step):
            r1 = min(R, r0 + step)
            cx.dma("pool", d2[r0:r1, :], s2[r0:r1, :], writes=[conv], stream="conv")

    convert(s_win, w_in, D, 4096, "s_win")
    convert(s_wrot, w_rot, D, 1024, "s_wrot")
    convert(s_wpool, w_pool, 512, 128, "s_wpool")
    convert(s_wa, w_a, 512, D, "s_wa")
    convert(s_wb, w_b, 512, D, "s_wb")
    convert(s_wout, w_out, D, D, "s_wout")
    convert(s_eg, w_eg, NE * D, DE, "s_eg")
    convert(s_eu, w_eu, NE * D, DE, "s_eu")
    convert(s_ed, w_ed, NE * DE, D, "s_ed")

    if STOP == 0:
        cx.drain("sp")
        return
    ident = sb("ident", [128, 128])
    identb = sb("identb", [128, 128], BF16)
    ones_ln = sb("ones_ln", [128, 128])
    fp = sb("fp", [128, 52])
    wr_sb = sb("wr_sb", [128, 8 * 20])
    br_sb = sb("br_sb", [128, 20])
    cB = Buf()
    cx.dma("sp", ident[:], ident_in[:, :], writes=[cB])
    cx.dma("sp", fp[:], fpar[:, :], writes=[cB])
    cx.dma("sp", wr_sb[:], w_r[:, :], writes=[cB])
    cx.dma("sp", br_sb[:], b_r[:, :], writes=[cB])
    _emit(cx, "dve", [cB], [cB], lambda: nc.vector.tensor_copy(out=identb[:], in_=ident[:]))
    _emit(cx, "dve", [], [cB], lambda: nc.vector.memset(ones_ln[:], 1.0 / D))

    ps = [st.enter_context(nc.psum_tensor(f"ps{i}", [128, 512], F32)) for i in range(8)]
    pb = [Buf() for _ in range(8)]

    def mm_group(out_ap, pairs, reads, wbuf):
        cx.deps("pe", reads, [wbuf])
        n = len(pairs)
        ins = None
        for i, (l, r) in enumerate(pairs):
            ins = nc.tensor.matmul(out_ap, l, r, start=(i == 0), stop=(i == n - 1))
        return cx.op("pe", ins, reads, [wbuf])

    def transpose(out_ap, in_ap, idt, reads, wbuf):
        cx.deps("pe", reads, [wbuf])
        ins = nc.tensor.transpose(out_ap, in_ap, idt)
        return cx.op("pe", ins, reads, [wbuf])

    def load_xT(x_dram, row0, ntok, xT_f, xT_b, xf_buf, xb_buf, xt_tiles, xt_bufs, psA, psB):
        nt = ntok // 128
        for t in range(nt):
            xt = xt_tiles[t % 2]
            xtb = xt_bufs[t % 2]
            cx.dma("sp", xt[:], x_dram[row0 + t * 128: row0 + (t + 1) * 128, :], writes=[xtb], stream="x")
            for half in range(2):
                pi = psA if half == 0 else psB
                for j in range(4):
                    kc = half * 4 + j
                    transpose(ps[pi][:, j * 128:(j + 1) * 128], xt[:, kc * 128:(kc + 1) * 128], ident[:],
                              [xtb, cB], pb[pi])
                src = ps[pi][:, :].rearrange("p (j t) -> p j t", j=4)
                if xT_f is not None:
                    dstf = xT_f[:, half * 4:(half + 1) * 4, t * 128:(t + 1) * 128]
                    _emit(cx, "act", [pb[pi]], [xf_buf], lambda: nc.scalar.copy(out=dstf, in_=src))
                dstb = xT_b[:, half * 4:(half + 1) * 4, t * 128:(t + 1) * 128]
                _emit(cx, "dve", [pb[pi]], [xb_buf], lambda: nc.vector.tensor_copy(out=dstb, in_=src))

    def barrier():
        for e in cx.eng:
            for e2 in cx.eng:
                if e2 != e and cx.cnt[e2] > 0:
                    cx._wait(e, (cx.sem[e2], cx.cnt[e2]))
            cx.drain(e)

    def Vv(reads, writes, fn):
        return _emit(cx, "dve", reads, writes, fn)

    def Aa(reads, writes, fn):
        return _emit(cx, "act", reads, writes, fn)

    def Pp(reads, writes, fn):
        return _emit(cx, "pool", reads, writes, fn)

    def load_wblk(tile, tb, src, c0, ncols=512, nk=8):
        cx.dma("sp", tile[:, 0:nk, 0:ncols], src[:, c0:c0 + ncols].rearrange("(kc p) j -> p kc j", p=128),
               reads=[convs[src.tensor.name]], writes=[tb], stream="w")

    def load_xT(x_dram, row0, ntok, xT_f, xT_b, xf_buf, xb_buf, xt_tiles, xt_bufs, psA, psB):
        nt = ntok // 128
        for t in range(nt):
            xt = xt_tiles[t % 2]
            xtb = xt_bufs[t % 2]
            cx.dma("sp", xt[:], x_dram[row0 + t * 128: row0 + (t + 1) * 128, :], writes=[xtb], stream="x")
            for half in range(2):
                pi = psA if half == 0 else psB
                for j in range(4):
                    kc = half * 4 + j
                    transpose(ps[pi][:, j * 128:(j + 1) * 128], xt[:, kc * 128:(kc + 1) * 128], ident[:],
                              [xtb, cB], pb[pi])
                src = ps[pi][:, :].rearrange("p (j t) -> p j t", j=4)
                dstb = xT_b[:, half * 4:(half + 1) * 4, t * 128:(t + 1) * 128]
                if xT_f is not None:
                    dstf = xT_f[:, half * 4:(half + 1) * 4, t * 128:(t + 1) * 128]
                    Aa([pb[pi]], [xf_buf], lambda: nc.scalar.copy(out=dstf, in_=src))
                    Vv([xf_buf], [xb_buf], lambda: nc.vector.tensor_copy(out=dstb, in_=dstf))
                else:
                    Vv([pb[pi]], [xb_buf], lambda: nc.vector.tensor_copy(out=dstb, in_=src))

    xt_tiles = [sb(f"xt{i}", [128, D]) for i in range(2)]
    xt_bufs = [Buf(), Buf()]
    xT_b = sb("xT_b", [128, 8, G], BF16)
    xb_buf = Buf()
    kmT = sb("kmT", [128, 4, 16])
    kmB = Buf()
    kmbd = sb("kmbd", [128, 4, 32])
    ones1 = sb("ones1", [1, 64])
    sel16 = sb("sel16", [16, 16 * 128])
    cx.dma("sp", sel16[:], sel16_in[:, :], writes=[cB])
    Vv([], [cB], lambda: nc.vector.memset(ones1[:], 1.0))

    xsT_f = sb("xsT_f", [128, 8, 4])
    xsfb = Buf()
    xsT_b = sb("xsT_b", [128, 8, 4], BF16)
    xsbb = Buf()
    attnT_s = sb("attnT_s", [64, NH, 4], BF16)
    atsb = Buf()
    pooledT_s = sb("pooledT_s", [128, 4, 4], BF16)
    plsb = Buf()
    with ExitStack() as sS:
        def sbs(name, shape, dt=F32):
            return sS.enter_context(nc.sbuf_tensor("s_" + name, list(shape), dt))
        xs_t = sbs("xs_t", [4, D])
        xsb = Buf()
        wS = [sbs(f"wS{i}", [128, 8, 512], BF16) for i in range(2)]
        wSb = [Buf(), Buf()]
        tok = [sbs(f"tok{i}", [4, 512]) for i in range(6)]
        tokb = [Buf() for _ in range(6)]
        cst = sbs("cst", [4, 2, 512])
        cstb = Buf()
        tmp4 = sbs("tmp4", [4, 512])
        tmp4b = Buf()
        st_t = sbs("st_t", [4, 15, 512])
        stb_ = Buf()
        ssum = sbs("ssum", [4, 512])
        ssb = Buf()
        diff4 = sbs("diff4", [4, 512])
        d4b = Buf()
        diffT_s = sbs("diffT_s", [128, 4, 4], BF16)
        dTsb = Buf()
        wpool_s = sbs("wpool_s", [128, 4, 128], BF16)
        wpsb = Buf()
        pt_sb = sbs("pt_sb", [128, 2], I32)
        idx8 = sbs("idx8", [128, 2, 8], I32)
        ptb = Buf()
        selT = sbs("selT", [4, 256])
        pairsel = sbs("pairsel", [128, 8])
        scb = Buf()
        q_bc = sbs("q_bc", [128, 512])
        qbb = Buf()
        KV = [sbs(f"KV{i}", [128, 8192]) for i in range(2)]
        KVb = [Buf(), Buf()]
        s_all = sbs("s_all", [128, 128, NH])
        sab = Buf()
        Pm = sbs("Pm", [128, 128, NH])
        Pmb = Buf()
        gpage = sbs("gpage", [128, NH])
        gpb = Buf()
        gpT = sbs("gpT", [NH, 128])
        gpTb = Buf()
        gblk = sbs("gblk", [NH, 64])
        gbb = Buf()
        top8s = sbs("top8s", [NH, 8])
        selb_ = sbs("selb", [NH, 64])
        selp = sbs("selp", [NH, 128])
        spb = Buf()
        maskp = sbs("maskp", [128, NH])
        mpb = Buf()
        den = sbs("den", [128, NH])
        denb = Buf()
        Oacc = sbs("Oacc", [128, 512])
        Oab = Buf()
        red = sbs("red", [128, 512])
        redb = Buf()
        snew = sbs("snew", [4, 3, NH])
        snb = Buf()
        Osum = sbs("Osum", [4, 512])
        Osb = Buf()
        attn_tok = sbs("attn_tok", [4, 512])
        atkb = Buf()

        cx.dma("sp", xs_t[:], xs4[:, :], writes=[xsb], stream="c")
        cx.dma("sp", cst[:, 0, :], cs8[:, :], writes=[cstb], stream="c")
        cx.dma("sp", cst[:, 1, :], sn8[:, :], writes=[cstb], stream="c")
        cx.dma("sp", st_t[:], st4[:, :].rearrange("p (r c) -> p r c", r=15), writes=[stb_], stream="c")
        cx.dma("sp", pt_sb[:], pt_in[:, :], writes=[ptb], stream="c")
        cx.dma("sp", selT[:], selT_in[:, :], writes=[scb], stream="c")
        cx.dma("sp", pairsel[:], pairsel_in[:, :], writes=[scb], stream="c")
        cx.dma("sp", wpool_s[:], s_wpool[:, :].rearrange("(g c) e -> c g e", g=4), reads=[convs["s_wpool"]], writes=[wpsb],
               stream="w")
        for kc in range(8):
            transpose(ps[0][:, kc * 4:(kc + 1) * 4], xs_t[0:4, kc * 128:(kc + 1) * 128], ident[0:4, 0:4],
                      [xsb, cB], pb[0])
        Aa([pb[0]], [xsfb], lambda: nc.scalar.copy(out=xsT_f[:, :, :],
                                                  in_=ps[0][:, 0:32].rearrange("p (k t) -> p k t", k=8)))
        Vv([xsfb], [xsbb], lambda: nc.vector.tensor_copy(out=xsT_b[:, :, :], in_=xsT_f[:, :, :]))
        for i, (srcw, c0) in enumerate(((s_win, 0), (s_wrot, 0), (s_win, 512), (s_wrot, 512), (s_win, 1024),
                                        (s_win, 1536))):
            load_wblk(wS[i % 2], wSb[i % 2], srcw, c0)
            pi = 2 + i % 2
            mm_group(ps[pi][0:4, :], [(xsT_b[:, kc, :], wS[i % 2][:, kc, :]) for kc in range(8)],
                     [wSb[i % 2], xsbb], pb[pi])
            Aa([pb[pi]], [tokb[i]], lambda: nc.scalar.copy(out=tok[i][:], in_=ps[pi][0:4, :]))
        for (a_, r_) in ((0, 1), (2, 3)):
            Vv([tokb[a_], cstb], [tokb[a_]], lambda: nc.vector.tensor_tensor(out=tok[a_][:], in0=tok[a_][:],
                                                                            in1=cst[:, 0, :], op=ALU.mult))
            Vv([tokb[r_], cstb], [tokb[r_]], lambda: nc.vector.tensor_tensor(out=tok[r_][:], in0=tok[r_][:],
                                                                            in1=cst[:, 1, :], op=ALU.mult))
            Vv([tokb[r_]], [tokb[a_]], lambda: nc.vector.tensor_tensor(out=tok[a_][:], in0=tok[a_][:], in1=tok[r_][:],
                                                                      op=ALU.add))
        q_tok, k_tok, v_tok, u_tok = tok[0], tok[2], tok[4], tok[5]
        qtb, ktb, vtb, utb = tokb[0], tokb[2], tokb[4], tokb[5]
        cx.dma("sp", ks_out[:, :], k_tok[:], reads=[ktb], stream="so")
        cx.dma("sp", vs_out[:, :], v_tok[:], reads=[vtb], stream="so")
        cx.dma("sp", ps_out[:, 14 * 512:15 * 512], u_tok[:], reads=[utb], stream="so")
        cx.dma("sp", ps_out[:, 0:14 * 512], st4[:, 512:15 * 512], stream="so")
        for g4, w_ in enumerate((2, 4, 8, 16)):
            c0 = g4 * 128
            Vv([stb_], [ssb], lambda: nc.vector.tensor_reduce(
                out=ssum[:, c0:c0 + 128], in_=st_t[:, 15 - (w_ - 1):15, c0:c0 + 128].rearrange("p r c -> p c r"),
                axis=AX.X, op=ALU.add))
            Vv([ssb, utb], [tmp4b], lambda: nc.vector.tensor_tensor(out=tmp4[:, c0:c0 + 128], in0=ssum[:, c0:c0 + 128],
                                                                   in1=u_tok[:, c0:c0 + 128], op=ALU.add))
            Vv([tmp4b, utb], [d4b], lambda: nc.vector.scalar_tensor_tensor(
                out=diff4[:, c0:c0 + 128], in0=tmp4[:, c0:c0 + 128], scalar=1.0 / w_, in1=u_tok[:, c0:c0 + 128],
                op0=ALU.mult, op1=ALU.subtract))
        for g4 in range(4):
            transpose(ps[1][:, g4 * 4:(g4 + 1) * 4], diff4[0:4, g4 * 128:(g4 + 1) * 128], ident[0:4, 0:4],
                      [d4b, cB], pb[1])
        Vv([pb[1]], [dTsb], lambda: nc.vector.tensor_copy(out=diffT_s[:, :, :],
                                                         in_=ps[1][:, 0:16].rearrange("p (g t) -> p g t", g=4)))
        for g4 in range(4):
            mm_group(ps[2][:, 0:4], [(wpool_s[:, g4, :], diffT_s[:, g4, :])], [wpsb, dTsb], pb[2])
            Vv([pb[2], cB], [plsb], lambda: nc.vector.tensor_scalar(out=pooledT_s[:, g4, :], in0=ps[2][:, 0:4],
                                                                   scalar1=fp[:, 48 + g4:49 + g4], scalar2=None,
                                                                   op0=ALU.mult))
        for c in range(8):
            Vv([ptb], [ptb], lambda: nc.vector.tensor_scalar(out=idx8[:, :, c], in0=pt_sb[:, :], scalar1=8.0,
                                                            scalar2=float(c), op0=ALU.mult, op1=ALU.add))
        nbuf = 0
        for t in range(2):
            mm_group(ps[3][:, :], [(selT[0:4, t * 128:(t + 1) * 128], q_tok[0:4, :])], [scb, qtb], pb[3])
            Aa([pb[3]], [qbb], lambda: nc.scalar.copy(out=q_bc[:], in_=ps[3][:, :]))
            for c in range(8):
                kb_ = nbuf % 2
                nbuf += 1
                cx.deps("pool", [ptb], [KVb[kb_]])
                ins = nc.gpsimd.indirect_dma_start(out=KV[kb_][:, :], out_offset=None, in_=ck[:, :],
                                                   in_offset=bass.IndirectOffsetOnAxis(ap=idx8[:, t, c:c + 1], axis=0))
                cx.dma_done("pool", ins, [ptb], [KVb[kb_]], "g")
                (Pp if c % 2 == 0 else Vv)([qbb], [KVb[kb_]], lambda: (nc.gpsimd if c % 2 == 0 else nc.vector).tensor_tensor(
                    out=KV[kb_][:, :].rearrange("p (r e) -> p r e", r=16),
                    in0=KV[kb_][:, :].rearrange("p (r e) -> p r e", r=16),
                    in1=q_bc[:, :].rearrange("p (o e) -> p o e", o=1).broadcast_to([128, 16, 512]), op=ALU.mult))
                Vv([KVb[kb_]], [sab], lambda: nc.vector.tensor_reduce(
                    out=s_all[:, c * 16:(c + 1) * 16, :].rearrange("p r h -> p (r h)"),
                    in_=KV[kb_][:, :].rearrange("p (a d) -> p a d", d=HD), axis=AX.X, op=ALU.add))
            Vv([sab], [gpb], lambda: nc.vector.tensor_reduce(out=gpage[:, :], in_=s_all[:, :, :].rearrange("p r h -> p h r"),
                                                            axis=AX.X, op=ALU.add))
            transpose(ps[4][0:NH, 0:128], gpage[:, :], ident[:], [gpb, cB], pb[4])
            Aa([pb[4]], [gpTb], lambda: nc.scalar.copy(out=gpT[:, :], in_=ps[4][0:NH, 0:128]))
            gv = gpT[:, :].rearrange("h (n two) -> h n two", two=2)
            Vv([gpTb], [gbb], lambda: nc.vector.tensor_tensor(out=gblk[:, :], in0=gv[:, :, 0], in1=gv[:, :, 1], op=ALU.add))
            for s2 in range(2):
                Vv([gbb], [gbb], lambda: nc.vector.max(out=top8s[:, :], in_=gblk[:, s2 * 32:(s2 + 1) * 32]))
                Vv([gbb], [gbb], lambda: nc.vector.tensor_scalar(out=selb_[:, s2 * 32:(s2 + 1) * 32],
                                                                in0=gblk[:, s2 * 32:(s2 + 1) * 32],
                                                                scalar1=top8s[:, 2:3], scalar2=None, op0=ALU.is_ge))
            sv = selp[:, :].rearrange("h (n two) -> h n two", two=2)
            Vv([gbb], [spb], lambda: nc.vector.tensor_copy(out=sv[:, :, 0], in_=selb_[:, :]))
            Vv([gbb], [spb], lambda: nc.vector.tensor_copy(out=sv[:, :, 1], in_=selb_[:, :]))
            transpose(ps[4][:, 128:128 + NH], selp[:, :], ident[0:NH, 0:NH], [spb, cB], pb[4])
            Aa([pb[4]], [mpb], lambda: nc.scalar.copy(out=maskp[:, :], in_=ps[4][:, 128:128 + NH]))
            Aa([sab], [Pmb], lambda: nc.scalar.activation(out=Pm[:, :, :], in_=s_all[:, :, :], func=AF.Exp, scale=0.125))
            Vv([mpb], [Pmb], lambda: nc.vector.tensor_tensor(
                out=Pm[:, :, :], in0=Pm[:, :, :],
                in1=maskp[:, :].rearrange("p (o h) -> p o h", o=1).broadcast_to([128, 128, NH]), op=ALU.mult))
            Vv([Pmb], [denb], lambda: nc.vector.tensor_reduce(out=den[:, :], in_=Pm[:, :, :].rearrange("p r h -> p h r"),
                                                             axis=AX.X, op=ALU.add))
            Vv([], [Oab], lambda: nc.vector.memset(Oacc[:], 0.0))
            for c in range(8):
                kb_ = nbuf % 2
                nbuf += 1
                cx.deps("pool", [ptb], [KVb[kb_]])
                ins = nc.gpsimd.indirect_dma_start(out=KV[kb_][:, :], out_offset=None, in_=cv[:, :],
                                                   in_offset=bass.IndirectOffsetOnAxis(ap=idx8[:, t, c:c + 1], axis=0))
                cx.dma_done("pool", ins, [ptb], [KVb[kb_]], "g")
                (Pp if c % 2 == 0 else Vv)([Pmb], [KVb[kb_]], lambda: (nc.gpsimd if c % 2 == 0 else nc.vector).tensor_tensor(
                    out=KV[kb_][:, :].rearrange("p (r h d) -> p r h d", r=16, h=NH),
                    in0=KV[kb_][:, :].rearrange("p (r h d) -> p r h d", r=16, h=NH),
                    in1=Pm[:, c * 16:(c + 1) * 16, :].rearrange("p r (h o) -> p r h o", o=1).broadcast_to([128, 16, NH, HD]),
                    op=ALU.mult))
                Vv([KVb[kb_]], [redb], lambda: nc.vector.tensor_reduce(
                    out=red[:, :], in_=KV[kb_][:, :].rearrange("p (r e) -> p e r", r=16), axis=AX.X, op=ALU.add))
                Vv([redb], [Oab], lambda: nc.vector.tensor_tensor(out=Oacc[:], in0=Oacc[:], in1=red[:], op=ALU.add))
            cx.deps("pe", [scb, Oab], [pb[5]] if t == 0 else [])
            ins = nc.tensor.matmul(ps[5][0:4, :], pairsel[:, t * 4:(t + 1) * 4], Oacc[:, :], start=(t == 0), stop=(t == 1))
            cx.op("pe", ins, [scb, Oab], [pb[5]] if t == 1 else [])
            cx.deps("pe", [scb, denb], [pb[6]] if t == 0 else [])
            ins = nc.tensor.matmul(ps[6][0:4, 0:NH], pairsel[:, t * 4:(t + 1) * 4], den[:, :], start=(t == 0), stop=(t == 1))
            cx.op("pe", ins, [scb, denb], [pb[6]] if t == 1 else [])
        Vv([qtb, ktb], [tmp4b], lambda: nc.vector.tensor_tensor(out=tmp4[:], in0=q_tok[:], in1=k_tok[:], op=ALU.mult))
        Vv([tmp4b], [snb], lambda: nc.vector.tensor_reduce(out=snew[:, 0, :], in_=tmp4[:, :].rearrange("p (h d) -> p h d", h=NH),
                                                          axis=AX.X, op=ALU.add))
        Aa([snb], [snb], lambda: nc.scalar.activation(out=snew[:, 1, :], in_=snew[:, 0, :], func=AF.Exp, scale=0.125))
        Vv([snb, vtb], [tmp4b], lambda: nc.vector.tensor_tensor(
            out=tmp4[:, :].rearrange("p (h d) -> p h d", h=NH), in0=v_tok[:, :].rearrange("p (h d) -> p h d", h=NH),
            in1=snew[:, 1, :].rearrange("p (h o) -> p h o", o=1).broadcast_to([4, NH, HD]), op=ALU.mult))
        Vv([pb[5], tmp4b], [Osb], lambda: nc.vector.tensor_tensor(out=Osum[:], in0=ps[5][0:4, :], in1=tmp4[:], op=ALU.add))
        Vv([pb[6], snb], [snb], lambda: nc.vector.tensor_tensor(out=snew[:, 2, :], in0=ps[6][0:4, 0:NH], in1=snew[:, 1, :],
                                                               op=ALU.add))
        Vv([snb], [snb], lambda: nc.vector.reciprocal(out=snew[:, 2, :], in_=snew[:, 2, :]))
        Vv([Osb, snb], [atkb], lambda: nc.vector.tensor_tensor(
            out=attn_tok[:, :].rearrange("p (h d) -> p h d", h=NH), in0=Osum[:, :].rearrange("p (h d) -> p h d", h=NH),
            in1=snew[:, 2, :].rearrange("p (h o) -> p h o", o=1).broadcast_to([4, NH, HD]), op=ALU.mult))
        for h in range(NH):
            transpose(ps[7][0:64, h * 4:(h + 1) * 4], attn_tok[0:4, h * 64:(h + 1) * 64], ident[0:4, 0:4],
                      [atkb, cB], pb[7])
        Aa([pb[7]], [atsb], lambda: nc.scalar.copy(out=attnT_s[:, :, :],
                                                  in_=ps[7][0:64, 0:32].rearrange("p (h t) -> p h t", h=NH)))
        barrier()
    if STOP == 2:
        cx.drain("sp")
        return

    with ExitStack() as s1:
        def sb1(name, shape, dt=F32):
            return s1.enter_context(nc.sbuf_tensor("p1_" + name, list(shape), dt))
        wk = sb1("wk", [128, 8, 512], BF16)
        wkr = sb1("wkr", [128, 8, 512], BF16)
        wv = sb1("wv", [128, 8, 512], BF16)
        wB = Buf()
        for (t_, c0, srcw) in ((wk, 512, s_win), (wkr, 512, s_wrot), (wv, 1024, s_win)):
            cx.dma("sp", t_[:], srcw[:, c0:c0 + 512].rearrange("(kc p) j -> p kc j", p=128),
                   reads=[convs[srcw.tensor.name]], writes=[wB], stream="w")
        cs_t = sb1("cs_t", [128, G])
        sn_t = sb1("sn_t", [128, G])
        csB = Buf()
        kT_f = sb1("kT_f", [128, G])
        kT_fb = Buf()
        kT_h = sb1("kT_h", [128, G], BF16)
        kT_hb = Buf()
        t1 = sb1("t1", [128, G])
        t1b = Buf()
        ktok = sb1("ktok", [128, 512])
        ktokb = Buf()
        vtok = sb1("vtok", [128, 512])
        vtokb = Buf()
        vaug = sb1("vaug", [128, NH, 4, 66], BF16)
        vaugb = Buf()
        Pp([], [vaugb], lambda: nc.gpsimd.memset(vaug[:], 1.0))

        for g in range(NG):
            load_xT(xseq, g * G, G, None, xT_b, None, xb_buf, xt_tiles, xt_bufs, 0, 1)
            cx.dma("sp", cs_t[:], cosk[:, g * G:(g + 1) * G], writes=[csB], stream="c")
            cx.dma("sp", sn_t[:], sink[:, g * G:(g + 1) * G], writes=[csB], stream="c")
            for c in range(4):
                mm_group(ps[2][:, :], [(wk[:, kc, c * 128:(c + 1) * 128], xT_b[:, kc, :]) for kc in range(8)],
                         [wB, xb_buf], pb[2])
                mm_group(ps[3][:, :], [(wkr[:, kc, c * 128:(c + 1) * 128], xT_b[:, kc, :]) for kc in range(8)],
                         [wB, xb_buf], pb[3])
                Vv([pb[2], csB], [t1b],
                   lambda: nc.vector.tensor_tensor(out=t1[:], in0=ps[2][:, :], in1=cs_t[:], op=ALU.mult))
                Vv([pb[3], csB], [kT_fb],
                   lambda: nc.vector.tensor_tensor(out=kT_f[:], in0=ps[3][:, :], in1=sn_t[:], op=ALU.mult))
                Vv([t1b], [kT_fb],
                   lambda: nc.vector.tensor_tensor(out=kT_f[:], in0=kT_f[:], in1=t1[:], op=ALU.add))
                Aa([kT_fb], [kT_hb], lambda: nc.scalar.copy(out=kT_h[:], in_=kT_f[:]))
                cx.dma("sp", s_kT[2 * c:2 * c + 2, :, g * G:(g + 1) * G].rearrange("h d t -> (h d) t"), kT_h[:],
                       reads=[kT_hb], stream="ks")
                Vv([kT_fb], [kmB],
                   lambda: nc.vector.tensor_reduce(out=kmT[:, c, 2 * g:2 * g + 2],
                                                   in_=kT_f[:, :].rearrange("p (b t) -> p b t", b=2),
                                                   axis=AX.X, op=ALU.add))
                for t in range(4):
                    transpose(ps[4][:, t * 128:(t + 1) * 128], kT_f[:, t * 128:(t + 1) * 128], ident[:],
                              [kT_fb, cB], pb[4])
                Aa([pb[4]], [ktokb], lambda: nc.scalar.copy(out=ktok[:, :], in_=ps[4][:, :]))
                cx.dma("sp", k_out[g * G:(g + 1) * G, c * 128:(c + 1) * 128].rearrange("(t p) j -> p t j", p=128),
                       ktok[:, :].rearrange("p (t j) -> p t j", t=4), reads=[ktokb], stream="ko")
            for t in range(4):
                mm_group(ps[5][:, :], [(xT_b[:, kc, t * 128:(t + 1) * 128], wv[:, kc, :]) for kc in range(8)],
                         [wB, xb_buf], pb[5])
                Aa([pb[5]], [vtokb], lambda: nc.scalar.copy(out=vtok[:], in_=ps[5][:, :]))
                Vv([vtokb], [vaugb],
                   lambda: nc.vector.tensor_copy(out=vaug[:, :, t, 0:64],
                                                 in_=vtok[:, :].rearrange("p (h d) -> p h d", h=NH)))
                cx.dma("sp", v_out[g * G + t * 128: g * G + (t + 1) * 128, :], vtok[:], reads=[vtokb], stream="vo")
            for h in range(NH):
                cx.dma("sp", s_v[h, :, g * 4:(g + 1) * 4, :], vaug[:, h, :, :], reads=[vaugb], stream="vs")
        Vv([], [kmB], lambda: nc.vector.memset(kmbd[:], 0.0))
        Vv([kmB], [kmB], lambda: nc.vector.tensor_scalar(out=kmbd[0:64, :, 0:16], in0=kmT[0:64, :, :],
                                                         scalar1=1.0 / 256, scalar2=None, op0=ALU.mult))
        Vv([kmB], [kmB], lambda: nc.vector.tensor_scalar(out=kmbd[64:128, :, 16:32], in0=kmT[64:128, :, :],
                                                         scalar1=1.0 / 256, scalar2=None, op0=ALU.mult))
        barrier()
    if STOP == 1:
        cx.drain("sp")
        return

    def layer_norm(zT, zb, gcol, bcol, outf, outfb, outb, outbb, tmp, tmpb, mean_sb, rstd_sb, stb, T):
        for oc in range(8):
            Aa([zb], [tmpb], lambda: nc.scalar.activation(out=tmp[:, 0:T], in_=zT[:, oc, 0:T], func=AF.Square))
            cx.deps("pe", [zb, cB], [pb[6]] if oc == 0 else [])
            ins = nc.tensor.matmul(ps[6][:, 0:T], ones_ln[:], zT[:, oc, 0:T], start=(oc == 0), stop=(oc == 7))
            cx.op("pe", ins, [zb, cB], [pb[6]] if oc == 7 else [])
            cx.deps("pe", [tmpb], [pb[7]] if oc == 0 else [])
            ins = nc.tensor.matmul(ps[7][:, 0:T], ones_ln[:], tmp[:, 0:T], start=(oc == 0), stop=(oc == 7))
            cx.op("pe", ins, [tmpb], [pb[7]] if oc == 7 else [])
        Aa([pb[6]], [stb], lambda: nc.scalar.copy(out=mean_sb[:, 0:T], in_=ps[6][:, 0:T]))
        Vv([stb], [tmpb], lambda: nc.vector.tensor_tensor(out=tmp[:, 0:T], in0=mean_sb[:, 0:T], in1=mean_sb[:, 0:T],
                                                         op=ALU.mult))
        Vv([pb[7], tmpb], [tmpb], lambda: nc.vector.tensor_tensor(out=tmp[:, 0:T], in0=ps[7][:, 0:T], in1=tmp[:, 0:T],
                                                                 op=ALU.subtract))
        Aa([tmpb], [tmpb], lambda: nc.scalar.activation(out=tmp[:, 0:T], in_=tmp[:, 0:T], func=AF.Sqrt, bias=eps_t[:, 0:1]))
        Vv([tmpb], [stb], lambda: nc.vector.reciprocal(out=rstd_sb[:, 0:T], in_=tmp[:, 0:T]))
        for oc in range(8):
            Vv([zb, stb], [tmpb], lambda: nc.vector.tensor_tensor(out=tmp[:, 0:T], in0=zT[:, oc, 0:T], in1=mean_sb[:, 0:T],
                                                                 op=ALU.subtract))
            Vv([stb], [tmpb], lambda: nc.vector.tensor_tensor(out=tmp[:, 0:T], in0=tmp[:, 0:T], in1=rstd_sb[:, 0:T],
                                                             op=ALU.mult))
            Vv([tmpb, cB], [outfb], lambda: nc.vector.tensor_scalar(out=outf[:, oc, 0:T], in0=tmp[:, 0:T],
                                                                   scalar1=fp[:, gcol + oc:gcol + oc + 1],
                                                                   scalar2=fp[:, bcol + oc:bcol + oc + 1],
                                                                   op0=ALU.mult, op1=ALU.add))
            if outb is not None:
                Aa([outfb], [outbb], lambda: nc.scalar.copy(out=outb[:, oc, 0:T], in_=outf[:, oc, 0:T]))

    eps_t = sb("eps_t", [128, 1])
    Vv([], [cB], lambda: nc.vector.memset(eps_t[:], LN_EPS))
    x1T_f = sb("x1T_f", [128, 8, G])
    x1fb = Buf()
    x1T_b = sb("x1T_b", [128, 8, G], BF16)
    x1bb = Buf()
    tmp = sb("ln_tmp", [128, G])
    tmpb = Buf()
    mean_sb = sb("mean_sb", [128, G])
    rstd_sb = sb("rstd_sb", [128, G])
    stb = Buf()

    def moe_and_out(sbb, row0, T, y_dst):
        nt = min(128, T)
        ntile = T // nt
        Lg = sbb("Lg", [128, 20])
        rs = sbb("rs", [128, 64])
        esel = sbb("esel", [128, 8])
        top8e = sbb("top8e", [128, 8])
        comb = sbb("comb", [128, 16])
        rb = Buf()
        combT = sbb("combT", [16, G])
        cTb = Buf()
        bce = sbb("bce", [128, G])
        bceb = Buf()
        wg = [sbb(f"wg{i}", [128, 8, DE], BF16) for i in range(3)]
        wu = [sbb(f"wu{i}", [128, 8, DE], BF16) for i in range(3)]
        wgb = [Buf(), Buf(), Buf()]
        wub = [Buf(), Buf(), Buf()]
        silt = [sbb(f"silt{i}", [128, G]) for i in range(2)]
        siltb = [Buf(), Buf()]
        hT = sbb("hT", [128, 32, G], BF16)
        hTb = Buf()
        wdblk2 = [sbb(f"wdblk{i}", [128, 32, 512], BF16) for i in range(2)]
        wdb2 = [Buf(), Buf()]
        for half in range(2):
            cx.dma("sp", wdblk2[half][:], s_ed[:, half * 512:(half + 1) * 512].rearrange("(j p) o -> p j o", p=128),
                   reads=[convs["s_ed"]], writes=[wdb2[half]], stream="wd")
        otile = sbb("otile", [128, D])
        otb = Buf()

        Vv([], [rb], lambda: nc.vector.memset(esel[:], NEG))
        for t in range(ntile):
            ts_ = slice(t * nt, (t + 1) * nt)
            P_ = slice(0, nt)
            cx.deps("pe", [x1fb, cB], [pb[0]])
            ins = None
            for kc in range(8):
                ins = nc.tensor.matmul(ps[0][P_, 0:20], x1T_f[:, kc, ts_], wr_sb[:, kc * 20:(kc + 1) * 20],
                                       start=(kc == 0), stop=(kc == 7))
            cx.op("pe", ins, [x1fb, cB], [pb[0]])
            Vv([pb[0], cB], [rb], lambda: nc.vector.tensor_tensor(out=Lg[P_, :], in0=ps[0][P_, 0:20], in1=br_sb[P_, :],
                                                                 op=ALU.add))
            Vv([rb], [rb], lambda: nc.vector.tensor_reduce(out=rs[P_, 0:1], in_=Lg[P_, 0:4], axis=AX.X, op=ALU.max))
            Vv([rb], [rb], lambda: nc.vector.tensor_scalar(out=rs[P_, 4:8], in0=Lg[P_, 0:4], scalar1=rs[P_, 0:1],
                                                          scalar2=None, op0=ALU.is_ge))
            Vv([rb], [rb], lambda: nc.vector.tensor_scalar(out=rs[P_, 1:2], in0=rs[P_, 0:1], scalar1=-1.0,
                                                          scalar2=None, op0=ALU.mult))
            Aa([rb], [rb], lambda: nc.scalar.activation(out=rs[P_, 8:12], in_=Lg[P_, 0:4], func=AF.Exp,
                                                       bias=rs[P_, 1:2], accum_out=rs[P_, 2:3]))
            Vv([rb], [rb], lambda: nc.vector.reciprocal(out=rs[P_, 3:4], in_=rs[P_, 2:3]))
            Vv([rb], [rb], lambda: nc.vector.tensor_scalar(out=esel[P_, 0:4], in0=Lg[P_, 4:8], scalar1=rs[P_, 4:5],
                                                          scalar2=None, op0=ALU.mult))
            for g4 in range(1, 4):
                Vv([rb], [rb], lambda: nc.vector.scalar_tensor_tensor(out=esel[P_, 0:4], in0=Lg[P_, 4 + 4 * g4:8 + 4 * g4],
                                                                     scalar=rs[P_, 4 + g4:5 + g4], in1=esel[P_, 0:4],
                                                                     op0=ALU.mult, op1=ALU.add))
            Vv([rb], [rb], lambda: nc.vector.max(out=top8e[P_, :], in_=esel[P_, :]))
            Vv([rb], [rb], lambda: nc.vector.tensor_tensor(out=rs[P_, 12:13], in0=top8e[P_, 0:1], in1=top8e[P_, 1:2],
                                                          op=ALU.subtract))
            Aa([rb], [rb], lambda: nc.scalar.activation(out=rs[P_, 13:14], in_=rs[P_, 12:13], func=AF.Sigmoid))
            Vv([rb], [rb], lambda: nc.vector.tensor_tensor(out=rs[P_, 14:15], in0=rs[P_, 13:14], in1=rs[P_, 3:4],
                                                          op=ALU.mult))
            Vv([rb], [rb], lambda: nc.vector.tensor_tensor(out=rs[P_, 15:16], in0=rs[P_, 3:4], in1=rs[P_, 14:15],
                                                          op=ALU.subtract))
            Vv([rb], [rb], lambda: nc.vector.tensor_tensor(out=rs[P_, 16:17], in0=rs[P_, 14:15], in1=rs[P_, 15:16],
                                                          op=ALU.subtract))
            Vv([rb], [rb], lambda: nc.vector.tensor_scalar(out=rs[P_, 20:24], in0=esel[P_, 0:4], scalar1=top8e[P_, 0:1],
                                                          scalar2=None, op0=ALU.is_ge))
            Vv([rb], [rb], lambda: nc.vector.tensor_scalar(out=rs[P_, 24:28], in0=esel[P_, 0:4], scalar1=top8e[P_, 1:2],
                                                          scalar2=None, op0=ALU.is_ge))
            Vv([rb], [rb], lambda: nc.vector.tensor_scalar(out=rs[P_, 28:32], in0=rs[P_, 24:28], scalar1=rs[P_, 15:16],
                                                          scalar2=None, op0=ALU.mult))
            Vv([rb], [rb], lambda: nc.vector.scalar_tensor_tensor(out=rs[P_, 28:32], in0=rs[P_, 20:24],
                                                                 scalar=rs[P_, 16:17], in1=rs[P_, 28:32],
                                                                 op0=ALU.mult, op1=ALU.add))
            for g4 in range(4):
                Vv([rb], [rb], lambda: nc.vector.tensor_scalar(out=comb[P_, 4 * g4:4 * g4 + 4], in0=rs[P_, 28:32],
                                                              scalar1=rs[P_, 4 + g4:5 + g4], scalar2=None,
                                                              op0=ALU.mult))
            transpose(ps[1][0:16, ts_], comb[P_, :], ident[P_, P_], [rb, cB], pb[1])
        Aa([pb[1]], [cTb], lambda: nc.scalar.copy(out=combT[:, 0:T], in_=ps[1][0:16, 0:T]))

        for e in range(NE):
            i2 = e % 3
            cx.dma("sp", wg[i2][:], s_eg[e * D:(e + 1) * D, :].rearrange("(kc p) f -> p kc f", p=128),
                   reads=[convs["s_eg"]], writes=[wgb[i2]], stream="we")
            cx.dma("sp", wu[i2][:], s_eu[e * D:(e + 1) * D, :].rearrange("(kc p) f -> p kc f", p=128),
                   reads=[convs["s_eu"]], writes=[wub[i2]], stream="we")
            mm_group(ps[2][:, 0:T], [(sel16[0:16, e * 128:(e + 1) * 128], combT[0:16, 0:T])], [cTb, cB], pb[2])
            Aa([pb[2]], [bceb], lambda: nc.scalar.copy(out=bce[:, 0:T], in_=ps[2][:, 0:T]))
            for fc in range(2):
                j = e * 2 + fc
                pg, pu, sj = 3 + j % 2, 5 + j % 2, j % 2
                fs = slice(fc * 128, (fc + 1) * 128)
                mm_group(ps[pg][:, 0:T], [(wg[i2][:, kc, fs], x1T_b[:, kc, 0:T]) for kc in range(8)],
                         [wgb[i2], x1bb], pb[pg])
                mm_group(ps[pu][:, 0:T], [(wu[i2][:, kc, fs], x1T_b[:, kc, 0:T]) for kc in range(8)],
                         [wub[i2], x1bb], pb[pu])
                Aa([pb[pg]], [siltb[sj]], lambda: nc.scalar.activation(out=silt[sj][:, 0:T], in_=ps[pg][:, 0:T],
                                                                      func=AF.Silu))
                Pp([bceb], [siltb[sj]], lambda: nc.gpsimd.tensor_tensor(out=silt[sj][:, 0:T], in0=silt[sj][:, 0:T],
                                                                       in1=bce[:, 0:T], op=ALU.mult))
                Vv([pb[pu], siltb[sj]], [hTb], lambda: nc.vector.tensor_tensor(out=hT[:, j, 0:T], in0=ps[pu][:, 0:T],
                                                                              in1=silt[sj][:, 0:T], op=ALU.mult))
        for half in range(2):
            wdblk, wdb = wdblk2[half], wdb2[half]
            for o4 in range(4):
                oc = half * 4 + o4
                pi = oc % 2
                mm_group(ps[pi][:, 0:T], [(wdblk[:, j, o4 * 128:(o4 + 1) * 128], hT[:, j, 0:T]) for j in range(32)],
                         [wdb, hTb], pb[pi])
                Vv([pb[pi], x1fb], [x1fb],
                   lambda: nc.vector.scalar_tensor_tensor(out=x1T_f[:, oc, 0:T], in0=x1T_f[:, oc, 0:T], scalar=ALPHA,
                                                          in1=ps[pi][:, 0:T], op0=ALU.mult, op1=ALU.add))
        layer_norm(x1T_f, x1fb, 16, 24, x1T_f, x1fb, None, None, tmp, tmpb, mean_sb, rstd_sb, stb, T)
        for t in range(ntile):
            ts_ = slice(t * nt, (t + 1) * nt)
            for half in range(2):
                pi = 3 + half
                for j in range(4):
                    oc = half * 4 + j
                    transpose(ps[pi][0:nt, j * 128:(j + 1) * 128], x1T_f[:, oc, ts_], ident[:], [x1fb, cB], pb[pi])
                if half == 0:
                    Aa([pb[pi]], [otb], lambda: nc.scalar.copy(out=otile[0:nt, 0:512], in_=ps[pi][0:nt, :]))
                else:
                    Vv([pb[pi]], [otb], lambda: nc.vector.tensor_copy(out=otile[0:nt, 512:1024], in_=ps[pi][0:nt, :]))
            cx.dma("sp", y_dst[row0 + t * nt: row0 + (t + 1) * nt, :], otile[0:nt, :], reads=[otb], stream="yo")

    def merge_ln1(sba, T, xT_f, xfb, xTb, xb_buf, attnT, atb, pooledT, plb):
        wblk = [sba(f"m_wblk{i}", [128, 8, 512], BF16) for i in range(2)]
        wblkb = [Buf(), Buf()]
        wa_t = sba("wa_t", [64, NH, 512], BF16)
        wab = Buf()
        wb_t = sba("wb_t", [128, 4, 512], BF16)
        wbb = Buf()
        sga = sba("m_sga", [128, T])
        sgab = Buf()
        sgb = sba("m_sgb", [128, T])
        sgbb = Buf()
        t1 = sba("m_t1", [128, T])
        t1b = Buf()
        t2 = sba("m_t2", [128, T])
        t2b = Buf()
        mergedT = sba("mergedT", [128, 8, T], BF16)
        mgb = Buf()
        for half in range(2):
            load_wblk(wblk[0], wblkb[0], s_win, 2048 + half * 512)
            load_wblk(wblk[1], wblkb[1], s_win, 3072 + half * 512)
            cx.dma("sp", wa_t[:], s_wa[:, half * 512:(half + 1) * 512].rearrange("(h d) o -> d h o", h=NH),
                   reads=[convs["s_wa"]], writes=[wab], stream="w")
            cx.dma("sp", wb_t[:], s_wb[:, half * 512:(half + 1) * 512].rearrange("(g c) o -> c g o", g=4),
                   reads=[convs["s_wb"]], writes=[wbb], stream="w")
            for o4 in range(4):
                oc = half * 4 + o4
                cs_ = slice(o4 * 128, (o4 + 1) * 128)
                mm_group(ps[2][:, 0:T], [(wblk[0][:, kc, cs_], xTb[:, kc, 0:T]) for kc in range(8)],
                         [wblkb[0], xb_buf], pb[2])
                mm_group(ps[3][:, 0:T], [(wblk[1][:, kc, cs_], xTb[:, kc, 0:T]) for kc in range(8)],
                         [wblkb[1], xb_buf], pb[3])
                Aa([pb[2], cB], [sgab], lambda: nc.scalar.activation(out=sga[:], in_=ps[2][:, 0:T], func=AF.Sigmoid,
                                                                    bias=fp[:, 32 + oc:33 + oc]))
                Aa([pb[3], cB], [sgbb], lambda: nc.scalar.activation(out=sgb[:], in_=ps[3][:, 0:T], func=AF.Sigmoid,
                                                                    bias=fp[:, 40 + oc:41 + oc]))
                mm_group(ps[0][:, 0:T], [(wa_t[0:64, h, cs_], attnT[0:64, h, 0:T]) for h in range(NH)],
                         [wab, atb], pb[0])
                mm_group(ps[1][:, 0:T], [(wb_t[:, g4, cs_], pooledT[:, g4, 0:T]) for g4 in range(4)],
                         [wbb, plb], pb[1])
                Vv([pb[0], sgab], [t1b], lambda: nc.vector.tensor_tensor(out=t1[:], in0=ps[0][:, 0:T], in1=sga[:],
                                                                        op=ALU.mult))
                Vv([pb[1], sgbb], [t2b], lambda: nc.vector.tensor_tensor(out=t2[:], in0=ps[1][:, 0:T], in1=sgb[:],
                                                                        op=ALU.mult))
                Vv([t1b, t2b], [mgb], lambda: nc.vector.tensor_tensor(out=mergedT[:, oc, :], in0=t1[:], in1=t2[:],
                                                                     op=ALU.add))
        for half in range(2):
            load_wblk(wblk[half], wblkb[half], s_wout, half * 512)
            for o4 in range(4):
                oc = half * 4 + o4
                pi = 2 + (oc % 2)
                mm_group(ps[pi][:, 0:T], [(wblk[half][:, kc, o4 * 128:(o4 + 1) * 128], mergedT[:, kc, :])
                                          for kc in range(8)], [wblkb[half], mgb], pb[pi])
                Vv([pb[pi], xfb], [xfb],
                   lambda: nc.vector.scalar_tensor_tensor(out=xT_f[:, oc, 0:T], in0=xT_f[:, oc, 0:T], scalar=ALPHA,
                                                          in1=ps[pi][:, 0:T], op0=ALU.mult, op1=ALU.add))
        layer_norm(xT_f, xfb, 0, 8, x1T_f, x1fb, x1T_b, x1bb, tmp, tmpb, mean_sb, rstd_sb, stb, T)

    for slot in range(NSLOT):
        cx.epoch()
        nkt = 8 * (slot + 1)
        with ExitStack() as sA:
            def sba(name, shape, dt=F32):
                return sA.enter_context(nc.sbuf_tensor(f"a{slot}_" + name, list(shape), dt))
            xT_f = sba("xT_f", [128, 8, G])
            xfb = Buf()
            attnT = sba("attnT", [64, NH, G], BF16)
            atb = Buf()
            pooledT = sba("pooledT", [128, 4, G], BF16)
            plb = Buf()
            sA1 = ExitStack()

            def sba1(name, shape, dt=F32):
                return sA1.enter_context(nc.sbuf_tensor(f"a1{slot}_" + name, list(shape), dt))
            wblk = [sba1(f"wblk{i}", [128, 8, 512], BF16) for i in range(2)]
            wblkb = [Buf(), Buf()]
            cq_t = sba1("cq_t", [128, G])
            sq_t = sba1("sq_t", [128, G])
            cqb = Buf()
            qT_f = sba1("qT_f", [128, 4, G])
            qfb = Buf()
            qaug = sba1("qaug", [80, NH, G], BF16)
            qab = Buf()
            t1 = sba1("t1", [128, G])
            t1b = Buf()
            kaug = [sba1(f"kaug{i}", [80, SEQ], BF16) for i in range(2)]
            kab = [Buf(), Buf()]
            vh = [sba1(f"vh{i}", [128, 32, 66], BF16) for i in range(2)]
            vhb = [Buf(), Buf()]
            pT = [sba1(f"pT{i}", [128, G], BF16) for i in range(3)]
            pTb = [Buf(), Buf(), Buf()]
            cmk = sba1("cmk", [128, 8, G], BF16)
            cmkb = Buf()
            rden = sba1("rden", [65, G])
            rdb = Buf()
            rden0 = sba1("rden0", [1, G])
            rd0b = Buf()
            bcs = sba1("bcs", [64, G])
            bcsb = Buf()
            gm = sba1("gm", [128, 128])
            gmb = Buf()
            msk = sba1("msk", [128, 3, 128])
            mskb = Buf()
            top8 = sba1("top8", [128, NH, 8])
            top8b = Buf()
            sel = sba1("sel", [128, 128])
            selb = Buf()
            biasw = sba1("biasw", [128, NH, 32])
            bwb = Buf()
            uT = sba1("uT", [128, 4, 16 + G])
            uTb = Buf()
            xh = sba1("xh", [16, D])
            xhb = Buf()
            xhT = sba1("xhT", [128, 8, 16], BF16)
            xhTb = Buf()
            pa = sba1("pa", [128, 16 + G])
            pab = Buf()
            pbt = sba1("pbt", [128, 16 + G])
            pbb = Buf()
            invt = sba1("invt", [128, G])
            invb = Buf()
            diffT = sba1("diffT", [128, G], BF16)
            dfb = Buf()
            wpool_t = sba1("wpool_t", [128, 4, 128], BF16)
            wpb = Buf()
            sga = sba1("sga", [128, G])
            sgab = Buf()

            row0 = slot * G
            load_xT(xown, row0, G, xT_f, xT_b, xfb, xb_buf, xt_tiles, xt_bufs, 0, 1)
            cx.dma("sp", cq_t[:], cosq[:, row0:row0 + G], writes=[cqb], stream="c")
            cx.dma("sp", sq_t[:], sinq[:, row0:row0 + G], writes=[cqb], stream="c")
            cx.dma("sp", cmk[:], cmask[slot % 2, :, :].rearrange("p (j t) -> p j t", j=8), writes=[cmkb], stream="c")
            Vv([], [bwb], lambda: nc.vector.memset(biasw[:], 0.0))
            Vv([], [pab], lambda: nc.vector.memset(pa[:], 0.0))
            Vv([], [pbb], lambda: nc.vector.memset(pbt[:], 0.0))

            load_wblk(wblk[0], wblkb[0], s_win, 0)
            load_wblk(wblk[1], wblkb[1], s_wrot, 0)
            for c in range(4):
                mm_group(ps[2][:, :], [(wblk[0][:, kc, c * 128:(c + 1) * 128], xT_b[:, kc, :]) for kc in range(8)],
                         [wblkb[0], xb_buf], pb[2])
                mm_group(ps[3][:, :], [(wblk[1][:, kc, c * 128:(c + 1) * 128], xT_b[:, kc, :]) for kc in range(8)],
                         [wblkb[1], xb_buf], pb[3])
                Vv([pb[2], cqb], [t1b],
                   lambda: nc.vector.tensor_tensor(out=t1[:], in0=ps[2][:, :], in1=cq_t[:], op=ALU.mult))
                Vv([pb[3], cqb], [qfb],
                   lambda: nc.vector.tensor_tensor(out=qT_f[:, c, :], in0=ps[3][:, :], in1=sq_t[:], op=ALU.mult))
                Vv([t1b], [qfb],
                   lambda: nc.vector.tensor_tensor(out=qT_f[:, c, :], in0=qT_f[:, c, :], in1=t1[:], op=ALU.add))
                Aa([qfb], [qab], lambda: nc.scalar.copy(out=qaug[0:64, 2 * c, :], in_=qT_f[0:64, c, :]))
                Aa([qfb], [qab], lambda: nc.scalar.copy(out=qaug[0:64, 2 * c + 1, :], in_=qT_f[64:128, c, :]))

            for t in range(4):
                tr0 = row0 + t * 128
                cx.dma("sp", msk[:, 0, :], negm[tr0:tr0 + 128, :], writes=[mskb], stream="m")
                cx.dma("sp", msk[:, 1, :], valm[tr0:tr0 + 128, :], writes=[mskb], stream="m")
                cx.dma("sp", msk[:, 2, :], ownm[tr0:tr0 + 128, :], writes=[mskb], stream="m")
                cx.deps("pe", [qfb, kmB], [pb[4]])
                ins = None
                for c in range(4):
                    ins = nc.tensor.matmul(ps[4][:, c * 32:(c + 1) * 32], qT_f[:, c, t * 128:(t + 1) * 128],
                                           kmbd[:, c, :], start=True, stop=True)
                cx.op("pe", ins, [qfb, kmB], [pb[4]])
                Vv([pb[4], mskb], [gmb],
                   lambda: nc.vector.tensor_tensor(out=gm[:], in0=ps[4][:, 0:128], in1=msk[:, 0, :], op=ALU.add))
                for h in range(NH):
                    Vv([gmb], [top8b], lambda: nc.vector.max(out=top8[:, h, :], in_=gm[:, h * 16:(h + 1) * 16]))
                for h in range(NH):
                    Vv([gmb, top8b], [selb],
                       lambda: nc.vector.tensor_scalar(out=sel[:, h * 16:(h + 1) * 16], in0=gm[:, h * 16:(h + 1) * 16],
                                                       scalar1=top8[:, h, 2:3], scalar2=None, op0=ALU.is_ge))
                Vv([selb, mskb], [selb],
                   lambda: nc.vector.tensor_tensor(out=sel[:], in0=sel[:], in1=msk[:, 1, :], op=ALU.mult))
                Vv([selb, mskb], [selb],
                   lambda: nc.vector.tensor_tensor(out=sel[:], in0=sel[:], in1=msk[:, 2, :], op=ALU.add))
                Vv([selb], [bwb],
                   lambda: nc.vector.tensor_scalar(out=biasw[:, :, 0:16],
                                                   in0=sel[:, :].rearrange("p (h n) -> p h n", h=NH),
                                                   scalar1=BIG, scalar2=-BIG, op0=ALU.mult, op1=ALU.add))
                for b2 in range(2):
                    transpose(ps[5][:, b2 * 128:(b2 + 1) * 128],
                              biasw[:, b2 * 4:(b2 + 1) * 4, :].rearrange("p h n -> p (h n)"), ident[:],
                              [bwb, cB], pb[5])
                for h in range(NH):
                    p0 = (h % 4) * 32
                    c0 = (h // 4) * 128
                    Aa([pb[5]], [qab], lambda: nc.scalar.copy(out=qaug[64:80, h, t * 128:(t + 1) * 128],
                                                             in_=ps[5][p0:p0 + 16, c0:c0 + 128]))

            nkeys = nkt * 128
            for h in range(NH):
                kb_ = h % 2
                cx.dma("sp", kaug[kb_][0:64, 0:nkeys], s_kT[h, :, 0:nkeys], writes=[kab[kb_]], stream="ka")
                cx.dma("sp", kaug[kb_][64:80, 0:nkeys], blkind[:, 0:nkeys], writes=[kab[kb_]], stream="ka")
                cx.dma("sp", vh[kb_][:, 0:nkt, :], s_v[h, :, 0:nkt, :], writes=[vhb[kb_]], stream="va")
                for kt in range(nkt):
                    sbk = 6 + (kt % 2)
                    pj = kt % 3
                    mm_group(ps[sbk][:, :], [(kaug[kb_][0:80, kt * 128:(kt + 1) * 128], qaug[0:80, h, :])],
                             [kab[kb_], qab], pb[sbk])
                    Aa([pb[sbk]], [pTb[pj]],
                       lambda: nc.scalar.activation(out=pT[pj][:], in_=ps[sbk][:, :], func=AF.Exp, scale=0.125))
                    if kt >= nkt - 8:
                        jj = kt - (nkt - 8)
                        Pp([cmkb], [pTb[pj]],
                           lambda: nc.gpsimd.tensor_tensor(out=pT[pj][:], in0=pT[pj][:], in1=cmk[:, jj, :], op=ALU.mult))
                    cx.deps("pe", [vhb[kb_], pTb[pj]], [pb[4]] if kt == 0 else [])
                    ins = nc.tensor.matmul(ps[4][0:65, :], vh[kb_][:, kt, 0:65], pT[pj][:], start=(kt == 0),
                                           stop=(kt == nkt - 1))
                    cx.op("pe", ins, [vhb[kb_], pTb[pj]], [pb[4]] if kt == nkt - 1 else [])
                Vv([pb[4]], [rdb], lambda: nc.vector.reciprocal(out=rden[64:65, :], in_=ps[4][64:65, :]))
                Aa([rdb], [rd0b], lambda: nc.scalar.copy(out=rden0[0:1, :], in_=rden[64:65, :]))
                mm_group(ps[5][0:64, :], [(ones1[0:1, 0:64], rden0[0:1, :])], [rd0b, cB], pb[5])
                Aa([pb[5]], [bcsb], lambda: nc.scalar.copy(out=bcs[:], in_=ps[5][0:64, :]))
                Vv([pb[4], bcsb], [atb],
                   lambda: nc.vector.tensor_tensor(out=attnT[0:64, h, :], in0=ps[4][0:64, :], in1=bcs[:], op=ALU.mult))

            cx.dma("sp", xh[:], xhalo[slot * 16:(slot + 1) * 16, :], writes=[xhb], stream="c")
            for kc in range(8):
                transpose(ps[0][:, kc * 16:(kc + 1) * 16], xh[:, kc * 128:(kc + 1) * 128], ident[0:16, 0:16],
                          [xhb, cB], pb[0])
            Vv([pb[0]], [xhTb], lambda: nc.vector.tensor_copy(out=xhT[:, :, :],
                                                             in_=ps[0][:, 0:128].rearrange("p (k t) -> p k t", k=8)))
            load_wblk(wblk[0], wblkb[0], s_win, 1536)
            cx.dma("sp", wpool_t[:], s_wpool[:, :].rearrange("(g c) e -> c g e", g=4), reads=[convs["s_wpool"]], writes=[wpb],
                   stream="w")
            for g4 in range(4):
                mm_group(ps[2][:, :], [(wblk[0][:, kc, g4 * 128:(g4 + 1) * 128], xT_b[:, kc, :]) for kc in range(8)],
                         [wblkb[0], xb_buf], pb[2])
                mm_group(ps[3][:, 0:16], [(wblk[0][:, kc, g4 * 128:(g4 + 1) * 128], xhT[:, kc, :]) for kc in range(8)],
                         [wblkb[0], xhTb], pb[3])
                Aa([pb[2]], [uTb], lambda: nc.scalar.copy(out=uT[:, g4, 16:16 + G], in_=ps[2][:, :]))
                Aa([pb[3]], [uTb], lambda: nc.scalar.copy(out=uT[:, g4, 0:16], in_=ps[3][:, 0:16]))
                cur, curb = uT[:, g4, :], uTb
                L = 16 + G
                for k in range(g4 + 1):
                    sh = 1 << k
                    nxt, nxtb = (pa, pab) if k % 2 == 0 else (pbt, pbb)
                    Vv([curb], [nxtb], lambda: nc.vector.tensor_tensor(out=nxt[:, sh:L], in0=cur[:, sh:L],
                                                                      in1=cur[:, 0:L - sh], op=ALU.add))
                    cur, curb = nxt[:, :], nxtb
                cx.dma("sp", invt[:], invc[slot, :, g4 * G:(g4 + 1) * G], writes=[invb], stream="c")
                Vv([curb, invb], [t1b], lambda: nc.vector.tensor_tensor(out=t1[:], in0=cur[:, 16:L], in1=invt[:],
                                                                       op=ALU.mult))
                Vv([t1b, uTb], [dfb], lambda: nc.vector.tensor_tensor(out=diffT[:], in0=t1[:], in1=uT[:, g4, 16:L],
                                                                     op=ALU.subtract))
                mm_group(ps[5][:, :], [(wpool_t[:, g4, :], diffT[:])], [wpb, dfb], pb[5])
                Vv([pb[5], cB], [plb], lambda: nc.vector.tensor_scalar(out=pooledT[:, g4, :], in0=ps[5][:, :],
                                                                      scalar1=fp[:, 48 + g4:49 + g4], scalar2=None,
                                                                      op0=ALU.mult))
            if slot == NSLOT - 1:
                for g4 in range(4):
                    transpose(ps[0][:, g4 * 128:(g4 + 1) * 128], uT[:, g4, 16 + G - 128:16 + G], ident[:],
                              [uTb, cB], pb[0])
                Aa([pb[0]], [sgab], lambda: nc.scalar.copy(out=sga[:], in_=ps[0][:, :]))
                cx.dma("sp", pool_out[:, :], sga[112:128, :], reads=[sgab], stream="po")

            barrier()
            sA1.close()
            merge_ln1(sba, G, xT_f, xfb, xT_b, xb_buf, attnT, atb, pooledT, plb)
            barrier()

        with ExitStack() as sB:
            def sbb(name, shape, dt=F32):
                return sB.enter_context(nc.sbuf_tensor(f"b{slot}_" + name, list(shape), dt))
            moe_and_out(sbb, slot * G, G, y_out)
            barrier()

    with ExitStack() as sC:
        def sbc(name, shape, dt=F32):
            return sC.enter_context(nc.sbuf_tensor("c_" + name, list(shape), dt))
        merge_ln1(sbc, 4, xsT_f, xsfb, xsT_b, xsbb, attnT_s, atsb, pooledT_s, plsb)
        barrier()
    with ExitStack() as sD:
        def sbd(name, shape, dt=F32):
            return sD.enter_context(nc.sbuf_tensor("d_" + name, list(shape), dt))
        moe_and_out(sbd, 0, 4, ys_out)
        barrier()
    cx.drain("sp")


_PROG = None


def _rope_tables(pos):
    half = HD // 2
    inv_freq = 1.0 / (10000.0 ** (np.arange(0, HD, 2, dtype=np.float32) / HD))
    ang = pos.astype(np.float32)[None, :] * inv_freq[:, None].astype(np.float32)
    cos = np.cos(ang).astype(np.float32)
    sin = np.sin(ang).astype(np.float32)
    cos64 = np.concatenate([cos, cos], 0)
    sin64 = np.concatenate([-sin, sin], 0)
    return np.ascontiguousarray(np.concatenate([cos64, cos64], 0)), np.ascontiguousarray(np.concatenate([sin64, sin64], 0))


def kernel(**inp):
    global _PROG
    x_prompt = np.asarray(inp["x_prompt"], np.float32)
    w_in = np.asarray(inp["w_in"], np.float32)[0]
    wqk = w_in[:, 0:1024].reshape(D, 16, 2, 32)
    w_rot = np.ascontiguousarray(wqk[:, :, ::-1, :].reshape(D, 1024))
    w_r = np.concatenate([np.asarray(inp["w_group_router"], np.float32)[0],
                          np.asarray(inp["w_expert_router"], np.float32)[0].transpose(1, 0, 2).reshape(D, 16)], 1)
    w_r = np.ascontiguousarray(w_r.reshape(8, 128, 20).transpose(1, 0, 2).reshape(128, 160))
    b_r = np.concatenate([np.asarray(inp["b_group_router"], np.float32)[0],
                          np.asarray(inp["b_expert_router"], np.float32)[0].reshape(16)])
    b_r = np.ascontiguousarray(np.broadcast_to(b_r[None, :], (128, 20)))

    def fm(v):
        return np.asarray(v, np.float32).reshape(8, 128).T

    fpar = np.concatenate([fm(inp["ln1_g"][0]), fm(inp["ln1_b"][0]), fm(inp["ln2_g"][0]), fm(inp["ln2_b"][0]),
                           fm(inp["b_gate"][0, 0]), fm(inp["b_gate"][0, 1]),
                           np.asarray(inp["pool_scale"], np.float32)[0].reshape(4, 128).T], 1)
    fpar = np.ascontiguousarray(fpar)
    cosk, sink = _rope_tables(np.arange(SEQ))
    blk = (np.arange(SEQ)[None, :] // 256 == np.arange(16)[:, None]).astype(ml_dtypes.bfloat16)
    ident = np.eye(128, dtype=np.float32)
    sel16 = np.ascontiguousarray(np.repeat(np.eye(16, dtype=np.float32)[:, :, None], 128, axis=2).reshape(16, 16 * 128))
    kk = np.arange(128)[:, None]
    qq = np.arange(G)[None, :]
    mt = []
    for t in range(4):
        kp = 128 * t + kk
        mt.append(1.0 - ((kp // 256 == qq // 256) & (kp > qq)).astype(np.float32))
    ones = np.ones((128, G), np.float32)
    zeros = np.zeros((128, G), np.float32)
    lower = np.concatenate(mt + [zeros] * 4, 1)
    upper = np.concatenate([ones] * 4 + mt, 1)

    x_sample = np.asarray(inp["x_sample"], np.float32)[:, 0, :]
    ck = np.asarray(inp["cache_k"], np.float32)[0].reshape(2560 * 8, 8192)
    cv = np.asarray(inp["cache_v"], np.float32)[0].reshape(2560 * 8, 8192)
    page_table = np.asarray(inp["page_table"], np.int32)
    state_pool = np.asarray(inp["state_pool"], np.float32)[0]
    c8, s8 = _rope_tables(np.array([8192]))
    cs8 = np.ascontiguousarray(np.tile(c8[0:64, 0][None, :], (4, NH)))
    sn8 = np.ascontiguousarray(np.tile(s8[0:64, 0][None, :], (4, NH)))
    pp = np.arange(128)
    selT = np.zeros((4, 256), np.float32)
    pairsel = np.zeros((128, 8), np.float32)
    for t in range(2):
        for p_ in range(128):
            selT[2 * t + p_ // 64, t * 128 + p_] = 1.0
            pairsel[p_, t * 4 + 2 * t + p_ // 64] = 1.0
    in_maps = []
    for c in range(8):
        s, p = c // 2, c % 2
        groups = [0, 3, 4, 7] if p == 0 else [1, 2, 5, 6]
        xs = x_prompt[s]
        xo = np.concatenate([xs[g * G:(g + 1) * G] for g in groups], 0)
        xh = np.zeros((NSLOT * 16, D), np.float32)
        for i, g in enumerate(groups):
            if g > 0:
                xh[i * 16:(i + 1) * 16] = xs[g * G - 16:g * G]
        posq = np.concatenate([np.arange(g * G, (g + 1) * G) for g in groups])
        cq, sq = _rope_tables(posq)
        own = posq // 256
        nb = np.arange(16)[None, :]
        valm = np.ascontiguousarray(np.tile((nb < own[:, None]).astype(np.float32), (1, NH)))
        ownm = np.ascontiguousarray(np.tile((nb == own[:, None]).astype(np.float32), (1, NH)))
        negm = np.ascontiguousarray(np.tile(np.where(nb < own[:, None], 0.0, NEG).astype(np.float32), (1, NH)))
        cm = np.stack([lower if (g % 2 == 0) else upper for g in groups[:2]], 0).astype(ml_dtypes.bfloat16)
        invc = np.zeros((NSLOT, 128, 4 * G), np.float32)
        for i, g in enumerate(groups):
            pos = np.arange(g * G, (g + 1) * G)
            for gi, w in enumerate((2, 4, 8, 16)):
                invc[i, :, gi * G:(gi + 1) * G] = (1.0 / np.minimum(w, pos + 1))[None, :]
        in_maps.append({
            "xseq": np.ascontiguousarray(xs), "xown": np.ascontiguousarray(xo), "xhalo": xh,
            "w_in": w_in, "w_rot": w_rot,
            "w_pool": np.asarray(inp["w_pool"], np.float32)[0].reshape(512, 128),
            "w_a": np.asarray(inp["w_branch_a"], np.float32)[0], "w_b": np.asarray(inp["w_branch_b"], np.float32)[0],
            "w_out": np.asarray(inp["w_out"], np.float32)[0],
            "w_eg": np.asarray(inp["w_e_gate"], np.float32)[0].reshape(NE * D, DE),
            "w_eu": np.asarray(inp["w_e_up"], np.float32)[0].reshape(NE * D, DE),
            "w_ed": np.asarray(inp["w_e_down"], np.float32)[0].reshape(NE * DE, D),
            "w_r": w_r, "b_r": b_r, "fpar": fpar, "cosk": cosk, "sink": sink, "cosq": cq, "sinq": sq,
            "negm": negm, "valm": valm, "ownm": ownm, "cmask": np.ascontiguousarray(cm.reshape(2, 128, 8 * G)),
            "blkind": blk, "invc": invc, "ident": ident, "sel16": sel16,
            "xs4": np.ascontiguousarray(x_sample[4 * c:4 * c + 4]), "ck": ck, "cv": cv,
            "pt": np.ascontiguousarray(page_table[4 * c:4 * c + 4].reshape(2, 128).T),
            "st4": np.ascontiguousarray(state_pool[4 * c:4 * c + 4].reshape(4, 15 * 512)),
            "cs8": cs8, "sn8": sn8, "selT": selT, "pairsel": pairsel,
        })
    if _PROG is None:
        _PROG = build_program()
    res = run_bass_kernel_spmd(_PROG, in_maps, core_ids=list(range(8)))
    R = res.results
    y_p = np.zeros((4, SEQ, D), np.float32)
    k_p = np.zeros((1, 4, SEQ, NH, HD), np.float32)
    v_p = np.zeros((1, 4, SEQ, NH, HD), np.float32)
    pool_p = np.zeros((1, 4, 15, 512), np.float32)
    for c in range(8):
        s, p = c // 2, c % 2
        groups = [0, 3, 4, 7] if p == 0 else [1, 2, 5, 6]
        for i, g in enumerate(groups):
            y_p[s, g * G:(g + 1) * G] = R[c]["y_out"][i * G:(i + 1) * G]
        if p == 0:
            k_p[0, s] = R[c]["k_out"].reshape(SEQ, NH, HD)
            v_p[0, s] = R[c]["v_out"].reshape(SEQ, NH, HD)
            pool_p[0, s] = R[c]["pool_out"][1:16]
    y_s = np.zeros((32, 1, D), np.float32)
    k_s = np.zeros((1, 32, 1, NH, HD), np.float32)
    v_s = np.zeros((1, 32, 1, NH, HD), np.float32)
    pool_s = np.zeros((1, 32, 15, 512), np.float32)
    for c in range(8):
        y_s[4 * c:4 * c + 4, 0] = R[c]["ys_out"]
        k_s[0, 4 * c:4 * c + 4, 0] = R[c]["ks_out"].reshape(4, NH, HD)
        v_s[0, 4 * c:4 * c + 4, 0] = R[c]["vs_out"].reshape(4, NH, HD)
        pool_s[0, 4 * c:4 * c + 4] = R[c]["ps_out"].reshape(4, 15, 512)
    return (y_p, y_s, k_p, v_p, pool_p, k_s, v_s, pool_s)
```

```python
import numpy as np
import ml_dtypes
import concourse.bass as bass
import concourse.mybir as mybir
from concourse.bass_utils import run_bass_kernel_spmd

F32 = mybir.dt.float32
BF16 = mybir.dt.bfloat16
I32 = mybir.dt.int32
ALU = mybir.AluOpType
AF = mybir.ActivationFunctionType
AX = mybir.AxisListType

D = 1024
SEQ = 4096
NH = 8
HD = 64
G = 512
NG = 8
NSLOT = 4
ALPHA = 2.0 ** 0.25
BIG = 30000.0
NEG = -1.0e30
LN_EPS = 1e-5
NE = 16
DE = 256
STOP = 99


class Buf:
    __slots__ = ("w", "r")

    def __init__(self):
        self.w = None
        self.r = []


class Ctx:
    def __init__(self, nc, stack):
        self.nc = nc
        self.stack = stack
        self.eng = {"pe": nc.tensor, "act": nc.scalar, "dve": nc.vector, "pool": nc.gpsimd, "sp": nc.sync}
        self.sem = {}
        self.cnt = {}
        self.waited = {e: {} for e in self.eng}
        self.nsem = 0
        for e in self.eng:
            self._new_sem(e)
        self.dsem = {}
        self.dcnt = {}
        self.dnext = {}

    def _new_sem(self, e):
        self.nsem += 1
        s = self.stack.enter_context(self.nc.semaphore(f"s_{e}_{self.nsem}"))
        self.sem[e] = s
        self.cnt[e] = 0

    def epoch(self):
        for e in self.eng:
            if self.cnt[e] > 20000:
                self._new_sem(e)

    def _wait(self, e, tok):
        if tok is None:
            return
        sem, val = tok
        key = id(sem)
        if self.waited[e].get(key, 0) >= val:
            return
        self.waited[e][key] = val
        self.eng[e].wait_ge(sem, val)

    def deps(self, e, reads, writes):
        for b in reads:
            self._wait(e, b.w)
        for b in writes:
            self._wait(e, b.w)
            for t in b.r:
                self._wait(e, t)

    def done(self, tok, reads, writes):
        for b in reads:
            b.r.append(tok)
            if len(b.r) > 12:
                b.r = b.r[-12:]
        for b in writes:
            b.w = tok
            b.r = []

    def op(self, e, ins, reads=(), writes=()):
        self.cnt[e] += 1
        ins.then_inc(self.sem[e], 1)
        tok = (self.sem[e], self.cnt[e])
        self.waited[e][id(self.sem[e])] = max(self.waited[e].get(id(self.sem[e]), 0), 0)
        self.done(tok, reads, writes)
        return tok

    NROT = 4

    def dma(self, q, out, in_, reads=(), writes=(), stream="d", **kw):
        e = q
        self.deps(e, reads, writes)
        key = (q, stream)
        if key not in self.dsem:
            sems = []
            for i in range(self.NROT):
                self.nsem += 1
                sems.append(self.stack.enter_context(self.nc.semaphore(f"dma_{q}_{stream}_{self.nsem}")))
            self.dsem[key] = sems
            self.dcnt[key] = [0] * self.NROT
            self.dnext[key] = 0
        i = self.dnext[key]
        self.dnext[key] = (i + 1) % self.NROT
        sem = self.dsem[key][i]
        if self.dcnt[key][i] > 0:
            self._wait(e, (sem, self.dcnt[key][i]))
        self.dcnt[key][i] += 16
        self.eng[e].dma_start(out=out, in_=in_, **kw).then_inc(sem, 16)
        tok = (sem, self.dcnt[key][i])
        self.done(tok, reads, writes)
        return tok

    def dma_done(self, q, ins, reads, writes, stream):
        key = (q, stream)
        if key not in self.dsem:
            sems = []
            for i in range(self.NROT):
                self.nsem += 1
                sems.append(self.stack.enter_context(self.nc.semaphore(f"dma_{q}_{stream}_{self.nsem}")))
            self.dsem[key] = sems
            self.dcnt[key] = [0] * self.NROT
            self.dnext[key] = 0
        i = self.dnext[key]
        self.dnext[key] = (i + 1) % self.NROT
        sem = self.dsem[key][i]
        self.dcnt[key][i] += 16
        ins.then_inc(sem, 16)
        tok = (sem, self.dcnt[key][i])
        self.done(tok, reads, writes)
        return tok

    def drain(self, e):
        for key, sems in self.dsem.items():
            for i, sem in enumerate(sems):
                if self.dcnt[key][i] > 0:
                    self._wait(e, (sem, self.dcnt[key][i]))


def _emit(cx, e, reads, writes, fn):
    cx.deps(e, reads, writes)
    ins = fn()
    return cx.op(e, ins, reads, writes)


def build_program():
    from contextlib import ExitStack
    nc = bass.Bass("TRN2", target_bir_lowering=False)
    st = ExitStack()
    with st:
        _build(nc, st)
    return nc


def _build(nc, st):
    from contextlib import ExitStack
    cx = Ctx(nc, st)

    def din(name, shape, dt=F32):
        return nc.dram_tensor(name, list(shape), dt, kind="ExternalInput").ap()

    def dout(name, shape, dt=F32):
        return nc.dram_tensor(name, list(shape), dt, kind="ExternalOutput").ap()

    def dscr(name, shape, dt=BF16):
        return nc.dram_tensor(name, list(shape), dt, kind="Internal").ap()

    def sb(name, shape, dt=F32):
        return st.enter_context(nc.sbuf_tensor("sb_" + name, list(shape), dt))

    xseq = din("xseq", [SEQ, D])
    xown = din("xown", [NSLOT * G, D])
    xhalo = din("xhalo", [NSLOT * 16, D])
    w_in = din("w_in", [D, 4096])
    w_rot = din("w_rot", [D, 1024])
    w_pool = din("w_pool", [4 * 128, 128])
    w_a = din("w_a", [512, D])
    w_b = din("w_b", [512, D])
    w_out = din("w_out", [D, D])
    w_eg = din("w_eg", [NE * D, DE])
    w_eu = din("w_eu", [NE * D, DE])
    w_ed = din("w_ed", [NE * DE, D])
    w_r = din("w_r", [128, 8 * 20])
    b_r = din("b_r", [128, 20])
    fpar = din("fpar", [128, 52])
    cosk = din("cosk", [128, SEQ])
    sink = din("sink", [128, SEQ])
    cosq = din("cosq", [128, NSLOT * G])
    sinq = din("sinq", [128, NSLOT * G])
    negm = din("negm", [NSLOT * G, 128])
    valm = din("valm", [NSLOT * G, 128])
    ownm = din("ownm", [NSLOT * G, 128])
    sel16_in = din("sel16", [16, 16 * 128])
    cmask = din("cmask", [2, 128, 8 * G], BF16)
    blkind = din("blkind", [16, SEQ], BF16)
    invc = din("invc", [NSLOT, 128, 4 * G])
    ident_in = din("ident", [128, 128])
    xs4 = din("xs4", [4, D])
    ck = din("ck", [2560 * 8, 8192])
    cv = din("cv", [2560 * 8, 8192])
    pt_in = din("pt", [128, 2], I32)
    st4 = din("st4", [4, 15 * 512])
    cs8 = din("cs8", [4, 512])
    sn8 = din("sn8", [4, 512])
    selT_in = din("selT", [4, 256])
    pairsel_in = din("pairsel", [128, 8])

    y_out = dout("y_out", [NSLOT * G, D])
    k_out = dout("k_out", [SEQ, 512])
    v_out = dout("v_out", [SEQ, 512])
    pool_out = dout("pool_out", [16, 512])
    ys_out = dout("ys_out", [4, D])
    ks_out = dout("ks_out", [4, 512])
    vs_out = dout("vs_out", [4, 512])
    ps_out = dout("ps_out", [4, 15 * 512])

    s_win = dscr("s_win", [D, 4096])
    s_wrot = dscr("s_wrot", [D, 1024])
    s_wpool = dscr("s_wpool", [512, 128])
    s_wa = dscr("s_wa", [512, D])
    s_wb = dscr("s_wb", [512, D])
    s_wout = dscr("s_wout", [D, D])
    s_eg = dscr("s_eg", [NE * D, DE])
    s_eu = dscr("s_eu", [NE * D, DE])
    s_ed = dscr("s_ed", [NE * DE, D])
    s_kT = dscr("s_kT", [NH, HD, SEQ])
    s_v = dscr("s_v", [NH, 128, 32, 66])

    convs = {}

    def convert(dst, src, rows, cols, name):
        conv = Buf()
        convs[name] = conv
        tot = rows * cols
        L = 2048 if tot % 2048 == 0 else cols
        R = tot // L
        if cols != L:
            if cols > L:
                s2 = src.rearrange("r (a b) -> (r a) b", b=L)
                d2 = dst.rearrange("r (a b) -> (r a) b", b=L)
            else:
                s2 = src.rearrange("(r a) b -> r (a b)", a=L // cols)
                d2 = dst.rearrange("(r a) b -> r (a b)", a=L // cols)
        else:
            s2, d2 = src, dst
        step = 512
        for r0 in range(0, R, step):
            r1 = min(R, r0 + step)
            cx.dma("pool", d2[r0:r1, :], s2[r0:r1, :], writes=[conv], stream="conv")

    convert(s_win, w_in, D, 4096, "s_win")
    convert(s_wrot, w_rot, D, 1024, "s_wrot")
    convert(s_wpool, w_pool, 512, 128, "s_wpool")
    convert(s_wa, w_a, 512, D, "s_wa")
    convert(s_wb, w_b, 512, D, "s_wb")
    convert(s_wout, w_out, D, D, "s_wout")
    convert(s_eg, w_eg, NE * D, DE, "s_eg")
    convert(s_eu, w_eu, NE * D, DE, "s_eu")
    convert(s_ed, w_ed, NE * DE, D, "s_ed")

    if STOP == 0:
        cx.drain("sp")
        return
    ident = sb("ident", [128, 128])
    identb = sb("identb", [128, 128], BF16)
    ones_ln = sb("ones_ln", [128, 128])
    fp = sb("fp", [128, 52])
    wr_sb = sb("wr_sb", [128, 8 * 20])
    br_sb = sb("br_sb", [128, 20])
    cB = Buf()
    cx.dma("sp", ident[:], ident_in[:, :], writes=[cB])
    cx.dma("sp", fp[:], fpar[:, :], writes=[cB])
    cx.dma("sp", wr_sb[:], w_r[:, :], writes=[cB])
    cx.dma("sp", br_sb[:], b_r[:, :], writes=[cB])
    _emit(cx, "dve", [cB], [cB], lambda: nc.vector.tensor_copy(out=identb[:], in_=ident[:]))
    _emit(cx, "dve", [], [cB], lambda: nc.vector.memset(ones_ln[:], 1.0 / D))

    ps = [st.enter_context(nc.psum_tensor(f"ps{i}", [128, 512], F32)) for i in range(8)]
    pb = [Buf() for _ in range(8)]

    def mm_group(out_ap, pairs, reads, wbuf):
        cx.deps("pe", reads, [wbuf])
        n = len(pairs)
        ins = None
        for i, (l, r) in enumerate(pairs):
            ins = nc.tensor.matmul(out_ap, l, r, start=(i == 0), stop=(i == n - 1))
        return cx.op("pe", ins, reads, [wbuf])

    def transpose(out_ap, in_ap, idt, reads, wbuf):
        cx.deps("pe", reads, [wbuf])
        ins = nc.tensor.transpose(out_ap, in_ap, idt)
        return cx.op("pe", ins, reads, [wbuf])

    def load_xT(x_dram, row0, ntok, xT_f, xT_b, xf_buf, xb_buf, xt_tiles, xt_bufs, psA, psB):
        nt = ntok // 128
        for t in range(nt):
            xt = xt_tiles[t % 2]
            xtb = xt_bufs[t % 2]
            cx.dma("sp", xt[:], x_dram[row0 + t * 128: row0 + (t + 1) * 128, :], writes=[xtb], stream="x")
            for half in range(2):
                pi = psA if half == 0 else psB
                for j in range(4):
                    kc = half * 4 + j
                    transpose(ps[pi][:, j * 128:(j + 1) * 128], xt[:, kc * 128:(kc + 1) * 128], ident[:],
                              [xtb, cB], pb[pi])
                src = ps[pi][:, :].rearrange("p (j t) -> p j t", j=4)
                if xT_f is not None:
                    dstf = xT_f[:, half * 4:(half + 1) * 4, t * 128:(t + 1) * 128]
                    _emit(cx, "act", [pb[pi]], [xf_buf], lambda: nc.scalar.copy(out=dstf, in_=src))
                dstb = xT_b[:, half * 4:(half + 1) * 4, t * 128:(t + 1) * 128]
                _emit(cx, "dve", [pb[pi]], [xb_buf], lambda: nc.vector.tensor_copy(out=dstb, in_=src))

    def barrier():
        for e in cx.eng:
            for e2 in cx.eng:
                if e2 != e and cx.cnt[e2] > 0:
                    cx._wait(e, (cx.sem[e2], cx.cnt[e2]))
            cx.drain(e)

    def Vv(reads, writes, fn):
        return _emit(cx, "dve", reads, writes, fn)

    def Aa(reads, writes, fn):
        return _emit(cx, "act", reads, writes, fn)

    def Pp(reads, writes, fn):
        return _emit(cx, "pool", reads, writes, fn)

    def load_wblk(tile, tb, src, c0, ncols=512, nk=8):
        cx.dma("sp", tile[:, 0:nk, 0:ncols], src[:, c0:c0 + ncols].rearrange("(kc p) j -> p kc j", p=128),
               reads=[convs[src.tensor.name]], writes=[tb], stream="w")

    def load_xT(x_dram, row0, ntok, xT_f, xT_b, xf_buf, xb_buf, xt_tiles, xt_bufs, psA, psB):
        nt = ntok // 128
        for t in range(nt):
            xt = xt_tiles[t % 2]
            xtb = xt_bufs[t % 2]
            cx.dma("sp", xt[:], x_dram[row0 + t * 128: row0 + (t + 1) * 128, :], writes=[xtb], stream="x")
            for half in range(2):
                pi = psA if half == 0 else psB
                for j in range(4):
                    kc = half * 4 + j
                    transpose(ps[pi][:, j * 128:(j + 1) * 128], xt[:, kc * 128:(kc + 1) * 128], ident[:],
                              [xtb, cB], pb[pi])
                src = ps[pi][:, :].rearrange("p (j t) -> p j t", j=4)
                dstb = xT_b[:, half * 4:(half + 1) * 4, t * 128:(t + 1) * 128]
                if xT_f is not None:
                    dstf = xT_f[:, half * 4:(half + 1) * 4, t * 128:(t + 1) * 128]
                    Aa([pb[pi]], [xf_buf], lambda: nc.scalar.copy(out=dstf, in_=src))
                    Vv([xf_buf], [xb_buf], lambda: nc.vector.tensor_copy(out=dstb, in_=dstf))
                else:
                    Vv([pb[pi]], [xb_buf], lambda: nc.vector.tensor_copy(out=dstb, in_=src))

    xt_tiles = [sb(f"xt{i}", [128, D]) for i in range(2)]
    xt_bufs = [Buf(), Buf()]
    xT_b = sb("xT_b", [128, 8, G], BF16)
    xb_buf = Buf()
    kmT = sb("kmT", [128, 4, 16])
    kmB = Buf()
    kmbd = sb("kmbd", [128, 4, 32])
    ones1 = sb("ones1", [1, 64])
    sel16 = sb("sel16", [16, 16 * 128])
    cx.dma("sp", sel16[:], sel16_in[:, :], writes=[cB])
    Vv([], [cB], lambda: nc.vector.memset(ones1[:], 1.0))

    xsT_f = sb("xsT_f", [128, 8, 4])
    xsfb = Buf()
    xsT_b = sb("xsT_b", [128, 8, 4], BF16)
    xsbb = Buf()
    attnT_s = sb("attnT_s", [64, NH, 4], BF16)
    atsb = Buf()
    pooledT_s = sb("pooledT_s", [128, 4, 4], BF16)
    plsb = Buf()
    with ExitStack() as sS:
        def sbs(name, shape, dt=F32):
            return sS.enter_context(nc.sbuf_tensor("s_" + name, list(shape), dt))
        xs_t = sbs("xs_t", [4, D])
        xsb = Buf()
        wS = [sbs(f"wS{i}", [128, 8, 512], BF16) for i in range(2)]
        wSb = [Buf(), Buf()]
        tok = [sbs(f"tok{i}", [4, 512]) for i in range(6)]
        tokb = [Buf() for _ in range(6)]
        cst = sbs("cst", [4, 2, 512])
        cstb = Buf()
        tmp4 = sbs("tmp4", [4, 512])
        tmp4b = Buf()
        st_t = sbs("st_t", [4, 15, 512])
        stb_ = Buf()
        ssum = sbs("ssum", [4, 512])
        ssb = Buf()
        diff4 = sbs("diff4", [4, 512])
        d4b = Buf()
        diffT_s = sbs("diffT_s", [128, 4, 4], BF16)
        dTsb = Buf()
        wpool_s = sbs("wpool_s", [128, 4, 128], BF16)
        wpsb = Buf()
        pt_sb = sbs("pt_sb", [128, 2], I32)
        idx8 = sbs("idx8", [128, 2, 8], I32)
        ptb = Buf()
        selT = sbs("selT", [4, 256])
        pairsel = sbs("pairsel", [128, 8])
        scb = Buf()
        q_bc = sbs("q_bc", [128, 512])
        qbb = Buf()
        KV = [sbs(f"KV{i}", [128, 8192]) for i in range(2)]
        KVb = [Buf(), Buf()]
        s_all = sbs("s_all", [128, 128, NH])
        sab = Buf()
        Pm = sbs("Pm", [128, 128, NH])
        Pmb = Buf()
        gpage = sbs("gpage", [128, NH])
        gpb = Buf()
        gpT = sbs("gpT", [NH, 128])
        gpTb = Buf()
        gblk = sbs("gblk", [NH, 64])
        gbb = Buf()
        top8s = sbs("top8s", [NH, 8])
        selb_ = sbs("selb", [NH, 64])
        selp = sbs("selp", [NH, 128])
        spb = Buf()
        maskp = sbs("maskp", [128, NH])
        mpb = Buf()
        den = sbs("den", [128, NH])
        denb = Buf()
        Oacc = sbs("Oacc", [128, 512])
        Oab = Buf()
        red = sbs("red", [128, 512])
        redb = Buf()
        snew = sbs("snew", [4, 3, NH])
        snb = Buf()
        Osum = sbs("Osum", [4, 512])
        Osb = Buf()
        attn_tok = sbs("attn_tok", [4, 512])
        atkb = Buf()

        cx.dma("sp", xs_t[:], xs4[:, :], writes=[xsb], stream="c")
        cx.dma("sp", cst[:, 0, :], cs8[:, :], writes=[cstb], stream="c")
        cx.dma("sp", cst[:, 1, :], sn8[:, :], writes=[cstb], stream="c")
        cx.dma("sp", st_t[:], st4[:, :].rearrange("p (r c) -> p r c", r=15), writes=[stb_], stream="c")
        cx.dma("sp", pt_sb[:], pt_in[:, :], writes=[ptb], stream="c")
        cx.dma("sp", selT[:], selT_in[:, :], writes=[scb], stream="c")
        cx.dma("sp", pairsel[:], pairsel_in[:, :], writes=[scb], stream="c")
        cx.dma("sp", wpool_s[:], s_wpool[:, :].rearrange("(g c) e -> c g e", g=4), reads=[convs["s_wpool"]], writes=[wpsb],
               stream="w")
        for kc in range(8):
            transpose(ps[0][:, kc * 4:(kc + 1) * 4], xs_t[0:4, kc * 128:(kc + 1) * 128], ident[0:4, 0:4],
                      [xsb, cB], pb[0])
        Aa([pb[0]], [xsfb], lambda: nc.scalar.copy(out=xsT_f[:, :, :],
                                                  in_=ps[0][:, 0:32].rearrange("p (k t) -> p k t", k=8)))
        Vv([xsfb], [xsbb], lambda: nc.vector.tensor_copy(out=xsT_b[:, :, :], in_=xsT_f[:, :, :]))
        for i, (srcw, c0) in enumerate(((s_win, 0), (s_wrot, 0), (s_win, 512), (s_wrot, 512), (s_win, 1024),
                                        (s_win, 1536))):
            load_wblk(wS[i % 2], wSb[i % 2], srcw, c0)
            pi = 2 + i % 2
            mm_group(ps[pi][0:4, :], [(xsT_b[:, kc, :], wS[i % 2][:, kc, :]) for kc in range(8)],
                     [wSb[i % 2], xsbb], pb[pi])
            Aa([pb[pi]], [tokb[i]], lambda: nc.scalar.copy(out=tok[i][:], in_=ps[pi][0:4, :]))
        for (a_, r_) in ((0, 1), (2, 3)):
            Vv([tokb[a_], cstb], [tokb[a_]], lambda: nc.vector.tensor_tensor(out=tok[a_][:], in0=tok[a_][:],
                                                                            in1=cst[:, 0, :], op=ALU.mult))
            Vv([tokb[r_], cstb], [tokb[r_]], lambda: nc.vector.tensor_tensor(out=tok[r_][:], in0=tok[r_][:],
                                                                            in1=cst[:, 1, :], op=ALU.mult))
            Vv([tokb[r_]], [tokb[a_]], lambda: nc.vector.tensor_tensor(out=tok[a_][:], in0=tok[a_][:], in1=tok[r_][:],
                                                                      op=ALU.add))
        q_tok, k_tok, v_tok, u_tok = tok[0], tok[2], tok[4], tok[5]
        qtb, ktb, vtb, utb = tokb[0], tokb[2], tokb[4], tokb[5]
        cx.dma("sp", ks_out[:, :], k_tok[:], reads=[ktb], stream="so")
        cx.dma("sp", vs_out[:, :], v_tok[:], reads=[vtb], stream="so")
        cx.dma("sp", ps_out[:, 14 * 512:15 * 512], u_tok[:], reads=[utb], stream="so")
        cx.dma("sp", ps_out[:, 0:14 * 512], st4[:, 512:15 * 512], stream="so")
        for g4, w_ in enumerate((2, 4, 8, 16)):
            c0 = g4 * 128
            Vv([stb_], [ssb], lambda: nc.vector.tensor_reduce(
                out=ssum[:, c0:c0 + 128], in_=st_t[:, 15 - (w_ - 1):15, c0:c0 + 128].rearrange("p r c -> p c r"),
                axis=AX.X, op=ALU.add))
            Vv([ssb, utb], [tmp4b], lambda: nc.vector.tensor_tensor(out=tmp4[:, c0:c0 + 128], in0=ssum[:, c0:c0 + 128],
                                                                   in1=u_tok[:, c0:c0 + 128], op=ALU.add))
            Vv([tmp4b, utb], [d4b], lambda: nc.vector.scalar_tensor_tensor(
                out=diff4[:, c0:c0 + 128], in0=tmp4[:, c0:c0 + 128], scalar=1.0 / w_, in1=u_tok[:, c0:c0 + 128],
                op0=ALU.mult, op1=ALU.subtract))
        for g4 in range(4):
            transpose(ps[1][:, g4 * 4:(g4 + 1) * 4], diff4[0:4, g4 * 128:(g4 + 1) * 128], ident[0:4, 0:4],
                      [d4b, cB], pb[1])
        Vv([pb[1]], [dTsb], lambda: nc.vector.tensor_copy(out=diffT_s[:, :, :],
                                                         in_=ps[1][:, 0:16].rearrange("p (g t) -> p g t", g=4)))
        for g4 in range(4):
            mm_group(ps[2][:, 0:4], [(wpool_s[:, g4, :], diffT_s[:, g4, :])], [wpsb, dTsb], pb[2])
            Vv([pb[2], cB], [plsb], lambda: nc.vector.tensor_scalar(out=pooledT_s[:, g4, :], in0=ps[2][:, 0:4],
                                                                   scalar1=fp[:, 48 + g4:49 + g4], scalar2=None,
                                                                   op0=ALU.mult))
        for c in range(8):
            Vv([ptb], [ptb], lambda: nc.vector.tensor_scalar(out=idx8[:, :, c], in0=pt_sb[:, :], scalar1=8.0,
                                                            scalar2=float(c), op0=ALU.mult, op1=ALU.add))
        nbuf = 0
        for t in range(2):
            mm_group(ps[3][:, :], [(selT[0:4, t * 128:(t + 1) * 128], q_tok[0:4, :])], [scb, qtb], pb[3])
            Aa([pb[3]], [qbb], lambda: nc.scalar.copy(out=q_bc[:], in_=ps[3][:, :]))
            for c in range(8):
                kb_ = nbuf % 2
                nbuf += 1
                cx.deps("pool", [ptb], [KVb[kb_]])
                ins = nc.gpsimd.indirect_dma_start(out=KV[kb_][:, :], out_offset=None, in_=ck[:, :],
                                                   in_offset=bass.IndirectOffsetOnAxis(ap=idx8[:, t, c:c + 1], axis=0))
                cx.dma_done("pool", ins, [ptb], [KVb[kb_]], "g")
                (Pp if c % 2 == 0 else Vv)([qbb], [KVb[kb_]], lambda: (nc.gpsimd if c % 2 == 0 else nc.vector).tensor_tensor(
                    out=KV[kb_][:, :].rearrange("p (r e) -> p r e", r=16),
                    in0=KV[kb_][:, :].rearrange("p (r e) -> p r e", r=16),
                    in1=q_bc[:, :].rearrange("p (o e) -> p o e", o=1).broadcast_to([128, 16, 512]), op=ALU.mult))
                Vv([KVb[kb_]], [sab], lambda: nc.vector.tensor_reduce(
                    out=s_all[:, c * 16:(c + 1) * 16, :].rearrange("p r h -> p (r h)"),
                    in_=KV[kb_][:, :].rearrange("p (a d) -> p a d", d=HD), axis=AX.X, op=ALU.add))
            Vv([sab], [gpb], lambda: nc.vector.tensor_reduce(out=gpage[:, :], in_=s_all[:, :, :].rearrange("p r h -> p h r"),
                                                            axis=AX.X, op=ALU.add))
            transpose(ps[4][0:NH, 0:128], gpage[:, :], ident[:], [gpb, cB], pb[4])
            Aa([pb[4]], [gpTb], lambda: nc.scalar.copy(out=gpT[:, :], in_=ps[4][0:NH, 0:128]))
            gv = gpT[:, :].rearrange("h (n two) -> h n two", two=2)
            Vv([gpTb], [gbb], lambda: nc.vector.tensor_tensor(out=gblk[:, :], in0=gv[:, :, 0], in1=gv[:, :, 1], op=ALU.add))
            for s2 in range(2):
                Vv([gbb], [gbb], lambda: nc.vector.max(out=top8s[:, :], in_=gblk[:, s2 * 32:(s2 + 1) * 32]))
                Vv([gbb], [gbb], lambda: nc.vector.tensor_scalar(out=selb_[:, s2 * 32:(s2 + 1) * 32],
                                                                in0=gblk[:, s2 * 32:(s2 + 1) * 32],
                                                                scalar1=top8s[:, 2:3], scalar2=None, op0=ALU.is_ge))
            sv = selp[:, :].rearrange("h (n two) -> h n two", two=2)
            Vv([gbb], [spb], lambda: nc.vector.tensor_copy(out=sv[:, :, 0], in_=selb_[:, :]))
            Vv([gbb], [spb], lambda: nc.vector.tensor_copy(out=sv[:, :, 1], in_=selb_[:, :]))
            transpose(ps[4][:, 128:128 + NH], selp[:, :], ident[0:NH, 0:NH], [spb, cB], pb[4])
            Aa([pb[4]], [mpb], lambda: nc.scalar.copy(out=maskp[:, :], in_=ps[4][:, 128:128 + NH]))
            Aa([sab], [Pmb], lambda: nc.scalar.activation(out=Pm[:, :, :], in_=s_all[:, :, :], func=AF.Exp, scale=0.125))
            Vv([mpb], [Pmb], lambda: nc.vector.tensor_tensor(
                out=Pm[:, :, :], in0=Pm[:, :, :],
                in1=maskp[:, :].rearrange("p (o h) -> p o h", o=1).broadcast_to([128, 128, NH]), op=ALU.mult))
            Vv([Pmb], [denb], lambda: nc.vector.tensor_reduce(out=den[:, :], in_=Pm[:, :, :].rearrange("p r h -> p h r"),
                                                             axis=AX.X, op=ALU.add))
            Vv([], [Oab], lambda: nc.vector.memset(Oacc[:], 0.0))
            for c in range(8):
                kb_ = nbuf % 2
                nbuf += 1
                cx.deps("pool", [ptb], [KVb[kb_]])
                ins = nc.gpsimd.indirect_dma_start(out=KV[kb_][:, :], out_offset=None, in_=cv[:, :],
                                                   in_offset=bass.IndirectOffsetOnAxis(ap=idx8[:, t, c:c + 1], axis=0))
                cx.dma_done("pool", ins, [ptb], [KVb[kb_]], "g")
                (Pp if c % 2 == 0 else Vv)([Pmb], [KVb[kb_]], lambda: (nc.gpsimd if c % 2 == 0 else nc.vector).tensor_tensor(
                    out=KV[kb_][:, :].rearrange("p (r h d) -> p r h d", r=16, h=NH),
                    in0=KV[kb_][:, :].rearrange("p (r h d) -> p r h d", r=16, h=NH),
                    in1=Pm[:, c * 16:(c + 1) * 16, :].rearrange("p r (h o) -> p r h o", o=1).broadcast_to([128, 16, NH, HD]),
                    op=ALU.mult))
                Vv([KVb[kb_]], [redb], lambda: nc.vector.tensor_reduce(
                    out=red[:, :], in_=KV[kb_][:, :].rearrange("p (r e) -> p e r", r=16), axis=AX.X, op=ALU.add))
                Vv([redb], [Oab], lambda: nc.vector.tensor_tensor(out=Oacc[:], in0=Oacc[:], in1=red[:], op=ALU.add))
            cx.deps("pe", [scb, Oab], [pb[5]] if t == 0 else [])
            ins = nc.tensor.matmul(ps[5][0:4, :], pairsel[:, t * 4:(t + 1) * 4], Oacc[:, :], start=(t == 0), stop=(t == 1))
            cx.op("pe", ins, [scb, Oab], [pb[5]] if t == 1 else [])
            cx.deps("pe", [scb, denb], [pb[6]] if t == 0 else [])
            ins = nc.tensor.matmul(ps[6][0:4, 0:NH], pairsel[:, t * 4:(t + 1) * 4], den[:, :], start=(t == 0), stop=(t == 1))
            cx.op("pe", ins, [scb, denb], [pb[6]] if t == 1 else [])
        Vv([qtb, ktb], [tmp4b], lambda: nc.vector.tensor_tensor(out=tmp4[:], in0=q_tok[:], in1=k_tok[:], op=ALU.mult))
        Vv([tmp4b], [snb], lambda: nc.vector.tensor_reduce(out=snew[:, 0, :], in_=tmp4[:, :].rearrange("p (h d) -> p h d", h=NH),
                                                          axis=AX.X, op=ALU.add))
        Aa([snb], [snb], lambda: nc.scalar.activation(out=snew[:, 1, :], in_=snew[:, 0, :], func=AF.Exp, scale=0.125))
        Vv([snb, vtb], [tmp4b], lambda: nc.vector.tensor_tensor(
            out=tmp4[:, :].rearrange("p (h d) -> p h d", h=NH), in0=v_tok[:, :].rearrange("p (h d) -> p h d", h=NH),
            in1=snew[:, 1, :].rearrange("p (h o) -> p h o", o=1).broadcast_to([4, NH, HD]), op=ALU.mult))
        Vv([pb[5], tmp4b], [Osb], lambda: nc.vector.tensor_tensor(out=Osum[:], in0=ps[5][0:4, :], in1=tmp4[:], op=ALU.add))
        Vv([pb[6], snb], [snb], lambda: nc.vector.tensor_tensor(out=snew[:, 2, :], in0=ps[6][0:4, 0:NH], in1=snew[:, 1, :],
                                                               op=ALU.add))
        Vv([snb], [snb], lambda: nc.vector.reciprocal(out=snew[:, 2, :], in_=snew[:, 2, :]))
        Vv([Osb, snb], [atkb], lambda: nc.vector.tensor_tensor(
            out=attn_tok[:, :].rearrange("p (h d) -> p h d", h=NH), in0=Osum[:, :].rearrange("p (h d) -> p h d", h=NH),
            in1=snew[:, 2, :].rearrange("p (h o) -> p h o", o=1).broadcast_to([4, NH, HD]), op=ALU.mult))
        for h in range(NH):
            transpose(ps[7][0:64, h * 4:(h + 1) * 4], attn_tok[0:4, h * 64:(h + 1) * 64], ident[0:4, 0:4],
                      [atkb, cB], pb[7])
        Aa([pb[7]], [atsb], lambda: nc.scalar.copy(out=attnT_s[:, :, :],
                                                  in_=ps[7][0:64, 0:32].rearrange("p (h t) -> p h t", h=NH)))
        barrier()
    if STOP == 2:
        cx.drain("sp")
        return

    with ExitStack() as s1:
        def sb1(name, shape, dt=F32):
            return s1.enter_context(nc.sbuf_tensor("p1_" + name, list(shape), dt))
        wk = sb1("wk", [128, 8, 512], BF16)
        wkr = sb1("wkr", [128, 8, 512], BF16)
        wv = sb1("wv", [128, 8, 512], BF16)
        wB = Buf()
        for (t_, c0, srcw) in ((wk, 512, s_win), (wkr, 512, s_wrot), (wv, 1024, s_win)):
            cx.dma("sp", t_[:], srcw[:, c0:c0 + 512].rearrange("(kc p) j -> p kc j", p=128),
                   reads=[convs[srcw.tensor.name]], writes=[wB], stream="w")
        cs_t = sb1("cs_t", [128, G])
        sn_t = sb1("sn_t", [128, G])
        csB = Buf()
        kT_f = sb1("kT_f", [128, G])
        kT_fb = Buf()
        kT_h = sb1("kT_h", [128, G], BF16)
        kT_hb = Buf()
        t1 = sb1("t1", [128, G])
        t1b = Buf()
        ktok = sb1("ktok", [128, 512])
        ktokb = Buf()
        vtok = sb1("vtok", [128, 512])
        vtokb = Buf()
        vaug = sb1("vaug", [128, NH, 4, 66], BF16)
        vaugb = Buf()
        Pp([], [vaugb], lambda: nc.gpsimd.memset(vaug[:], 1.0))

        for g in range(NG):
            load_xT(xseq, g * G, G, None, xT_b, None, xb_buf, xt_tiles, xt_bufs, 0, 1)
            cx.dma("sp", cs_t[:], cosk[:, g * G:(g + 1) * G], writes=[csB], stream="c")
            cx.dma("sp", sn_t[:], sink[:, g * G:(g + 1) * G], writes=[csB], stream="c")
            for c in range(4):
                mm_group(ps[2][:, :], [(wk[:, kc, c * 128:(c + 1) * 128], xT_b[:, kc, :]) for kc in range(8)],
                         [wB, xb_buf], pb[2])
                mm_group(ps[3][:, :], [(wkr[:, kc, c * 128:(c + 1) * 128], xT_b[:, kc, :]) for kc in range(8)],
                         [wB, xb_buf], pb[3])
                Vv([pb[2], csB], [t1b],
                   lambda: nc.vector.tensor_tensor(out=t1[:], in0=ps[2][:, :], in1=cs_t[:], op=ALU.mult))
                Vv([pb[3], csB], [kT_fb],
                   lambda: nc.vector.tensor_tensor(out=kT_f[:], in0=ps[3][:, :], in1=sn_t[:], op=ALU.mult))
                Vv([t1b], [kT_fb],
                   lambda: nc.vector.tensor_tensor(out=kT_f[:], in0=kT_f[:], in1=t1[:], op=ALU.add))
                Aa([kT_fb], [kT_hb], lambda: nc.scalar.copy(out=kT_h[:], in_=kT_f[:]))
                cx.dma("sp", s_kT[2 * c:2 * c + 2, :, g * G:(g + 1) * G].rearrange("h d t -> (h d) t"), kT_h[:],
                       reads=[kT_hb], stream="ks")
                Vv([kT_fb], [kmB],
                   lambda: nc.vector.tensor_reduce(out=kmT[:, c, 2 * g:2 * g + 2],
                                                   in_=kT_f[:, :].rearrange("p (b t) -> p b t", b=2),
                                                   axis=AX.X, op=ALU.add))
                for t in range(4):
                    transpose(ps[4][:, t * 128:(t + 1) * 128], kT_f[:, t * 128:(t + 1) * 128], ident[:],
                              [kT_fb, cB], pb[4])
                Aa([pb[4]], [ktokb], lambda: nc.scalar.copy(out=ktok[:, :], in_=ps[4][:, :]))
                cx.dma("sp", k_out[g * G:(g + 1) * G, c * 128:(c + 1) * 128].rearrange("(t p) j -> p t j", p=128),
                       ktok[:, :].rearrange("p (t j) -> p t j", t=4), reads=[ktokb], stream="ko")
            for t in range(4):
                mm_group(ps[5][:, :], [(xT_b[:, kc, t * 128:(t + 1) * 128], wv[:, kc, :]) for kc in range(8)],
                         [wB, xb_buf], pb[5])
                Aa([pb[5]], [vtokb], lambda: nc.scalar.copy(out=vtok[:], in_=ps[5][:, :]))
                Vv([vtokb], [vaugb],
                   lambda: nc.vector.tensor_copy(out=vaug[:, :, t, 0:64],
                                                 in_=vtok[:, :].rearrange("p (h d) -> p h d", h=NH)))
                cx.dma("sp", v_out[g * G + t * 128: g * G + (t + 1) * 128, :], vtok[:], reads=[vtokb], stream="vo")
            for h in range(NH):
                cx.dma("sp", s_v[h, :, g * 4:(g + 1) * 4, :], vaug[:, h, :, :], reads=[vaugb], stream="vs")
        Vv([], [kmB], lambda: nc.vector.memset(kmbd[:], 0.0))
        Vv([kmB], [kmB], lambda: nc.vector.tensor_scalar(out=kmbd[0:64, :, 0:16], in0=kmT[0:64, :, :],
                                                         scalar1=1.0 / 256, scalar2=None, op0=ALU.mult))
        Vv([kmB], [kmB], lambda: nc.vector.tensor_scalar(out=kmbd[64:128, :, 16:32], in0=kmT[64:128, :, :],
                                                         scalar1=1.0 / 256, scalar2=None, op0=ALU.mult))
        barrier()
    if STOP == 1:
        cx.drain("sp")
        return

    def layer_norm(zT, zb, gcol, bcol, outf, outfb, outb, outbb, tmp, tmpb, mean_sb, rstd_sb, stb, T):
        for oc in range(8):
            Aa([zb], [tmpb], lambda: nc.scalar.activation(out=tmp[:, 0:T], in_=zT[:, oc, 0:T], func=AF.Square))
            cx.deps("pe", [zb, cB], [pb[6]] if oc == 0 else [])
            ins = nc.tensor.matmul(ps[6][:, 0:T], ones_ln[:], zT[:, oc, 0:T], start=(oc == 0), stop=(oc == 7))
            cx.op("pe", ins, [zb, cB], [pb[6]] if oc == 7 else [])
            cx.deps("pe", [tmpb], [pb[7]] if oc == 0 else [])
            ins = nc.tensor.matmul(ps[7][:, 0:T], ones_ln[:], tmp[:, 0:T], start=(oc == 0), stop=(oc == 7))
            cx.op("pe", ins, [tmpb], [pb[7]] if oc == 7 else [])
        Aa([pb[6]], [stb], lambda: nc.scalar.copy(out=mean_sb[:, 0:T], in_=ps[6][:, 0:T]))
        Vv([stb], [tmpb], lambda: nc.vector.tensor_tensor(out=tmp[:, 0:T], in0=mean_sb[:, 0:T], in1=mean_sb[:, 0:T],
                                                         op=ALU.mult))
        Vv([pb[7], tmpb], [tmpb], lambda: nc.vector.tensor_tensor(out=tmp[:, 0:T], in0=ps[7][:, 0:T], in1=tmp[:, 0:T],
                                                                 op=ALU.subtract))
        Aa([tmpb], [tmpb], lambda: nc.scalar.activation(out=tmp[:, 0:T], in_=tmp[:, 0:T], func=AF.Sqrt, bias=eps_t[:, 0:1]))
        Vv([tmpb], [stb], lambda: nc.vector.reciprocal(out=rstd_sb[:, 0:T], in_=tmp[:, 0:T]))
        for oc in range(8):
            Vv([zb, stb], [tmpb], lambda: nc.vector.tensor_tensor(out=tmp[:, 0:T], in0=zT[:, oc, 0:T], in1=mean_sb[:, 0:T],
                                                                 op=ALU.subtract))
            Vv([stb], [tmpb], lambda: nc.vector.tensor_tensor(out=tmp[:, 0:T], in0=tmp[:, 0:T], in1=rstd_sb[:, 0:T],
                                                             op=ALU.mult))
            Vv([tmpb, cB], [outfb], lambda: nc.vector.tensor_scalar(out=outf[:, oc, 0:T], in0=tmp[:, 0:T],
                                                                   scalar1=fp[:, gcol + oc:gcol + oc + 1],
                                                                   scalar2=fp[:, bcol + oc:bcol + oc + 1],
                                                                   op0=ALU.mult, op1=ALU.add))
            if outb is not None:
                Aa([outfb], [outbb], lambda: nc.scalar.copy(out=outb[:, oc, 0:T], in_=outf[:, oc, 0:T]))

    eps_t = sb("eps_t", [128, 1])
    Vv([], [cB], lambda: nc.vector.memset(eps_t[:], LN_EPS))
    x1T_f = sb("x1T_f", [128, 8, G])
    x1fb = Buf()
    x1T_b = sb("x1T_b", [128, 8, G], BF16)
    x1bb = Buf()
    tmp = sb("ln_tmp", [128, G])
    tmpb = Buf()
    mean_sb = sb("mean_sb", [128, G])
    rstd_sb = sb("rstd_sb", [128, G])
    stb = Buf()

    def moe_and_out(sbb, row0, T, y_dst):
        nt = min(128, T)
        ntile = T // nt
        Lg = sbb("Lg", [128, 20])
        rs = sbb("rs", [128, 64])
        esel = sbb("esel", [128, 8])
        top8e = sbb("top8e", [128, 8])
        comb = sbb("comb", [128, 16])
        rb = Buf()
        combT = sbb("combT", [16, G])
        cTb = Buf()
        bce = sbb("bce", [128, G])
        bceb = Buf()
        wg = [sbb(f"wg{i}", [128, 8, DE], BF16) for i in range(3)]
        wu = [sbb(f"wu{i}", [128, 8, DE], BF16) for i in range(3)]
        wgb = [Buf(), Buf(), Buf()]
        wub = [Buf(), Buf(), Buf()]
        silt = [sbb(f"silt{i}", [128, G]) for i in range(2)]
        siltb = [Buf(), Buf()]
        hT = sbb("hT", [128, 32, G], BF16)
        hTb = Buf()
        wdblk2 = [sbb(f"wdblk{i}", [128, 32, 512], BF16) for i in range(2)]
        wdb2 = [Buf(), Buf()]
        for half in range(2):
            cx.dma("sp", wdblk2[half][:], s_ed[:, half * 512:(half + 1) * 512].rearrange("(j p) o -> p j o", p=128),
                   reads=[convs["s_ed"]], writes=[wdb2[half]], stream="wd")
        otile = sbb("otile", [128, D])
        otb = Buf()

        Vv([], [rb], lambda: nc.vector.memset(esel[:], NEG))
        for t in range(ntile):
            ts_ = slice(t * nt, (t + 1) * nt)
            P_ = slice(0, nt)
            cx.deps("pe", [x1fb, cB], [pb[0]])
            ins = None
            for kc in range(8):
                ins = nc.tensor.matmul(ps[0][P_, 0:20], x1T_f[:, kc, ts_], wr_sb[:, kc * 20:(kc + 1) * 20],
                                       start=(kc == 0), stop=(kc == 7))
            cx.op("pe", ins, [x1fb, cB], [pb[0]])
            Vv([pb[0], cB], [rb], lambda: nc.vector.tensor_tensor(out=Lg[P_, :], in0=ps[0][P_, 0:20], in1=br_sb[P_, :],
                                                                 op=ALU.add))
            Vv([rb], [rb], lambda: nc.vector.tensor_reduce(out=rs[P_, 0:1], in_=Lg[P_, 0:4], axis=AX.X, op=ALU.max))
            Vv([rb], [rb], lambda: nc.vector.tensor_scalar(out=rs[P_, 4:8], in0=Lg[P_, 0:4], scalar1=rs[P_, 0:1],
                                                          scalar2=None, op0=ALU.is_ge))
            Vv([rb], [rb], lambda: nc.vector.tensor_scalar(out=rs[P_, 1:2], in0=rs[P_, 0:1], scalar1=-1.0,
                                                          scalar2=None, op0=ALU.mult))
            Aa([rb], [rb], lambda: nc.scalar.activation(out=rs[P_, 8:12], in_=Lg[P_, 0:4], func=AF.Exp,
                                                       bias=rs[P_, 1:2], accum_out=rs[P_, 2:3]))
            Vv([rb], [rb], lambda: nc.vector.reciprocal(out=rs[P_, 3:4], in_=rs[P_, 2:3]))
            Vv([rb], [rb], lambda: nc.vector.tensor_scalar(out=esel[P_, 0:4], in0=Lg[P_, 4:8], scalar1=rs[P_, 4:5],
                                                          scalar2=None, op0=ALU.mult))
            for g4 in range(1, 4):
                Vv([rb], [rb], lambda: nc.vector.scalar_tensor_tensor(out=esel[P_, 0:4], in0=Lg[P_, 4 + 4 * g4:8 + 4 * g4],
                                                                     scalar=rs[P_, 4 + g4:5 + g4], in1=esel[P_, 0:4],
                                                                     op0=ALU.mult, op1=ALU.add))
            Vv([rb], [rb], lambda: nc.vector.max(out=top8e[P_, :], in_=esel[P_, :]))
            Vv([rb], [rb], lambda: nc.vector.tensor_tensor(out=rs[P_, 12:13], in0=top8e[P_, 0:1], in1=top8e[P_, 1:2],
                                                          op=ALU.subtract))
            Aa([rb], [rb], lambda: nc.scalar.activation(out=rs[P_, 13:14], in_=rs[P_, 12:13], func=AF.Sigmoid))
            Vv([rb], [rb], lambda: nc.vector.tensor_tensor(out=rs[P_, 14:15], in0=rs[P_, 13:14], in1=rs[P_, 3:4],
                                                          op=ALU.mult))
            Vv([rb], [rb], lambda: nc.vector.tensor_tensor(out=rs[P_, 15:16], in0=rs[P_, 3:4], in1=rs[P_, 14:15],
                                                          op=ALU.subtract))
            Vv([rb], [rb], lambda: nc.vector.tensor_tensor(out=rs[P_, 16:17], in0=rs[P_, 14:15], in1=rs[P_, 15:16],
                                                          op=ALU.subtract))
            Vv([rb], [rb], lambda: nc.vector.tensor_scalar(out=rs[P_, 20:24], in0=esel[P_, 0:4], scalar1=top8e[P_, 0:1],
                                                          scalar2=None, op0=ALU.is_ge))
            Vv([rb], [rb], lambda: nc.vector.tensor_scalar(out=rs[P_, 24:28], in0=esel[P_, 0:4], scalar1=top8e[P_, 1:2],
                                                          scalar2=None, op0=ALU.is_ge))
            Vv([rb], [rb], lambda: nc.vector.tensor_scalar(out=rs[P_, 28:32], in0=rs[P_, 24:28], scalar1=rs[P_, 15:16],
                                                          scalar2=None, op0=ALU.mult))
            Vv([rb], [rb], lambda: nc.vector.scalar_tensor_tensor(out=rs[P_, 28:32], in0=rs[P_, 20:24],
                                                                 scalar=rs[P_, 16:17], in1=rs[P_, 28:32],
                                                                 op0=ALU.mult, op1=ALU.add))
            for g4 in range(4):
                Vv([rb], [rb], lambda: nc.vector.tensor_scalar(out=comb[P_, 4 * g4:4 * g4 + 4], in0=rs[P_, 28:32],
                                                              scalar1=rs[P_, 4 + g4:5 + g4], scalar2=None,
                                                              op0=ALU.mult))
            transpose(ps[1][0:16, ts_], comb[P_, :], ident[P_, P_], [rb, cB], pb[1])
        Aa([pb[1]], [cTb], lambda: nc.scalar.copy(out=combT[:, 0:T], in_=ps[1][0:16, 0:T]))

        for e in range(NE):
            i2 = e % 3
            cx.dma("sp", wg[i2][:], s_eg[e * D:(e + 1) * D, :].rearrange("(kc p) f -> p kc f", p=128),
                   reads=[convs["s_eg"]], writes=[wgb[i2]], stream="we")
            cx.dma("sp", wu[i2][:], s_eu[e * D:(e + 1) * D, :].rearrange("(kc p) f -> p kc f", p=128),
                   reads=[convs["s_eu"]], writes=[wub[i2]], stream="we")
            mm_group(ps[2][:, 0:T], [(sel16[0:16, e * 128:(e + 1) * 128], combT[0:16, 0:T])], [cTb, cB], pb[2])
            Aa([pb[2]], [bceb], lambda: nc.scalar.copy(out=bce[:, 0:T], in_=ps[2][:, 0:T]))
            for fc in range(2):
                j = e * 2 + fc
                pg, pu, sj = 3 + j % 2, 5 + j % 2, j % 2
                fs = slice(fc * 128, (fc + 1) * 128)
                mm_group(ps[pg][:, 0:T], [(wg[i2][:, kc, fs], x1T_b[:, kc, 0:T]) for kc in range(8)],
                         [wgb[i2], x1bb], pb[pg])
                mm_group(ps[pu][:, 0:T], [(wu[i2][:, kc, fs], x1T_b[:, kc, 0:T]) for kc in range(8)],
                         [wub[i2], x1bb], pb[pu])
                Aa([pb[pg]], [siltb[sj]], lambda: nc.scalar.activation(out=silt[sj][:, 0:T], in_=ps[pg][:, 0:T],
                                                                      func=AF.Silu))
                Pp([bceb], [siltb[sj]], lambda: nc.gpsimd.tensor_tensor(out=silt[sj][:, 0:T], in0=silt[sj][:, 0:T],
                                                                       in1=bce[:, 0:T], op=ALU.mult))
                Vv([pb[pu], siltb[sj]], [hTb], lambda: nc.vector.tensor_tensor(out=hT[:, j, 0:T], in0=ps[pu][:, 0:T],
                                                                              in1=silt[sj][:, 0:T], op=ALU.mult))
        for half in range(2):
            wdblk, wdb = wdblk2[half], wdb2[half]
            for o4 in range(4):
                oc = half * 4 + o4
                pi = oc % 2
                mm_group(ps[pi][:, 0:T], [(wdblk[:, j, o4 * 128:(o4 + 1) * 128], hT[:, j, 0:T]) for j in range(32)],
                         [wdb, hTb], pb[pi])
                Vv([pb[pi], x1fb], [x1fb],
                   lambda: nc.vector.scalar_tensor_tensor(out=x1T_f[:, oc, 0:T], in0=x1T_f[:, oc, 0:T], scalar=ALPHA,
                                                          in1=ps[pi][:, 0:T], op0=ALU.mult, op1=ALU.add))
        layer_norm(x1T_f, x1fb, 16, 24, x1T_f, x1fb, None, None, tmp, tmpb, mean_sb, rstd_sb, stb, T)
        for t in range(ntile):
            ts_ = slice(t * nt, (t + 1) * nt)
            for half in range(2):
                pi = 3 + half
                for j in range(4):
                    oc = half * 4 + j
                    transpose(ps[pi][0:nt, j * 128:(j + 1) * 128], x1T_f[:, oc, ts_], ident[:], [x1fb, cB], pb[pi])
                if half == 0:
                    Aa([pb[pi]], [otb], lambda: nc.scalar.copy(out=otile[0:nt, 0:512], in_=ps[pi][0:nt, :]))
                else:
                    Vv([pb[pi]], [otb], lambda: nc.vector.tensor_copy(out=otile[0:nt, 512:1024], in_=ps[pi][0:nt, :]))
            cx.dma("sp", y_dst[row0 + t * nt: row0 + (t + 1) * nt, :], otile[0:nt, :], reads=[otb], stream="yo")

    def merge_ln1(sba, T, xT_f, xfb, xTb, xb_buf, attnT, atb, pooledT, plb):
        wblk = [sba(f"m_wblk{i}", [128, 8, 512], BF16) for i in range(2)]
        wblkb = [Buf(), Buf()]
        wa_t = sba("wa_t", [64, NH, 512], BF16)
        wab = Buf()
        wb_t = sba("wb_t", [128, 4, 512], BF16)
        wbb = Buf()
        sga = sba("m_sga", [128, T])
        sgab = Buf()
        sgb = sba("m_sgb", [128, T])
        sgbb = Buf()
        t1 = sba("m_t1", [128, T])
        t1b = Buf()
        t2 = sba("m_t2", [128, T])
        t2b = Buf()
        mergedT = sba("mergedT", [128, 8, T], BF16)
        mgb = Buf()
        for half in range(2):
            load_wblk(wblk[0], wblkb[0], s_win, 2048 + half * 512)
            load_wblk(wblk[1], wblkb[1], s_win, 3072 + half * 512)
            cx.dma("sp", wa_t[:], s_wa[:, half * 512:(half + 1) * 512].rearrange("(h d) o -> d h o", h=NH),
                   reads=[convs["s_wa"]], writes=[wab], stream="w")
            cx.dma("sp", wb_t[:], s_wb[:, half * 512:(half + 1) * 512].rearrange("(g c) o -> c g o", g=4),
                   reads=[convs["s_wb"]], writes=[wbb], stream="w")
            for o4 in range(4):
                oc = half * 4 + o4
                cs_ = slice(o4 * 128, (o4 + 1) * 128)
                mm_group(ps[2][:, 0:T], [(wblk[0][:, kc, cs_], xTb[:, kc, 0:T]) for kc in range(8)],
                         [wblkb[0], xb_buf], pb[2])
                mm_group(ps[3][:, 0:T], [(wblk[1][:, kc, cs_], xTb[:, kc, 0:T]) for kc in range(8)],
                         [wblkb[1], xb_buf], pb[3])
                Aa([pb[2], cB], [sgab], lambda: nc.scalar.activation(out=sga[:], in_=ps[2][:, 0:T], func=AF.Sigmoid,
                                                                    bias=fp[:, 32 + oc:33 + oc]))
                Aa([pb[3], cB], [sgbb], lambda: nc.scalar.activation(out=sgb[:], in_=ps[3][:, 0:T], func=AF.Sigmoid,
                                                                    bias=fp[:, 40 + oc:41 + oc]))
                mm_group(ps[0][:, 0:T], [(wa_t[0:64, h, cs_], attnT[0:64, h, 0:T]) for h in range(NH)],
                         [wab, atb], pb[0])
                mm_group(ps[1][:, 0:T], [(wb_t[:, g4, cs_], pooledT[:, g4, 0:T]) for g4 in range(4)],
                         [wbb, plb], pb[1])
                Vv([pb[0], sgab], [t1b], lambda: nc.vector.tensor_tensor(out=t1[:], in0=ps[0][:, 0:T], in1=sga[:],
                                                                        op=ALU.mult))
                Vv([pb[1], sgbb], [t2b], lambda: nc.vector.tensor_tensor(out=t2[:], in0=ps[1][:, 0:T], in1=sgb[:],
                                                                        op=ALU.mult))
                Vv([t1b, t2b], [mgb], lambda: nc.vector.tensor_tensor(out=mergedT[:, oc, :], in0=t1[:], in1=t2[:],
                                                                     op=ALU.add))
        for half in range(2):
            load_wblk(wblk[half], wblkb[half], s_wout, half * 512)
            for o4 in range(4):
                oc = half * 4 + o4
                pi = 2 + (oc % 2)
                mm_group(ps[pi][:, 0:T], [(wblk[half][:, kc, o4 * 128:(o4 + 1) * 128], mergedT[:, kc, :])
                                          for kc in range(8)], [wblkb[half], mgb], pb[pi])
                Vv([pb[pi], xfb], [xfb],
                   lambda: nc.vector.scalar_tensor_tensor(out=xT_f[:, oc, 0:T], in0=xT_f[:, oc, 0:T], scalar=ALPHA,
                                                          in1=ps[pi][:, 0:T], op0=ALU.mult, op1=ALU.add))
        layer_norm(xT_f, xfb, 0, 8, x1T_f, x1fb, x1T_b, x1bb, tmp, tmpb, mean_sb, rstd_sb, stb, T)

    for slot in range(NSLOT):
        cx.epoch()
        nkt = 8 * (slot + 1)
        with ExitStack() as sA:
            def sba(name, shape, dt=F32):
                return sA.enter_context(nc.sbuf_tensor(f"a{slot}_" + name, list(shape), dt))
            xT_f = sba("xT_f", [128, 8, G])
            xfb = Buf()
            attnT = sba("attnT", [64, NH, G], BF16)
            atb = Buf()
            pooledT = sba("pooledT", [128, 4, G], BF16)
            plb = Buf()
            sA1 = ExitStack()

            def sba1(name, shape, dt=F32):
                return sA1.enter_context(nc.sbuf_tensor(f"a1{slot}_" + name, list(shape), dt))
            wblk = [sba1(f"wblk{i}", [128, 8, 512], BF16) for i in range(2)]
            wblkb = [Buf(), Buf()]
            cq_t = sba1("cq_t", [128, G])
            sq_t = sba1("sq_t", [128, G])
            cqb = Buf()
            qT_f = sba1("qT_f", [128, 4, G])
            qfb = Buf()
            qaug = sba1("qaug", [80, NH, G], BF16)
            qab = Buf()
            t1 = sba1("t1", [128, G])
            t1b = Buf()
            kaug = [sba1(f"kaug{i}", [80, SEQ], BF16) for i in range(2)]
            kab = [Buf(), Buf()]
            vh = [sba1(f"vh{i}", [128, 32, 66], BF16) for i in range(2)]
            vhb = [Buf(), Buf()]
            pT = [sba1(f"pT{i}", [128, G], BF16) for i in range(4)]
            pTb = [Buf(), Buf(), Buf(), Buf()]
            cmk = sba1("cmk", [128, 8, G], BF16)
            cmkb = Buf()
            rden = sba1("rden", [65, G])
            rdb = Buf()
            rden0 = sba1("rden0", [1, G])
            rd0b = Buf()
            bcs = sba1("bcs", [64, G])
            bcsb = Buf()
            gm = sba1("gm", [128, 128])
            gmb = Buf()
            msk = sba1("msk", [128, 3, 128])
            mskb = Buf()
            top8 = sba1("top8", [128, NH, 8])
            top8b = Buf()
            sel = sba1("sel", [128, 128])
            selb = Buf()
            biasw = sba1("biasw", [128, NH, 32])
            bwb = Buf()
            uT = sba1("uT", [128, 4, 16 + G])
            uTb = Buf()
            xh = sba1("xh", [16, D])
            xhb = Buf()
            xhT = sba1("xhT", [128, 8, 16], BF16)
            xhTb = Buf()
            pa = sba1("pa", [128, 16 + G])
            pab = Buf()
            pbt = sba1("pbt", [128, 16 + G])
            pbb = Buf()
            invt = sba1("invt", [128, G])
            invb = Buf()
            diffT = sba1("diffT", [128, G], BF16)
            dfb = Buf()
            wpool_t = sba1("wpool_t", [128, 4, 128], BF16)
            wpb = Buf()
            sga = sba1("sga", [128, G])
            sgab = Buf()

            row0 = slot * G
            load_xT(xown, row0, G, xT_f, xT_b, xfb, xb_buf, xt_tiles, xt_bufs, 0, 1)
            cx.dma("sp", cq_t[:], cosq[:, row0:row0 + G], writes=[cqb], stream="c")
            cx.dma("sp", sq_t[:], sinq[:, row0:row0 + G], writes=[cqb], stream="c")
            cx.dma("sp", cmk[:], cmask[slot % 2, :, :].rearrange("p (j t) -> p j t", j=8), writes=[cmkb], stream="c")
            Vv([], [bwb], lambda: nc.vector.memset(biasw[:], 0.0))
            Vv([], [pab], lambda: nc.vector.memset(pa[:], 0.0))
            Vv([], [pbb], lambda: nc.vector.memset(pbt[:], 0.0))

            load_wblk(wblk[0], wblkb[0], s_win, 0)
            load_wblk(wblk[1], wblkb[1], s_wrot, 0)
            for c in range(4):
                mm_group(ps[2][:, :], [(wblk[0][:, kc, c * 128:(c + 1) * 128], xT_b[:, kc, :]) for kc in range(8)],
                         [wblkb[0], xb_buf], pb[2])
                mm_group(ps[3][:, :], [(wblk[1][:, kc, c * 128:(c + 1) * 128], xT_b[:, kc, :]) for kc in range(8)],
                         [wblkb[1], xb_buf], pb[3])
                Vv([pb[2], cqb], [t1b],
                   lambda: nc.vector.tensor_tensor(out=t1[:], in0=ps[2][:, :], in1=cq_t[:], op=ALU.mult))
                Vv([pb[3], cqb], [qfb],
                   lambda: nc.vector.tensor_tensor(out=qT_f[:, c, :], in0=ps[3][:, :], in1=sq_t[:], op=ALU.mult))
                Vv([t1b], [qfb],
                   lambda: nc.vector.tensor_tensor(out=qT_f[:, c, :], in0=qT_f[:, c, :], in1=t1[:], op=ALU.add))
                Aa([qfb], [qab], lambda: nc.scalar.copy(out=qaug[0:64, 2 * c, :], in_=qT_f[0:64, c, :]))
                Aa([qfb], [qab], lambda: nc.scalar.copy(out=qaug[0:64, 2 * c + 1, :], in_=qT_f[64:128, c, :]))

            for t in range(4):
                tr0 = row0 + t * 128
                cx.dma("sp", msk[:, 0, :], negm[tr0:tr0 + 128, :], writes=[mskb], stream="m")
                cx.dma("sp", msk[:, 1, :], valm[tr0:tr0 + 128, :], writes=[mskb], stream="m")
                cx.dma("sp", msk[:, 2, :], ownm[tr0:tr0 + 128, :], writes=[mskb], stream="m")
                cx.deps("pe", [qfb, kmB], [pb[4]])
                ins = None
                for c in range(4):
                    ins = nc.tensor.matmul(ps[4][:, c * 32:(c + 1) * 32], qT_f[:, c, t * 128:(t + 1) * 128],
                                           kmbd[:, c, :], start=True, stop=True)
                cx.op("pe", ins, [qfb, kmB], [pb[4]])
                Vv([pb[4], mskb], [gmb],
                   lambda: nc.vector.tensor_tensor(out=gm[:], in0=ps[4][:, 0:128], in1=msk[:, 0, :], op=ALU.add))
                for h in range(NH):
                    Vv([gmb], [top8b], lambda: nc.vector.max(out=top8[:, h, :], in_=gm[:, h * 16:(h + 1) * 16]))
                for h in range(NH):
                    Vv([gmb, top8b], [selb],
                       lambda: nc.vector.tensor_scalar(out=sel[:, h * 16:(h + 1) * 16], in0=gm[:, h * 16:(h + 1) * 16],
                                                       scalar1=top8[:, h, 2:3], scalar2=None, op0=ALU.is_ge))
                Vv([selb, mskb], [selb],
                   lambda: nc.vector.tensor_tensor(out=sel[:], in0=sel[:], in1=msk[:, 1, :], op=ALU.mult))
                Vv([selb, mskb], [selb],
                   lambda: nc.vector.tensor_tensor(out=sel[:], in0=sel[:], in1=msk[:, 2, :], op=ALU.add))
                Vv([selb], [bwb],
                   lambda: nc.vector.tensor_scalar(out=biasw[:, :, 0:16],
                                                   in0=sel[:, :].rearrange("p (h n) -> p h n", h=NH),
                                                   scalar1=BIG, scalar2=-BIG, op0=ALU.mult, op1=ALU.add))
                for b2 in range(2):
                    transpose(ps[5][:, b2 * 128:(b2 + 1) * 128],
                              biasw[:, b2 * 4:(b2 + 1) * 4, :].rearrange("p h n -> p (h n)"), ident[:],
                              [bwb, cB], pb[5])
                for h in range(NH):
                    p0 = (h % 4) * 32
                    c0 = (h // 4) * 128
                    Aa([pb[5]], [qab], lambda: nc.scalar.copy(out=qaug[64:80, h, t * 128:(t + 1) * 128],
                                                             in_=ps[5][p0:p0 + 16, c0:c0 + 128]))

            nkeys = nkt * 128
            LOOK = 3
            sbanks = [6, 7, 0, 1]

            def epilogue(h_):
                ob = 4 + h_ % 2
                Vv([pb[ob]], [rdb], lambda: nc.vector.reciprocal(out=rden[64:65, :], in_=ps[ob][64:65, :]))
                Vv([rdb], [rd0b], lambda: nc.vector.tensor_copy(out=rden0[0:1, :], in_=rden[64:65, :]))
                mm_group(ps[2][0:64, :], [(ones1[0:1, 0:64], rden0[0:1, :])], [rd0b, cB], pb[2])
                Vv([pb[2]], [bcsb], lambda: nc.vector.tensor_copy(out=bcs[:], in_=ps[2][0:64, :]))
                Vv([pb[ob], bcsb], [atb],
                   lambda: nc.vector.tensor_tensor(out=attnT[0:64, h_, :], in0=ps[ob][0:64, :], in1=bcs[:], op=ALU.mult))

            pending = None
            for h in range(NH):
                kb_ = h % 2
                ob = 4 + h % 2
                cx.dma("sp", kaug[kb_][0:64, 0:nkeys], s_kT[h, :, 0:nkeys], writes=[kab[kb_]], stream="ka")
                cx.dma("sp", kaug[kb_][64:80, 0:nkeys], blkind[:, 0:nkeys], writes=[kab[kb_]], stream="ka")
                cx.dma("sp", vh[kb_][:, 0:nkt, :], s_v[h, :, 0:nkt, :], writes=[vhb[kb_]], stream="va")

                def issue_S(kt_):
                    bk = sbanks[kt_ % 4]
                    mm_group(ps[bk][:, :], [(kaug[kb_][0:80, kt_ * 128:(kt_ + 1) * 128], qaug[0:80, h, :])],
                             [kab[kb_], qab], pb[bk])

                for kt in range(min(LOOK, nkt)):
                    issue_S(kt)
                for kt in range(nkt):
                    if kt + LOOK < nkt:
                        issue_S(kt + LOOK)
                    sbk = sbanks[kt % 4]
                    pj = kt % 4
                    Aa([pb[sbk]], [pTb[pj]],
                       lambda: nc.scalar.activation(out=pT[pj][:], in_=ps[sbk][:, :], func=AF.Exp, scale=0.125))
                    if kt >= nkt - 8:
                        jj = kt - (nkt - 8)
                        Pp([cmkb], [pTb[pj]],
                           lambda: nc.gpsimd.tensor_tensor(out=pT[pj][:], in0=pT[pj][:], in1=cmk[:, jj, :], op=ALU.mult))
                    cx.deps("pe", [vhb[kb_], pTb[pj]], [pb[ob]] if kt == 0 else [])
                    ins = nc.tensor.matmul(ps[ob][0:65, :], vh[kb_][:, kt, 0:65], pT[pj][:], start=(kt == 0),
                                           stop=(kt == nkt - 1))
                    cx.op("pe", ins, [vhb[kb_], pTb[pj]], [pb[ob]] if kt == nkt - 1 else [])
                if pending is not None:
                    epilogue(pending)
                pending = h
            epilogue(pending)

            cx.dma("sp", xh[:], xhalo[slot * 16:(slot + 1) * 16, :], writes=[xhb], stream="c")
            for kc in range(8):
                transpose(ps[0][:, kc * 16:(kc + 1) * 16], xh[:, kc * 128:(kc + 1) * 128], ident[0:16, 0:16],
                          [xhb, cB], pb[0])
            Vv([pb[0]], [xhTb], lambda: nc.vector.tensor_copy(out=xhT[:, :, :],
                                                             in_=ps[0][:, 0:128].rearrange("p (k t) -> p k t", k=8)))
            load_wblk(wblk[0], wblkb[0], s_win, 1536)
            cx.dma("sp", wpool_t[:], s_wpool[:, :].rearrange("(g c) e -> c g e", g=4), reads=[convs["s_wpool"]], writes=[wpb],
                   stream="w")
            for g4 in range(4):
                mm_group(ps[2][:, :], [(wblk[0][:, kc, g4 * 128:(g4 + 1) * 128], xT_b[:, kc, :]) for kc in range(8)],
                         [wblkb[0], xb_buf], pb[2])
                mm_group(ps[3][:, 0:16], [(wblk[0][:, kc, g4 * 128:(g4 + 1) * 128], xhT[:, kc, :]) for kc in range(8)],
                         [wblkb[0], xhTb], pb[3])
                Aa([pb[2]], [uTb], lambda: nc.scalar.copy(out=uT[:, g4, 16:16 + G], in_=ps[2][:, :]))
                Aa([pb[3]], [uTb], lambda: nc.scalar.copy(out=uT[:, g4, 0:16], in_=ps[3][:, 0:16]))
                cur, curb = uT[:, g4, :], uTb
                L = 16 + G
                for k in range(g4 + 1):
                    sh = 1 << k
                    nxt, nxtb = (pa, pab) if k % 2 == 0 else (pbt, pbb)
                    Vv([curb], [nxtb], lambda: nc.vector.tensor_tensor(out=nxt[:, sh:L], in0=cur[:, sh:L],
                                                                      in1=cur[:, 0:L - sh], op=ALU.add))
                    cur, curb = nxt[:, :], nxtb
                cx.dma("sp", invt[:], invc[slot, :, g4 * G:(g4 + 1) * G], writes=[invb], stream="c")
                Vv([curb, invb], [t1b], lambda: nc.vector.tensor_tensor(out=t1[:], in0=cur[:, 16:L], in1=invt[:],
                                                                       op=ALU.mult))
                Vv([t1b, uTb], [dfb], lambda: nc.vector.tensor_tensor(out=diffT[:], in0=t1[:], in1=uT[:, g4, 16:L],
                                                                     op=ALU.subtract))
                mm_group(ps[5][:, :], [(wpool_t[:, g4, :], diffT[:])], [wpb, dfb], pb[5])
                Vv([pb[5], cB], [plb], lambda: nc.vector.tensor_scalar(out=pooledT[:, g4, :], in0=ps[5][:, :],
                                                                      scalar1=fp[:, 48 + g4:49 + g4], scalar2=None,
                                                                      op0=ALU.mult))
            if slot == NSLOT - 1:
                for g4 in range(4):
                    transpose(ps[0][:, g4 * 128:(g4 + 1) * 128], uT[:, g4, 16 + G - 128:16 + G], ident[:],
                              [uTb, cB], pb[0])
                Aa([pb[0]], [sgab], lambda: nc.scalar.copy(out=sga[:], in_=ps[0][:, :]))
                cx.dma("sp", pool_out[:, :], sga[112:128, :], reads=[sgab], stream="po")

            barrier()
            sA1.close()
            merge_ln1(sba, G, xT_f, xfb, xT_b, xb_buf, attnT, atb, pooledT, plb)
            barrier()

        with ExitStack() as sB:
            def sbb(name, shape, dt=F32):
                return sB.enter_context(nc.sbuf_tensor(f"b{slot}_" + name, list(shape), dt))
            moe_and_out(sbb, slot * G, G, y_out)
            barrier()

    with ExitStack() as sC:
        def sbc(name, shape, dt=F32):
            return sC.enter_context(nc.sbuf_tensor("c_" + name, list(shape), dt))
        merge_ln1(sbc, 4, xsT_f, xsfb, xsT_b, xsbb, attnT_s, atsb, pooledT_s, plsb)
        barrier()
    with ExitStack() as sD:
        def sbd(name, shape, dt=F32):
            return sD.enter_context(nc.sbuf_tensor("d_" + name, list(shape), dt))
        moe_and_out(sbd, 0, 4, ys_out)
        barrier()
    cx.drain("sp")


_PROG = None


def _rope_tables(pos):
    half = HD // 2
    inv_freq = 1.0 / (10000.0 ** (np.arange(0, HD, 2, dtype=np.float32) / HD))
    ang = pos.astype(np.float32)[None, :] * inv_freq[:, None].astype(np.float32)
    cos = np.cos(ang).astype(np.float32)
    sin = np.sin(ang).astype(np.float32)
    cos64 = np.concatenate([cos, cos], 0)
    sin64 = np.concatenate([-sin, sin], 0)
    return np.ascontiguousarray(np.concatenate([cos64, cos64], 0)), np.ascontiguousarray(np.concatenate([sin64, sin64], 0))


def kernel(**inp):
    global _PROG
    x_prompt = np.asarray(inp["x_prompt"], np.float32)
    w_in = np.asarray(inp["w_in"], np.float32)[0]
    wqk = w_in[:, 0:1024].reshape(D, 16, 2, 32)
    w_rot = np.ascontiguousarray(wqk[:, :, ::-1, :].reshape(D, 1024))
    w_r = np.concatenate([np.asarray(inp["w_group_router"], np.float32)[0],
                          np.asarray(inp["w_expert_router"], np.float32)[0].transpose(1, 0, 2).reshape(D, 16)], 1)
    w_r = np.ascontiguousarray(w_r.reshape(8, 128, 20).transpose(1, 0, 2).reshape(128, 160))
    b_r = np.concatenate([np.asarray(inp["b_group_router"], np.float32)[0],
                          np.asarray(inp["b_expert_router"], np.float32)[0].reshape(16)])
    b_r = np.ascontiguousarray(np.broadcast_to(b_r[None, :], (128, 20)))

    def fm(v):
        return np.asarray(v, np.float32).reshape(8, 128).T

    fpar = np.concatenate([fm(inp["ln1_g"][0]), fm(inp["ln1_b"][0]), fm(inp["ln2_g"][0]), fm(inp["ln2_b"][0]),
                           fm(inp["b_gate"][0, 0]), fm(inp["b_gate"][0, 1]),
                           np.asarray(inp["pool_scale"], np.float32)[0].reshape(4, 128).T], 1)
    fpar = np.ascontiguousarray(fpar)
    cosk, sink = _rope_tables(np.arange(SEQ))
    blk = (np.arange(SEQ)[None, :] // 256 == np.arange(16)[:, None]).astype(ml_dtypes.bfloat16)
    ident = np.eye(128, dtype=np.float32)
    sel16 = np.ascontiguousarray(np.repeat(np.eye(16, dtype=np.float32)[:, :, None], 128, axis=2).reshape(16, 16 * 128))
    kk = np.arange(128)[:, None]
    qq = np.arange(G)[None, :]
    mt = []
    for t in range(4):
        kp = 128 * t + kk
        mt.append(1.0 - ((kp // 256 == qq // 256) & (kp > qq)).astype(np.float32))
    ones = np.ones((128, G), np.float32)
    zeros = np.zeros((128, G), np.float32)
    lower = np.concatenate(mt + [zeros] * 4, 1)
    upper = np.concatenate([ones] * 4 + mt, 1)

    x_sample = np.asarray(inp["x_sample"], np.float32)[:, 0, :]
    ck = np.asarray(inp["cache_k"], np.float32)[0].reshape(2560 * 8, 8192)
    cv = np.asarray(inp["cache_v"], np.float32)[0].reshape(2560 * 8, 8192)
    page_table = np.asarray(inp["page_table"], np.int32)
    state_pool = np.asarray(inp["state_pool"], np.float32)[0]
    c8, s8 = _rope_tables(np.array([8192]))
    cs8 = np.ascontiguousarray(np.tile(c8[0:64, 0][None, :], (4, NH)))
    sn8 = np.ascontiguousarray(np.tile(s8[0:64, 0][None, :], (4, NH)))
    pp = np.arange(128)
    selT = np.zeros((4, 256), np.float32)
    pairsel = np.zeros((128, 8), np.float32)
    for t in range(2):
        for p_ in range(128):
            selT[2 * t + p_ // 64, t * 128 + p_] = 1.0
            pairsel[p_, t * 4 + 2 * t + p_ // 64] = 1.0
    in_maps = []
    for c in range(8):
        s, p = c // 2, c % 2
        groups = [0, 3, 4, 7] if p == 0 else [1, 2, 5, 6]
        xs = x_prompt[s]
        xo = np.concatenate([xs[g * G:(g + 1) * G] for g in groups], 0)
        xh = np.zeros((NSLOT * 16, D), np.float32)
        for i, g in enumerate(groups):
            if g > 0:
                xh[i * 16:(i + 1) * 16] = xs[g * G - 16:g * G]
        posq = np.concatenate([np.arange(g * G, (g + 1) * G) for g in groups])
        cq, sq = _rope_tables(posq)
        own = posq // 256
        nb = np.arange(16)[None, :]
        valm = np.ascontiguousarray(np.tile((nb < own[:, None]).astype(np.float32), (1, NH)))
        ownm = np.ascontiguousarray(np.tile((nb == own[:, None]).astype(np.float32), (1, NH)))
        negm = np.ascontiguousarray(np.tile(np.where(nb < own[:, None], 0.0, NEG).astype(np.float32), (1, NH)))
        cm = np.stack([lower if (g % 2 == 0) else upper for g in groups[:2]], 0).astype(ml_dtypes.bfloat16)
        invc = np.zeros((NSLOT, 128, 4 * G), np.float32)
        for i, g in enumerate(groups):
            pos = np.arange(g * G, (g + 1) * G)
            for gi, w in enumerate((2, 4, 8, 16)):
                invc[i, :, gi * G:(gi + 1) * G] = (1.0 / np.minimum(w, pos + 1))[None, :]
        in_maps.append({
            "xseq": np.ascontiguousarray(xs), "xown": np.ascontiguousarray(xo), "xhalo": xh,
            "w_in": w_in, "w_rot": w_rot,
            "w_pool": np.asarray(inp["w_pool"], np.float32)[0].reshape(512, 128),
            "w_a": np.asarray(inp["w_branch_a"], np.float32)[0], "w_b": np.asarray(inp["w_branch_b"], np.float32)[0],
            "w_out": np.asarray(inp["w_out"], np.float32)[0],
            "w_eg": np.asarray(inp["w_e_gate"], np.float32)[0].reshape(NE * D, DE),
            "w_eu": np.asarray(inp["w_e_up"], np.float32)[0].reshape(NE * D, DE),
            "w_ed": np.asarray(inp["w_e_down"], np.float32)[0].reshape(NE * DE, D),
            "w_r": w_r, "b_r": b_r, "fpar": fpar, "cosk": cosk, "sink": sink, "cosq": cq, "sinq": sq,
            "negm": negm, "valm": valm, "ownm": ownm, "cmask": np.ascontiguousarray(cm.reshape(2, 128, 8 * G)),
            "blkind": blk, "invc": invc, "ident": ident, "sel16": sel16,
            "xs4": np.ascontiguousarray(x_sample[4 * c:4 * c + 4]), "ck": ck, "cv": cv,
            "pt": np.ascontiguousarray(page_table[4 * c:4 * c + 4].reshape(2, 128).T),
            "st4": np.ascontiguousarray(state_pool[4 * c:4 * c + 4].reshape(4, 15 * 512)),
            "cs8": cs8, "sn8": sn8, "selT": selT, "pairsel": pairsel,
        })
    if _PROG is None:
        _PROG = build_program()
    res = run_bass_kernel_spmd(_PROG, in_maps, core_ids=list(range(8)))
    R = res.results
    y_p = np.zeros((4, SEQ, D), np.float32)
    k_p = np.zeros((1, 4, SEQ, NH, HD), np.float32)
    v_p = np.zeros((1, 4, SEQ, NH, HD), np.float32)
    pool_p = np.zeros((1, 4, 15, 512), np.float32)
    for c in range(8):
        s, p = c // 2, c % 2
        groups = [0, 3, 4, 7] if p == 0 else [1, 2, 5, 6]
        for i, g in enumerate(groups):
            y_p[s, g * G:(g + 1) * G] = R[c]["y_out"][i * G:(i + 1) * G]
        if p == 0:
            k_p[0, s] = R[c]["k_out"].reshape(SEQ, NH, HD)
            v_p[0, s] = R[c]["v_out"].reshape(SEQ, NH, HD)
            pool_p[0, s] = R[c]["pool_out"][1:16]
    y_s = np.zeros((32, 1, D), np.float32)
    k_s = np.zeros((1, 32, 1, NH, HD), np.float32)
    v_s = np.zeros((1, 32, 1, NH, HD), np.float32)
    pool_s = np.zeros((1, 32, 15, 512), np.float32)
    for c in range(8):
        y_s[4 * c:4 * c + 4, 0] = R[c]["ys_out"]
        k_s[0, 4 * c:4 * c + 4, 0] = R[c]["ks_out"].reshape(4, NH, HD)
        v_s[0, 4 * c:4 * c + 4, 0] = R[c]["vs_out"].reshape(4, NH, HD)
        pool_s[0, 4 * c:4 * c + 4] = R[c]["ps_out"].reshape(4, 15, 512)
    return (y_p, y_s, k_p, v_p, pool_p, k_s, v_s, pool_s)
```

```python
import numpy as np
import ml_dtypes
import concourse.bass as bass
import concourse.mybir as mybir
from concourse.bass_utils import run_bass_kernel_spmd

F32 = mybir.dt.float32
BF16 = mybir.dt.bfloat16
I32 = mybir.dt.int32
ALU = mybir.AluOpType
AF = mybir.ActivationFunctionType
AX = mybir.AxisListType

D = 1024
SEQ = 4096
NH = 8
HD = 64
G = 512
NG = 8
NSLOT = 4
ALPHA = 2.0 ** 0.25
BIG = 30000.0
NEG = -1.0e30
LN_EPS = 1e-5
NE = 16
DE = 256
STOP = 99


class Buf:
    __slots__ = ("w", "r")

    def __init__(self):
        self.w = None
        self.r = []


class Ctx:
    def __init__(self, nc, stack):
        self.nc = nc
        self.stack = stack
        self.eng = {"pe": nc.tensor, "act": nc.scalar, "dve": nc.vector, "pool": nc.gpsimd, "sp": nc.sync}
        self.sem = {}
        self.cnt = {}
        self.waited = {e: {} for e in self.eng}
        self.nsem = 0
        for e in self.eng:
            self._new_sem(e)
        self.dsem = {}
        self.dcnt = {}
        self.dnext = {}

    def _new_sem(self, e):
        self.nsem += 1
        s = self.stack.enter_context(self.nc.semaphore(f"s_{e}_{self.nsem}"))
        self.sem[e] = s
        self.cnt[e] = 0

    def epoch(self):
        for e in self.eng:
            if self.cnt[e] > 20000:
                self._new_sem(e)

    def _wait(self, e, tok):
        if tok is None:
            return
        sem, val = tok
        key = id(sem)
        if self.waited[e].get(key, 0) >= val:
            return
        self.waited[e][key] = val
        self.eng[e].wait_ge(sem, val)

    def deps(self, e, reads, writes):
        for b in reads:
            self._wait(e, b.w)
        for b in writes:
            self._wait(e, b.w)
            for t in b.r:
                self._wait(e, t)

    def done(self, tok, reads, writes):
        for b in reads:
            b.r.append(tok)
            if len(b.r) > 12:
                b.r = b.r[-12:]
        for b in writes:
            b.w = tok
            b.r = []

    def op(self, e, ins, reads=(), writes=()):
        self.cnt[e] += 1
        ins.then_inc(self.sem[e], 1)
        tok = (self.sem[e], self.cnt[e])
        self.waited[e][id(self.sem[e])] = max(self.waited[e].get(id(self.sem[e]), 0), 0)
        self.done(tok, reads, writes)
        return tok

    NROT = 4

    def dma(self, q, out, in_, reads=(), writes=(), stream="d", **kw):
        e = q
        self.deps(e, reads, writes)
        key = (q, stream)
        if key not in self.dsem:
            sems = []
            for i in range(self.NROT):
                self.nsem += 1
                sems.append(self.stack.enter_context(self.nc.semaphore(f"dma_{q}_{stream}_{self.nsem}")))
            self.dsem[key] = sems
            self.dcnt[key] = [0] * self.NROT
            self.dnext[key] = 0
        i = self.dnext[key]
        self.dnext[key] = (i + 1) % self.NROT
        sem = self.dsem[key][i]
        if self.dcnt[key][i] > 0:
            self._wait(e, (sem, self.dcnt[key][i]))
        self.dcnt[key][i] += 16
        self.eng[e].dma_start(out=out, in_=in_, **kw).then_inc(sem, 16)
        tok = (sem, self.dcnt[key][i])
        self.done(tok, reads, writes)
        return tok

    def dma_done(self, q, ins, reads, writes, stream):
        key = (q, stream)
        if key not in self.dsem:
            sems = []
            for i in range(self.NROT):
                self.nsem += 1
                sems.append(self.stack.enter_context(self.nc.semaphore(f"dma_{q}_{stream}_{self.nsem}")))
            self.dsem[key] = sems
            self.dcnt[key] = [0] * self.NROT
            self.dnext[key] = 0
        i = self.dnext[key]
        self.dnext[key] = (i + 1) % self.NROT
        sem = self.dsem[key][i]
        self.dcnt[key][i] += 16
        ins.then_inc(sem, 16)
        tok = (sem, self.dcnt[key][i])
        self.done(tok, reads, writes)
        return tok

    def drain(self, e):
        for key, sems in self.dsem.items():
            for i, sem in enumerate(sems):
                if self.dcnt[key][i] > 0:
                    self._wait(e, (sem, self.dcnt[key][i]))


def _emit(cx, e, reads, writes, fn):
    cx.deps(e, reads, writes)
    ins = fn()
    return cx.op(e, ins, reads, writes)


def build_program():
    from contextlib import ExitStack
    nc = bass.Bass("TRN2", target_bir_lowering=False)
    st = ExitStack()
    with st:
        _build(nc, st)
    return nc


def _build(nc, st):
    from contextlib import ExitStack
    cx = Ctx(nc, st)

    def din(name, shape, dt=F32):
        return nc.dram_tensor(name, list(shape), dt, kind="ExternalInput").ap()

    def dout(name, shape, dt=F32):
        return nc.dram_tensor(name, list(shape), dt, kind="ExternalOutput").ap()

    def dscr(name, shape, dt=BF16):
        return nc.dram_tensor(name, list(shape), dt, kind="Internal").ap()

    def sb(name, shape, dt=F32):
        return st.enter_context(nc.sbuf_tensor("sb_" + name, list(shape), dt))

    xseq = din("xseq", [SEQ, D])
    xown = din("xown", [NSLOT * G, D])
    xhalo = din("xhalo", [NSLOT * 16, D])
    w_in = din("w_in", [D, 4096])
    w_rot = din("w_rot", [D, 1024])
    w_pool = din("w_pool", [4 * 128, 128])
    w_a = din("w_a", [512, D])
    w_b = din("w_b", [512, D])
    w_out = din("w_out", [D, D])
    w_eg = din("w_eg", [NE * D, DE])
    w_eu = din("w_eu", [NE * D, DE])
    w_ed = din("w_ed", [NE * DE, D])
    w_r = din("w_r", [128, 8 * 20])
    b_r = din("b_r", [128, 20])
    fpar = din("fpar", [128, 52])
    cosk = din("cosk", [128, SEQ])
    sink = din("sink", [128, SEQ])
    cosq = din("cosq", [128, NSLOT * G])
    sinq = din("sinq", [128, NSLOT * G])
    negm = din("negm", [NSLOT * G, 128])
    valm = din("valm", [NSLOT * G, 128])
    ownm = din("ownm", [NSLOT * G, 128])
    sel16_in = din("sel16", [16, 16 * 128])
    cmask = din("cmask", [2, 128, 8 * G], BF16)
    blkind = din("blkind", [16, SEQ], BF16)
    invc = din("invc", [NSLOT, 128, 4 * G])
    ident_in = din("ident", [128, 128])
    xs4 = din("xs4", [4, D])
    ck = din("ck", [2560 * 8, 8192])
    cv = din("cv", [2560 * 8, 8192])
    pt_in = din("pt", [128, 2], I32)
    st4 = din("st4", [4, 15 * 512])
    cs8 = din("cs8", [4, 512])
    sn8 = din("sn8", [4, 512])
    selT_in = din("selT", [4, 256])
    pairsel_in = din("pairsel", [128, 8])

    y_out = dout("y_out", [NSLOT * G, D])
    k_out = dout("k_out", [SEQ, 512])
    v_out = dout("v_out", [SEQ, 512])
    pool_out = dout("pool_out", [16, 512])
    ys_out = dout("ys_out", [4, D])
    ks_out = dout("ks_out", [4, 512])
    vs_out = dout("vs_out", [4, 512])
    ps_out = dout("ps_out", [4, 15 * 512])

    s_win = dscr("s_win", [D, 4096])
    s_wrot = dscr("s_wrot", [D, 1024])
    s_wpool = dscr("s_wpool", [512, 128])
    s_wa = dscr("s_wa", [512, D])
    s_wb = dscr("s_wb", [512, D])
    s_wout = dscr("s_wout", [D, D])
    s_eg = dscr("s_eg", [NE * D, DE])
    s_eu = dscr("s_eu", [NE * D, DE])
    s_ed = dscr("s_ed", [NE * DE, D])
    s_kT = dscr("s_kT", [NH, HD, SEQ])
    s_v = dscr("s_v", [NH, 128, 32, 66])

    convs = {}

    def convert(dst, src, rows, cols, name):
        conv = Buf()
        convs[name] = conv
        tot = rows * cols
        L = 2048 if tot % 2048 == 0 else cols
        R = tot // L
        if cols != L:
            if cols > L:
                s2 = src.rearrange("r (a b) -> (r a) b", b=L)
                d2 = dst.rearrange("r (a b) -> (r a) b", b=L)
            else:
                s2 = src.rearrange("(r a) b -> r (a b)", a=L // cols)
                d2 = dst.rearrange("(r a) b -> r (a b)", a=L // cols)
        else:
            s2, d2 = src, dst
        step = 512
        for r0 in range(0, R, step):
            r1 = min(R, r0 + step)
            cx.dma("pool", d2[r0:r1, :], s2[r0:r1, :], writes=[conv], stream="conv")

    convert(s_win, w_in, D, 4096, "s_win")
    convert(s_wrot, w_rot, D, 1024, "s_wrot")
    convert(s_wpool, w_pool, 512, 128, "s_wpool")
    convert(s_wa, w_a, 512, D, "s_wa")
    convert(s_wb, w_b, 512, D, "s_wb")
    convert(s_wout, w_out, D, D, "s_wout")
    convert(s_eg, w_eg, NE * D, DE, "s_eg")
    convert(s_eu, w_eu, NE * D, DE, "s_eu")
    convert(s_ed, w_ed, NE * DE, D, "s_ed")

    if STOP == 0:
        cx.drain("sp")
        return
    ident = sb("ident", [128, 128])
    identb = sb("identb", [128, 128], BF16)
    ones_ln = sb("ones_ln", [128, 128])
    fp = sb("fp", [128, 52])
    wr_sb = sb("wr_sb", [128, 8 * 20])
    br_sb = sb("br_sb", [128, 20])
    cB = Buf()
    cx.dma("sp", ident[:], ident_in[:, :], writes=[cB])
    cx.dma("sp", fp[:], fpar[:, :], writes=[cB])
    cx.dma("sp", wr_sb[:], w_r[:, :], writes=[cB])
    cx.dma("sp", br_sb[:], b_r[:, :], writes=[cB])
    _emit(cx, "dve", [cB], [cB], lambda: nc.vector.tensor_copy(out=identb[:], in_=ident[:]))
    _emit(cx, "dve", [], [cB], lambda: nc.vector.memset(ones_ln[:], 1.0 / D))

    ps = [st.enter_context(nc.psum_tensor(f"ps{i}", [128, 512], F32)) for i in range(8)]
    pb = [Buf() for _ in range(8)]

    def mm_group(out_ap, pairs, reads, wbuf):
        cx.deps("pe", reads, [wbuf])
        n = len(pairs)
        ins = None
        for i, (l, r) in enumerate(pairs):
            ins = nc.tensor.matmul(out_ap, l, r, start=(i == 0), stop=(i == n - 1))
        return cx.op("pe", ins, reads, [wbuf])

    def transpose(out_ap, in_ap, idt, reads, wbuf):
        cx.deps("pe", reads, [wbuf])
        ins = nc.tensor.transpose(out_ap, in_ap, idt)
        return cx.op("pe", ins, reads, [wbuf])

    def load_xT(x_dram, row0, ntok, xT_f, xT_b, xf_buf, xb_buf, xt_tiles, xt_bufs, psA, psB):
        nt = ntok // 128
        for t in range(nt):
            xt = xt_tiles[t % 2]
            xtb = xt_bufs[t % 2]
            cx.dma("sp", xt[:], x_dram[row0 + t * 128: row0 + (t + 1) * 128, :], writes=[xtb], stream="x")
            for half in range(2):
                pi = psA if half == 0 else psB
                for j in range(4):
                    kc = half * 4 + j
                    transpose(ps[pi][:, j * 128:(j + 1) * 128], xt[:, kc * 128:(kc + 1) * 128], ident[:],
                              [xtb, cB], pb[pi])
                src = ps[pi][:, :].rearrange("p (j t) -> p j t", j=4)
                if xT_f is not None:
                    dstf = xT_f[:, half * 4:(half + 1) * 4, t * 128:(t + 1) * 128]
                    _emit(cx, "act", [pb[pi]], [xf_buf], lambda: nc.scalar.copy(out=dstf, in_=src))
                dstb = xT_b[:, half * 4:(half + 1) * 4, t * 128:(t + 1) * 128]
                _emit(cx, "dve", [pb[pi]], [xb_buf], lambda: nc.vector.tensor_copy(out=dstb, in_=src))

    def barrier():
        for e in cx.eng:
            for e2 in cx.eng:
                if e2 != e and cx.cnt[e2] > 0:
                    cx._wait(e, (cx.sem[e2], cx.cnt[e2]))
            cx.drain(e)

    def Vv(reads, writes, fn):
        return _emit(cx, "dve", reads, writes, fn)

    def Aa(reads, writes, fn):
        return _emit(cx, "act", reads, writes, fn)

    def Pp(reads, writes, fn):
        return _emit(cx, "pool", reads, writes, fn)

    def load_wblk(tile, tb, src, c0, ncols=512, nk=8):
        cx.dma("sp", tile[:, 0:nk, 0:ncols], src[:, c0:c0 + ncols].rearrange("(kc p) j -> p kc j", p=128),
               reads=[convs[src.tensor.name]], writes=[tb], stream="w")

    def load_xT(x_dram, row0, ntok, xT_f, xT_b, xf_buf, xb_buf, xt_tiles, xt_bufs, psA, psB):
        nt = ntok // 128
        for t in range(nt):
            xt = xt_tiles[t % 2]
            xtb = xt_bufs[t % 2]
            cx.dma("sp", xt[:], x_dram[row0 + t * 128: row0 + (t + 1) * 128, :], writes=[xtb], stream="x")
            for half in range(2):
                pi = psA if half == 0 else psB
                for j in range(4):
                    kc = half * 4 + j
                    transpose(ps[pi][:, j * 128:(j + 1) * 128], xt[:, kc * 128:(kc + 1) * 128], ident[:],
                              [xtb, cB], pb[pi])
                src = ps[pi][:, :].rearrange("p (j t) -> p j t", j=4)
                dstb = xT_b[:, half * 4:(half + 1) * 4, t * 128:(t + 1) * 128]
                if xT_f is not None:
                    dstf = xT_f[:, half * 4:(half + 1) * 4, t * 128:(t + 1) * 128]
                    Aa([pb[pi]], [xf_buf], lambda: nc.scalar.copy(out=dstf, in_=src))
                    Vv([xf_buf], [xb_buf], lambda: nc.vector.tensor_copy(out=dstb, in_=dstf))
                else:
                    Vv([pb[pi]], [xb_buf], lambda: nc.vector.tensor_copy(out=dstb, in_=src))

    xt_tiles = [sb(f"xt{i}", [128, D]) for i in range(2)]
    xt_bufs = [Buf(), Buf()]
    xT_b = sb("xT_b", [128, 8, G], BF16)
    xb_buf = Buf()
    kmT = sb("kmT", [128, 4, 16])
    kmB = Buf()
    kmbd = sb("kmbd", [128, 4, 32])
    ones1 = sb("ones1", [1, 64])
    sel16 = sb("sel16", [16, 16 * 128])
    cx.dma("sp", sel16[:], sel16_in[:, :], writes=[cB])
    Vv([], [cB], lambda: nc.vector.memset(ones1[:], 1.0))

    xsT_f = sb("xsT_f", [128, 8, 4])
    xsfb = Buf()
    xsT_b = sb("xsT_b", [128, 8, 4], BF16)
    xsbb = Buf()
    attnT_s = sb("attnT_s", [64, NH, 4], BF16)
    atsb = Buf()
    pooledT_s = sb("pooledT_s", [128, 4, 4], BF16)
    plsb = Buf()
    with ExitStack() as sS:
        def sbs(name, shape, dt=F32):
            return sS.enter_context(nc.sbuf_tensor("s_" + name, list(shape), dt))
        xs_t = sbs("xs_t", [4, D])
        xsb = Buf()
        wS0 = sbs("wS0", [128, 8, 512], BF16)
        wS = [wS0, wS0]
        wSb0 = Buf()
        wSb = [wSb0, wSb0]
        tok = [sbs(f"tok{i}", [4, 512]) for i in range(6)]
        tokb = [Buf() for _ in range(6)]
        cst = sbs("cst", [4, 2, 512])
        cstb = Buf()
        tmp4 = sbs("tmp4", [4, 512])
        tmp4b = Buf()
        st_t = sbs("st_t", [4, 26, 128])
        stb_ = Buf()
        ssum = sbs("ssum", [4, 512])
        ssb = Buf()
        diff4 = sbs("diff4", [4, 512])
        d4b = Buf()
        diffT_s = sbs("diffT_s", [128, 4, 4], BF16)
        dTsb = Buf()
        wpool_s = sbs("wpool_s", [128, 4, 128], BF16)
        wpsb = Buf()
        pt_sb = sbs("pt_sb", [128, 2], I32)
        idx8 = sbs("idx8", [128, 2, 8], I32)
        ptb = Buf()
        selT = sbs("selT", [4, 256])
        pairsel = sbs("pairsel", [128, 8])
        scb = Buf()
        q_bc = sbs("q_bc", [128, 512])
        qbb = Buf()
        KV = [sbs(f"KV{i}", [128, 8192]) for i in range(2)]
        KVb = [Buf(), Buf()]
        s_all = sbs("s_all", [128, 128, NH])
        sab = Buf()
        Pm = sbs("Pm", [128, 128, NH])
        Pmb = Buf()
        gpage = sbs("gpage", [128, NH])
        gpb = Buf()
        gpT = sbs("gpT", [NH, 128])
        gpTb = Buf()
        gblk = sbs("gblk", [NH, 64])
        gbb = Buf()
        top8s = sbs("top8s", [NH, 8])
        selb_ = sbs("selb", [NH, 64])
        selp = sbs("selp", [NH, 128])
        spb = Buf()
        maskp = sbs("maskp", [128, NH])
        mpb = Buf()
        den = sbs("den", [128, NH])
        denb = Buf()
        Oacc = sbs("Oacc", [128, 512])
        Oab = Buf()
        red = sbs("red", [128, 512])
        redb = Buf()
        snew = sbs("snew", [4, 3, NH])
        snb = Buf()
        Osum = sbs("Osum", [4, 512])
        Osb = Buf()
        attn_tok = sbs("attn_tok", [4, 512])
        atkb = Buf()

        def phase_S():
            cx.dma("sp", xs_t[:], xs4[:, :], writes=[xsb], stream="c")
            cx.dma("sp", cst[:, 0, :], cs8[:, :], writes=[cstb], stream="c")
            cx.dma("sp", cst[:, 1, :], sn8[:, :], writes=[cstb], stream="c")
            st_v = st4[:, :].rearrange("p (r c) -> p r c", r=15)
            for g4, (w_, off_) in enumerate(((2, 0), (4, 1), (8, 4), (16, 11))):
                cx.dma("sp", st_t[:, off_:off_ + w_ - 1, :], st_v[:, 15 - (w_ - 1):15, g4 * 128:(g4 + 1) * 128],
                       writes=[stb_], stream="c")
            cx.dma("sp", pt_sb[:], pt_in[:, :], writes=[ptb], stream="c")
            cx.dma("sp", selT[:], selT_in[:, :], writes=[scb], stream="c")
            cx.dma("sp", pairsel[:], pairsel_in[:, :], writes=[scb], stream="c")
            cx.dma("sp", wpool_s[:], s_wpool[:, :].rearrange("(g c) e -> c g e", g=4), reads=[convs["s_wpool"]], writes=[wpsb],
                   stream="w")
            for kc in range(8):
                transpose(ps[0][:, kc * 4:(kc + 1) * 4], xs_t[0:4, kc * 128:(kc + 1) * 128], ident[0:4, 0:4],
                          [xsb, cB], pb[0])
            Aa([pb[0]], [xsfb], lambda: nc.scalar.copy(out=xsT_f[:, :, :],
                                                      in_=ps[0][:, 0:32].rearrange("p (k t) -> p k t", k=8)))
            Vv([xsfb], [xsbb], lambda: nc.vector.tensor_copy(out=xsT_b[:, :, :], in_=xsT_f[:, :, :]))
            for i, (srcw, c0) in enumerate(((s_win, 0), (s_wrot, 0), (s_win, 512), (s_wrot, 512), (s_win, 1024),
                                            (s_win, 1536))):
                load_wblk(wS[i % 2], wSb[i % 2], srcw, c0)
                pi = 2 + i % 2
                mm_group(ps[pi][0:4, :], [(xsT_b[:, kc, :], wS[i % 2][:, kc, :]) for kc in range(8)],
                         [wSb[i % 2], xsbb], pb[pi])
                Aa([pb[pi]], [tokb[i]], lambda: nc.scalar.copy(out=tok[i][:], in_=ps[pi][0:4, :]))
            for (a_, r_) in ((0, 1), (2, 3)):
                Vv([tokb[a_], cstb], [tokb[a_]], lambda: nc.vector.tensor_tensor(out=tok[a_][:], in0=tok[a_][:],
                                                                                in1=cst[:, 0, :], op=ALU.mult))
                Vv([tokb[r_], cstb], [tokb[r_]], lambda: nc.vector.tensor_tensor(out=tok[r_][:], in0=tok[r_][:],
                                                                                in1=cst[:, 1, :], op=ALU.mult))
                Vv([tokb[r_]], [tokb[a_]], lambda: nc.vector.tensor_tensor(out=tok[a_][:], in0=tok[a_][:], in1=tok[r_][:],
                                                                          op=ALU.add))
            q_tok, k_tok, v_tok, u_tok = tok[0], tok[2], tok[4], tok[5]
            qtb, ktb, vtb, utb = tokb[0], tokb[2], tokb[4], tokb[5]
            cx.dma("sp", ks_out[:, :], k_tok[:], reads=[ktb], stream="so")
            cx.dma("sp", vs_out[:, :], v_tok[:], reads=[vtb], stream="so")
            cx.dma("sp", ps_out[:, 14 * 512:15 * 512], u_tok[:], reads=[utb], stream="so")
            cx.dma("sp", ps_out[:, 0:14 * 512], st4[:, 512:15 * 512], stream="so")
            for g4, (w_, off_) in enumerate(((2, 0), (4, 1), (8, 4), (16, 11))):
                c0 = g4 * 128
                Vv([stb_], [ssb], lambda: nc.vector.tensor_reduce(
                    out=ssum[:, c0:c0 + 128], in_=st_t[:, off_:off_ + w_ - 1, :].rearrange("p r c -> p c r"),
                    axis=AX.X, op=ALU.add))
                Vv([ssb, utb], [tmp4b], lambda: nc.vector.tensor_tensor(out=tmp4[:, c0:c0 + 128], in0=ssum[:, c0:c0 + 128],
                                                                       in1=u_tok[:, c0:c0 + 128], op=ALU.add))
                Vv([tmp4b, utb], [d4b], lambda: nc.vector.scalar_tensor_tensor(
                    out=diff4[:, c0:c0 + 128], in0=tmp4[:, c0:c0 + 128], scalar=1.0 / w_, in1=u_tok[:, c0:c0 + 128],
                    op0=ALU.mult, op1=ALU.subtract))
            for g4 in range(4):
                transpose(ps[1][:, g4 * 4:(g4 + 1) * 4], diff4[0:4, g4 * 128:(g4 + 1) * 128], ident[0:4, 0:4],
                          [d4b, cB], pb[1])
            Vv([pb[1]], [dTsb], lambda: nc.vector.tensor_copy(out=diffT_s[:, :, :],
                                                             in_=ps[1][:, 0:16].rearrange("p (g t) -> p g t", g=4)))
            for g4 in range(4):
                mm_group(ps[2][:, 0:4], [(wpool_s[:, g4, :], diffT_s[:, g4, :])], [wpsb, dTsb], pb[2])
                Vv([pb[2], cB], [plsb], lambda: nc.vector.tensor_scalar(out=pooledT_s[:, g4, :], in0=ps[2][:, 0:4],
                                                                       scalar1=fp[:, 48 + g4:49 + g4], scalar2=None,
                                                                       op0=ALU.mult))
            for c in range(8):
                Vv([ptb], [ptb], lambda: nc.vector.tensor_scalar(out=idx8[:, :, c], in0=pt_sb[:, :], scalar1=8.0,
                                                                scalar2=float(c), op0=ALU.mult, op1=ALU.add))
            nbuf = 0
            for t in range(2):
                mm_group(ps[3][:, :], [(selT[0:4, t * 128:(t + 1) * 128], q_tok[0:4, :])], [scb, qtb], pb[3])
                Aa([pb[3]], [qbb], lambda: nc.scalar.copy(out=q_bc[:], in_=ps[3][:, :]))
                for c in range(8):
                    kb_ = nbuf % 2
                    nbuf += 1
                    cx.deps("pool", [ptb], [KVb[kb_]])
                    ins = nc.gpsimd.indirect_dma_start(out=KV[kb_][:, :], out_offset=None, in_=ck[:, :],
                                                       in_offset=bass.IndirectOffsetOnAxis(ap=idx8[:, t, c:c + 1], axis=0))
                    cx.dma_done("pool", ins, [ptb], [KVb[kb_]], "g")
                    (Pp if c % 2 == 0 else Vv)([qbb], [KVb[kb_]], lambda: (nc.gpsimd if c % 2 == 0 else nc.vector).tensor_tensor(
                        out=KV[kb_][:, :].rearrange("p (r e) -> p r e", r=16),
                        in0=KV[kb_][:, :].rearrange("p (r e) -> p r e", r=16),
                        in1=q_bc[:, :].rearrange("p (o e) -> p o e", o=1).broadcast_to([128, 16, 512]), op=ALU.mult))
                    Vv([KVb[kb_]], [sab], lambda: nc.vector.tensor_reduce(
                        out=s_all[:, c * 16:(c + 1) * 16, :].rearrange("p r h -> p (r h)"),
                        in_=KV[kb_][:, :].rearrange("p (a d) -> p a d", d=HD), axis=AX.X, op=ALU.add))
                    yield
                Vv([sab], [gpb], lambda: nc.vector.tensor_reduce(out=gpage[:, :], in_=s_all[:, :, :].rearrange("p r h -> p h r"),
                                                                axis=AX.X, op=ALU.add))
                transpose(ps[4][0:NH, 0:128], gpage[:, :], ident[:], [gpb, cB], pb[4])
                Aa([pb[4]], [gpTb], lambda: nc.scalar.copy(out=gpT[:, :], in_=ps[4][0:NH, 0:128]))
                gv = gpT[:, :].rearrange("h (n two) -> h n two", two=2)
                Vv([gpTb], [gbb], lambda: nc.vector.tensor_tensor(out=gblk[:, :], in0=gv[:, :, 0], in1=gv[:, :, 1], op=ALU.add))
                for s2 in range(2):
                    Vv([gbb], [gbb], lambda: nc.vector.max(out=top8s[:, :], in_=gblk[:, s2 * 32:(s2 + 1) * 32]))
                    Vv([gbb], [gbb], lambda: nc.vector.tensor_scalar(out=selb_[:, s2 * 32:(s2 + 1) * 32],
                                                                    in0=gblk[:, s2 * 32:(s2 + 1) * 32],
                                                                    scalar1=top8s[:, 2:3], scalar2=None, op0=ALU.is_ge))
                sv = selp[:, :].rearrange("h (n two) -> h n two", two=2)
                Vv([gbb], [spb], lambda: nc.vector.tensor_copy(out=sv[:, :, 0], in_=selb_[:, :]))
                Vv([gbb], [spb], lambda: nc.vector.tensor_copy(out=sv[:, :, 1], in_=selb_[:, :]))
                transpose(ps[4][:, 128:128 + NH], selp[:, :], ident[0:NH, 0:NH], [spb, cB], pb[4])
                Aa([pb[4]], [mpb], lambda: nc.scalar.copy(out=maskp[:, :], in_=ps[4][:, 128:128 + NH]))
                Aa([sab], [Pmb], lambda: nc.scalar.activation(out=Pm[:, :, :], in_=s_all[:, :, :], func=AF.Exp, scale=0.125))
                Vv([mpb], [Pmb], lambda: nc.vector.tensor_tensor(
                    out=Pm[:, :, :], in0=Pm[:, :, :],
                    in1=maskp[:, :].rearrange("p (o h) -> p o h", o=1).broadcast_to([128, 128, NH]), op=ALU.mult))
                Vv([Pmb], [denb], lambda: nc.vector.tensor_reduce(out=den[:, :], in_=Pm[:, :, :].rearrange("p r h -> p h r"),
                                                                 axis=AX.X, op=ALU.add))
                Vv([], [Oab], lambda: nc.vector.memset(Oacc[:], 0.0))
                for c in range(8):
                    kb_ = nbuf % 2
                    nbuf += 1
                    cx.deps("pool", [ptb], [KVb[kb_]])
                    ins = nc.gpsimd.indirect_dma_start(out=KV[kb_][:, :], out_offset=None, in_=cv[:, :],
                                                       in_offset=bass.IndirectOffsetOnAxis(ap=idx8[:, t, c:c + 1], axis=0))
                    cx.dma_done("pool", ins, [ptb], [KVb[kb_]], "g")
                    (Pp if c % 2 == 0 else Vv)([Pmb], [KVb[kb_]], lambda: (nc.gpsimd if c % 2 == 0 else nc.vector).tensor_tensor(
                        out=KV[kb_][:, :].rearrange("p (r h d) -> p r h d", r=16, h=NH),
                        in0=KV[kb_][:, :].rearrange("p (r h d) -> p r h d", r=16, h=NH),
                        in1=Pm[:, c * 16:(c + 1) * 16, :].rearrange("p r (h o) -> p r h o", o=1).broadcast_to([128, 16, NH, HD]),
                        op=ALU.mult))
                    Vv([KVb[kb_]], [redb], lambda: nc.vector.tensor_reduce(
                        out=red[:, :], in_=KV[kb_][:, :].rearrange("p (r e) -> p e r", r=16), axis=AX.X, op=ALU.add))
                    Vv([redb], [Oab], lambda: nc.vector.tensor_tensor(out=Oacc[:], in0=Oacc[:], in1=red[:], op=ALU.add))
                    yield
                mm_group(ps[3][0:4, :], [(pairsel[:, t * 4:(t + 1) * 4], Oacc[:, :])], [scb, Oab], pb[3])
                if t == 0:
                    Vv([pb[3]], [Osb], lambda: nc.vector.tensor_copy(out=Osum[:], in_=ps[3][0:4, :]))
                else:
                    Vv([pb[3]], [Osb], lambda: nc.vector.tensor_tensor(out=Osum[:], in0=Osum[:], in1=ps[3][0:4, :], op=ALU.add))
                mm_group(ps[4][0:4, 0:NH], [(pairsel[:, t * 4:(t + 1) * 4], den[:, :])], [scb, denb], pb[4])
                if t == 0:
                    Vv([pb[4]], [snb], lambda: nc.vector.tensor_copy(out=snew[:, 2, :], in_=ps[4][0:4, 0:NH]))
                else:
                    Vv([pb[4]], [snb], lambda: nc.vector.tensor_tensor(out=snew[:, 2, :], in0=snew[:, 2, :], in1=ps[4][0:4, 0:NH],
                                                                      op=ALU.add))
                yield
            Vv([qtb, ktb], [tmp4b], lambda: nc.vector.tensor_tensor(out=tmp4[:], in0=q_tok[:], in1=k_tok[:], op=ALU.mult))
            Vv([tmp4b], [snb], lambda: nc.vector.tensor_reduce(out=snew[:, 0, :], in_=tmp4[:, :].rearrange("p (h d) -> p h d", h=NH),
                                                              axis=AX.X, op=ALU.add))
            Aa([snb], [snb], lambda: nc.scalar.activation(out=snew[:, 1, :], in_=snew[:, 0, :], func=AF.Exp, scale=0.125))
            Vv([snb, vtb], [tmp4b], lambda: nc.vector.tensor_tensor(
                out=tmp4[:, :].rearrange("p (h d) -> p h d", h=NH), in0=v_tok[:, :].rearrange("p (h d) -> p h d", h=NH),
                in1=snew[:, 1, :].rearrange("p (h o) -> p h o", o=1).broadcast_to([4, NH, HD]), op=ALU.mult))
            Vv([tmp4b], [Osb], lambda: nc.vector.tensor_tensor(out=Osum[:], in0=Osum[:], in1=tmp4[:], op=ALU.add))
            Vv([snb], [snb], lambda: nc.vector.tensor_tensor(out=snew[:, 2, :], in0=snew[:, 2, :], in1=snew[:, 1, :],
                                                            op=ALU.add))
            Vv([snb], [snb], lambda: nc.vector.reciprocal(out=snew[:, 2, :], in_=snew[:, 2, :]))
            Vv([Osb, snb], [atkb], lambda: nc.vector.tensor_tensor(
                out=attn_tok[:, :].rearrange("p (h d) -> p h d", h=NH), in0=Osum[:, :].rearrange("p (h d) -> p h d", h=NH),
                in1=snew[:, 2, :].rearrange("p (h o) -> p h o", o=1).broadcast_to([4, NH, HD]), op=ALU.mult))
            for h in range(NH):
                transpose(ps[7][0:64, h * 4:(h + 1) * 4], attn_tok[0:4, h * 64:(h + 1) * 64], ident[0:4, 0:4],
                          [atkb, cB], pb[7])
            Aa([pb[7]], [atsb], lambda: nc.scalar.copy(out=attnT_s[:, :, :],
                                                      in_=ps[7][0:64, 0:32].rearrange("p (h t) -> p h t", h=NH)))
            yield

        sgen = phase_S()

        def s_step():
            next(sgen, None)

        with ExitStack() as s1:
            def sb1(name, shape, dt=F32):
                return s1.enter_context(nc.sbuf_tensor("p1_" + name, list(shape), dt))
            wk = sb1("wk", [128, 8, 512], BF16)
            wkr = sb1("wkr", [128, 8, 512], BF16)
            wv = sb1("wv", [128, 8, 512], BF16)
            wB = Buf()
            for (t_, c0, srcw) in ((wk, 512, s_win), (wkr, 512, s_wrot), (wv, 1024, s_win)):
                cx.dma("sp", t_[:], srcw[:, c0:c0 + 512].rearrange("(kc p) j -> p kc j", p=128),
                       reads=[convs[srcw.tensor.name]], writes=[wB], stream="w")
            cs_t = sb1("cs_t", [128, G])
            sn_t = sb1("sn_t", [128, G])
            csB = Buf()
            kT_f = sb1("kT_f", [128, G])
            kT_fb = Buf()
            kT_h = sb1("kT_h", [128, G], BF16)
            kT_hb = Buf()
            t1 = sb1("t1", [128, G])
            t1b = Buf()
            ktok = sb1("ktok", [128, 512])
            ktokb = Buf()
            vtok = sb1("vtok", [128, 512])
            vtokb = Buf()
            vaug = sb1("vaug", [128, NH, 4, 66], BF16)
            vaugb = Buf()
            Pp([], [vaugb], lambda: nc.gpsimd.memset(vaug[:], 1.0))

            for g in range(NG):
                load_xT(xseq, g * G, G, None, xT_b, None, xb_buf, xt_tiles, xt_bufs, 0, 1)
                cx.dma("sp", cs_t[:], cosk[:, g * G:(g + 1) * G], writes=[csB], stream="c")
                cx.dma("sp", sn_t[:], sink[:, g * G:(g + 1) * G], writes=[csB], stream="c")
                for c in range(4):
                    mm_group(ps[2][:, :], [(wk[:, kc, c * 128:(c + 1) * 128], xT_b[:, kc, :]) for kc in range(8)],
                             [wB, xb_buf], pb[2])
                    mm_group(ps[3][:, :], [(wkr[:, kc, c * 128:(c + 1) * 128], xT_b[:, kc, :]) for kc in range(8)],
                             [wB, xb_buf], pb[3])
                    Vv([pb[2], csB], [t1b],
                       lambda: nc.vector.tensor_tensor(out=t1[:], in0=ps[2][:, :], in1=cs_t[:], op=ALU.mult))
                    Vv([pb[3], csB], [kT_fb],
                       lambda: nc.vector.tensor_tensor(out=kT_f[:], in0=ps[3][:, :], in1=sn_t[:], op=ALU.mult))
                    Vv([t1b], [kT_fb],
                       lambda: nc.vector.tensor_tensor(out=kT_f[:], in0=kT_f[:], in1=t1[:], op=ALU.add))
                    Aa([kT_fb], [kT_hb], lambda: nc.scalar.copy(out=kT_h[:], in_=kT_f[:]))
                    cx.dma("sp", s_kT[2 * c:2 * c + 2, :, g * G:(g + 1) * G].rearrange("h d t -> (h d) t"), kT_h[:],
                           reads=[kT_hb], stream="ks")
                    Vv([kT_fb], [kmB],
                       lambda: nc.vector.tensor_reduce(out=kmT[:, c, 2 * g:2 * g + 2],
                                                       in_=kT_f[:, :].rearrange("p (b t) -> p b t", b=2),
                                                       axis=AX.X, op=ALU.add))
                    for t in range(4):
                        transpose(ps[4][:, t * 128:(t + 1) * 128], kT_f[:, t * 128:(t + 1) * 128], ident[:],
                                  [kT_fb, cB], pb[4])
                    Aa([pb[4]], [ktokb], lambda: nc.scalar.copy(out=ktok[:, :], in_=ps[4][:, :]))
                    cx.dma("sp", k_out[g * G:(g + 1) * G, c * 128:(c + 1) * 128].rearrange("(t p) j -> p t j", p=128),
                           ktok[:, :].rearrange("p (t j) -> p t j", t=4), reads=[ktokb], stream="ko")
                    s_step()
                for t in range(4):
                    mm_group(ps[5][:, :], [(xT_b[:, kc, t * 128:(t + 1) * 128], wv[:, kc, :]) for kc in range(8)],
                             [wB, xb_buf], pb[5])
                    Aa([pb[5]], [vtokb], lambda: nc.scalar.copy(out=vtok[:], in_=ps[5][:, :]))
                    Vv([vtokb], [vaugb],
                       lambda: nc.vector.tensor_copy(out=vaug[:, :, t, 0:64],
                                                     in_=vtok[:, :].rearrange("p (h d) -> p h d", h=NH)))
                    cx.dma("sp", v_out[g * G + t * 128: g * G + (t + 1) * 128, :], vtok[:], reads=[vtokb], stream="vo")
                    s_step()
                for h in range(NH):
                    cx.dma("sp", s_v[h, :, g * 4:(g + 1) * 4, :], vaug[:, h, :, :], reads=[vaugb], stream="vs")
            Vv([], [kmB], lambda: nc.vector.memset(kmbd[:], 0.0))
            Vv([kmB], [kmB], lambda: nc.vector.tensor_scalar(out=kmbd[0:64, :, 0:16], in0=kmT[0:64, :, :],
                                                             scalar1=1.0 / 256, scalar2=None, op0=ALU.mult))
            Vv([kmB], [kmB], lambda: nc.vector.tensor_scalar(out=kmbd[64:128, :, 16:32], in0=kmT[64:128, :, :],
                                                             scalar1=1.0 / 256, scalar2=None, op0=ALU.mult))
            for _ in sgen:
                pass
            barrier()
        barrier()
    if STOP == 2:
        cx.drain("sp")
        return

    if STOP == 1:
        cx.drain("sp")
        return

    def layer_norm(zT, zb, gcol, bcol, outf, outfb, outb, outbb, tmp, tmpb, mean_sb, rstd_sb, stb, T):
        for oc in range(8):
            Aa([zb], [tmpb], lambda: nc.scalar.activation(out=tmp[:, 0:T], in_=zT[:, oc, 0:T], func=AF.Square))
            cx.deps("pe", [zb, cB], [pb[6]] if oc == 0 else [])
            ins = nc.tensor.matmul(ps[6][:, 0:T], ones_ln[:], zT[:, oc, 0:T], start=(oc == 0), stop=(oc == 7))
            cx.op("pe", ins, [zb, cB], [pb[6]] if oc == 7 else [])
            cx.deps("pe", [tmpb], [pb[7]] if oc == 0 else [])
            ins = nc.tensor.matmul(ps[7][:, 0:T], ones_ln[:], tmp[:, 0:T], start=(oc == 0), stop=(oc == 7))
            cx.op("pe", ins, [tmpb], [pb[7]] if oc == 7 else [])
        Aa([pb[6]], [stb], lambda: nc.scalar.copy(out=mean_sb[:, 0:T], in_=ps[6][:, 0:T]))
        Vv([stb], [tmpb], lambda: nc.vector.tensor_tensor(out=tmp[:, 0:T], in0=mean_sb[:, 0:T], in1=mean_sb[:, 0:T],
                                                         op=ALU.mult))
        Vv([pb[7], tmpb], [tmpb], lambda: nc.vector.tensor_tensor(out=tmp[:, 0:T], in0=ps[7][:, 0:T], in1=tmp[:, 0:T],
                                                                 op=ALU.subtract))
        Aa([tmpb], [tmpb], lambda: nc.scalar.activation(out=tmp[:, 0:T], in_=tmp[:, 0:T], func=AF.Sqrt, bias=eps_t[:, 0:1]))
        Vv([tmpb], [stb], lambda: nc.vector.reciprocal(out=rstd_sb[:, 0:T], in_=tmp[:, 0:T]))
        for oc in range(8):
            Vv([zb, stb], [tmpb], lambda: nc.vector.tensor_tensor(out=tmp[:, 0:T], in0=zT[:, oc, 0:T], in1=mean_sb[:, 0:T],
                                                                 op=ALU.subtract))
            Vv([stb], [tmpb], lambda: nc.vector.tensor_tensor(out=tmp[:, 0:T], in0=tmp[:, 0:T], in1=rstd_sb[:, 0:T],
                                                             op=ALU.mult))
            Vv([tmpb, cB], [outfb], lambda: nc.vector.tensor_scalar(out=outf[:, oc, 0:T], in0=tmp[:, 0:T],
                                                                   scalar1=fp[:, gcol + oc:gcol + oc + 1],
                                                                   scalar2=fp[:, bcol + oc:bcol + oc + 1],
                                                                   op0=ALU.mult, op1=ALU.add))
            if outb is not None:
                Aa([outfb], [outbb], lambda: nc.scalar.copy(out=outb[:, oc, 0:T], in_=outf[:, oc, 0:T]))

    eps_t = sb("eps_t", [128, 1])
    Vv([], [cB], lambda: nc.vector.memset(eps_t[:], LN_EPS))
    x1T_f = sb("x1T_f", [128, 8, G])
    x1fb = Buf()
    x1T_b = sb("x1T_b", [128, 8, G], BF16)
    x1bb = Buf()
    tmp = sb("ln_tmp", [128, G])
    tmpb = Buf()
    mean_sb = sb("mean_sb", [128, G])
    rstd_sb = sb("rstd_sb", [128, G])
    stb = Buf()

    def moe_and_out(sbb, row0, T, y_dst):
        nt = min(128, T)
        ntile = T // nt
        Lg = sbb("Lg", [128, 20])
        rs = sbb("rs", [128, 64])
        esel = sbb("esel", [128, 8])
        top8e = sbb("top8e", [128, 8])
        comb = sbb("comb", [128, 16])
        rb = Buf()
        combT = sbb("combT", [16, G])
        cTb = Buf()
        bce = sbb("bce", [128, G])
        bceb = Buf()
        wg = [sbb(f"wg{i}", [128, 8, DE], BF16) for i in range(3)]
        wu = [sbb(f"wu{i}", [128, 8, DE], BF16) for i in range(3)]
        wgb = [Buf(), Buf(), Buf()]
        wub = [Buf(), Buf(), Buf()]
        silt = [sbb(f"silt{i}", [128, G]) for i in range(2)]
        siltb = [Buf(), Buf()]
        hT = sbb("hT", [128, 32, G], BF16)
        hTb = Buf()
        wdblk2 = [sbb(f"wdblk{i}", [128, 32, 512], BF16) for i in range(2)]
        wdb2 = [Buf(), Buf()]
        for half in range(2):
            cx.dma("sp", wdblk2[half][:], s_ed[:, half * 512:(half + 1) * 512].rearrange("(j p) o -> p j o", p=128),
                   reads=[convs["s_ed"]], writes=[wdb2[half]], stream="wd")
        otile = sbb("otile", [128, D])
        otb = Buf()

        Vv([], [rb], lambda: nc.vector.memset(esel[:], NEG))
        for t in range(ntile):
            ts_ = slice(t * nt, (t + 1) * nt)
            P_ = slice(0, nt)
            cx.deps("pe", [x1fb, cB], [pb[0]])
            ins = None
            for kc in range(8):
                ins = nc.tensor.matmul(ps[0][P_, 0:20], x1T_f[:, kc, ts_], wr_sb[:, kc * 20:(kc + 1) * 20],
                                       start=(kc == 0), stop=(kc == 7))
            cx.op("pe", ins, [x1fb, cB], [pb[0]])
            Vv([pb[0], cB], [rb], lambda: nc.vector.tensor_tensor(out=Lg[P_, :], in0=ps[0][P_, 0:20], in1=br_sb[P_, :],
                                                                 op=ALU.add))
            Vv([rb], [rb], lambda: nc.vector.tensor_reduce(out=rs[P_, 0:1], in_=Lg[P_, 0:4], axis=AX.X, op=ALU.max))
            Vv([rb], [rb], lambda: nc.vector.tensor_scalar(out=rs[P_, 4:8], in0=Lg[P_, 0:4], scalar1=rs[P_, 0:1],
                                                          scalar2=None, op0=ALU.is_ge))
            Vv([rb], [rb], lambda: nc.vector.tensor_scalar(out=rs[P_, 1:2], in0=rs[P_, 0:1], scalar1=-1.0,
                                                          scalar2=None, op0=ALU.mult))
            Aa([rb], [rb], lambda: nc.scalar.activation(out=rs[P_, 8:12], in_=Lg[P_, 0:4], func=AF.Exp,
                                                       bias=rs[P_, 1:2], accum_out=rs[P_, 2:3]))
            Vv([rb], [rb], lambda: nc.vector.reciprocal(out=rs[P_, 3:4], in_=rs[P_, 2:3]))
            Vv([rb], [rb], lambda: nc.vector.tensor_scalar(out=esel[P_, 0:4], in0=Lg[P_, 4:8], scalar1=rs[P_, 4:5],
                                                          scalar2=None, op0=ALU.mult))
            for g4 in range(1, 4):
                Vv([rb], [rb], lambda: nc.vector.scalar_tensor_tensor(out=esel[P_, 0:4], in0=Lg[P_, 4 + 4 * g4:8 + 4 * g4],
                                                                     scalar=rs[P_, 4 + g4:5 + g4], in1=esel[P_, 0:4],
                                                                     op0=ALU.mult, op1=ALU.add))
            Vv([rb], [rb], lambda: nc.vector.max(out=top8e[P_, :], in_=esel[P_, :]))
            Vv([rb], [rb], lambda: nc.vector.tensor_tensor(out=rs[P_, 12:13], in0=top8e[P_, 0:1], in1=top8e[P_, 1:2],
                                                          op=ALU.subtract))
            Aa([rb], [rb], lambda: nc.scalar.activation(out=rs[P_, 13:14], in_=rs[P_, 12:13], func=AF.Sigmoid))
            Vv([rb], [rb], lambda: nc.vector.tensor_tensor(out=rs[P_, 14:15], in0=rs[P_, 13:14], in1=rs[P_, 3:4],
                                                          op=ALU.mult))
            Vv([rb], [rb], lambda: nc.vector.tensor_tensor(out=rs[P_, 15:16], in0=rs[P_, 3:4], in1=rs[P_, 14:15],
                                                          op=ALU.subtract))
            Vv([rb], [rb], lambda: nc.vector.tensor_tensor(out=rs[P_, 16:17], in0=rs[P_, 14:15], in1=rs[P_, 15:16],
                                                          op=ALU.subtract))
            Vv([rb], [rb], lambda: nc.vector.tensor_scalar(out=rs[P_, 20:24], in0=esel[P_, 0:4], scalar1=top8e[P_, 0:1],
                                                          scalar2=None, op0=ALU.is_ge))
            Vv([rb], [rb], lambda: nc.vector.tensor_scalar(out=rs[P_, 24:28], in0=esel[P_, 0:4], scalar1=top8e[P_, 1:2],
                                                          scalar2=None, op0=ALU.is_ge))
            Vv([rb], [rb], lambda: nc.vector.tensor_scalar(out=rs[P_, 28:32], in0=rs[P_, 24:28], scalar1=rs[P_, 15:16],
                                                          scalar2=None, op0=ALU.mult))
            Vv([rb], [rb], lambda: nc.vector.scalar_tensor_tensor(out=rs[P_, 28:32], in0=rs[P_, 20:24],
                                                                 scalar=rs[P_, 16:17], in1=rs[P_, 28:32],
                                                                 op0=ALU.mult, op1=ALU.add))
            for g4 in range(4):
                Vv([rb], [rb], lambda: nc.vector.tensor_scalar(out=comb[P_, 4 * g4:4 * g4 + 4], in0=rs[P_, 28:32],
                                                              scalar1=rs[P_, 4 + g4:5 + g4], scalar2=None,
                                                              op0=ALU.mult))
            transpose(ps[1][0:16, ts_], comb[P_, :], ident[P_, P_], [rb, cB], pb[1])
        Aa([pb[1]], [cTb], lambda: nc.scalar.copy(out=combT[:, 0:T], in_=ps[1][0:16, 0:T]))

        for e in range(NE):
            i2 = e % 3
            cx.dma("sp", wg[i2][:], s_eg[e * D:(e + 1) * D, :].rearrange("(kc p) f -> p kc f", p=128),
                   reads=[convs["s_eg"]], writes=[wgb[i2]], stream="we")
            cx.dma("sp", wu[i2][:], s_eu[e * D:(e + 1) * D, :].rearrange("(kc p) f -> p kc f", p=128),
                   reads=[convs["s_eu"]], writes=[wub[i2]], stream="we")
            mm_group(ps[2][:, 0:T], [(sel16[0:16, e * 128:(e + 1) * 128], combT[0:16, 0:T])], [cTb, cB], pb[2])
            Aa([pb[2]], [bceb], lambda: nc.scalar.copy(out=bce[:, 0:T], in_=ps[2][:, 0:T]))
            for fc in range(2):
                j = e * 2 + fc
                pg, pu, sj = 3 + j % 2, 5 + j % 2, j % 2
                fs = slice(fc * 128, (fc + 1) * 128)
                mm_group(ps[pg][:, 0:T], [(wg[i2][:, kc, fs], x1T_b[:, kc, 0:T]) for kc in range(8)],
                         [wgb[i2], x1bb], pb[pg])
                mm_group(ps[pu][:, 0:T], [(wu[i2][:, kc, fs], x1T_b[:, kc, 0:T]) for kc in range(8)],
                         [wub[i2], x1bb], pb[pu])
                Aa([pb[pg]], [siltb[sj]], lambda: nc.scalar.activation(out=silt[sj][:, 0:T], in_=ps[pg][:, 0:T],
                                                                      func=AF.Silu))
                Pp([bceb], [siltb[sj]], lambda: nc.gpsimd.tensor_tensor(out=silt[sj][:, 0:T], in0=silt[sj][:, 0:T],
                                                                       in1=bce[:, 0:T], op=ALU.mult))
                Vv([pb[pu], siltb[sj]], [hTb], lambda: nc.vector.tensor_tensor(out=hT[:, j, 0:T], in0=ps[pu][:, 0:T],
                                                                              in1=silt[sj][:, 0:T], op=ALU.mult))
        for half in range(2):
            wdblk, wdb = wdblk2[half], wdb2[half]
            for o4 in range(4):
                oc = half * 4 + o4
                pi = oc % 2
                mm_group(ps[pi][:, 0:T], [(wdblk[:, j, o4 * 128:(o4 + 1) * 128], hT[:, j, 0:T]) for j in range(32)],
                         [wdb, hTb], pb[pi])
                Vv([pb[pi], x1fb], [x1fb],
                   lambda: nc.vector.scalar_tensor_tensor(out=x1T_f[:, oc, 0:T], in0=x1T_f[:, oc, 0:T], scalar=ALPHA,
                                                          in1=ps[pi][:, 0:T], op0=ALU.mult, op1=ALU.add))
        layer_norm(x1T_f, x1fb, 16, 24, x1T_f, x1fb, None, None, tmp, tmpb, mean_sb, rstd_sb, stb, T)
        for t in range(ntile):
            ts_ = slice(t * nt, (t + 1) * nt)
            for half in range(2):
                pi = 3 + half
                for j in range(4):
                    oc = half * 4 + j
                    transpose(ps[pi][0:nt, j * 128:(j + 1) * 128], x1T_f[:, oc, ts_], ident[:], [x1fb, cB], pb[pi])
                if half == 0:
                    Aa([pb[pi]], [otb], lambda: nc.scalar.copy(out=otile[0:nt, 0:512], in_=ps[pi][0:nt, :]))
                else:
                    Vv([pb[pi]], [otb], lambda: nc.vector.tensor_copy(out=otile[0:nt, 512:1024], in_=ps[pi][0:nt, :]))
            cx.dma("sp", y_dst[row0 + t * nt: row0 + (t + 1) * nt, :], otile[0:nt, :], reads=[otb], stream="yo")

    def merge_ln1(sba, T, xT_f, xfb, xTb, xb_buf, attnT, atb, pooledT, plb):
        wblk = [sba(f"m_wblk{i}", [128, 8, 512], BF16) for i in range(2)]
        wblkb = [Buf(), Buf()]
        wa_t = sba("wa_t", [64, NH, 512], BF16)
        wab = Buf()
        wb_t = sba("wb_t", [128, 4, 512], BF16)
        wbb = Buf()
        sga = sba("m_sga", [128, T])
        sgab = Buf()
        sgb = sba("m_sgb", [128, T])
        sgbb = Buf()
        t1 = sba("m_t1", [128, T])
        t1b = Buf()
        t2 = sba("m_t2", [128, T])
        t2b = Buf()
        mergedT = sba("mergedT", [128, 8, T], BF16)
        mgb = Buf()
        for half in range(2):
            load_wblk(wblk[0], wblkb[0], s_win, 2048 + half * 512)
            load_wblk(wblk[1], wblkb[1], s_win, 3072 + half * 512)
            cx.dma("sp", wa_t[:], s_wa[:, half * 512:(half + 1) * 512].rearrange("(h d) o -> d h o", h=NH),
                   reads=[convs["s_wa"]], writes=[wab], stream="w")
            cx.dma("sp", wb_t[:], s_wb[:, half * 512:(half + 1) * 512].rearrange("(g c) o -> c g o", g=4),
                   reads=[convs["s_wb"]], writes=[wbb], stream="w")
            for o4 in range(4):
                oc = half * 4 + o4
                cs_ = slice(o4 * 128, (o4 + 1) * 128)
                mm_group(ps[2][:, 0:T], [(wblk[0][:, kc, cs_], xTb[:, kc, 0:T]) for kc in range(8)],
                         [wblkb[0], xb_buf], pb[2])
                mm_group(ps[3][:, 0:T], [(wblk[1][:, kc, cs_], xTb[:, kc, 0:T]) for kc in range(8)],
                         [wblkb[1], xb_buf], pb[3])
                Aa([pb[2], cB], [sgab], lambda: nc.scalar.activation(out=sga[:], in_=ps[2][:, 0:T], func=AF.Sigmoid,
                                                                    bias=fp[:, 32 + oc:33 + oc]))
                Aa([pb[3], cB], [sgbb], lambda: nc.scalar.activation(out=sgb[:], in_=ps[3][:, 0:T], func=AF.Sigmoid,
                                                                    bias=fp[:, 40 + oc:41 + oc]))
                mm_group(ps[0][:, 0:T], [(wa_t[0:64, h, cs_], attnT[0:64, h, 0:T]) for h in range(NH)],
                         [wab, atb], pb[0])
                mm_group(ps[1][:, 0:T], [(wb_t[:, g4, cs_], pooledT[:, g4, 0:T]) for g4 in range(4)],
                         [wbb, plb], pb[1])
                Vv([pb[0], sgab], [t1b], lambda: nc.vector.tensor_tensor(out=t1[:], in0=ps[0][:, 0:T], in1=sga[:],
                                                                        op=ALU.mult))
                Vv([pb[1], sgbb], [t2b], lambda: nc.vector.tensor_tensor(out=t2[:], in0=ps[1][:, 0:T], in1=sgb[:],
                                                                        op=ALU.mult))
                Vv([t1b, t2b], [mgb], lambda: nc.vector.tensor_tensor(out=mergedT[:, oc, :], in0=t1[:], in1=t2[:],
                                                                     op=ALU.add))
        for half in range(2):
            load_wblk(wblk[half], wblkb[half], s_wout, half * 512)
            for o4 in range(4):
                oc = half * 4 + o4
                pi = 2 + (oc % 2)
                mm_group(ps[pi][:, 0:T], [(wblk[half][:, kc, o4 * 128:(o4 + 1) * 128], mergedT[:, kc, :])
                                          for kc in range(8)], [wblkb[half], mgb], pb[pi])
                Vv([pb[pi], xfb], [xfb],
                   lambda: nc.vector.scalar_tensor_tensor(out=xT_f[:, oc, 0:T], in0=xT_f[:, oc, 0:T], scalar=ALPHA,
                                                          in1=ps[pi][:, 0:T], op0=ALU.mult, op1=ALU.add))
        layer_norm(xT_f, xfb, 0, 8, x1T_f, x1fb, x1T_b, x1bb, tmp, tmpb, mean_sb, rstd_sb, stb, T)

    for slot in range(NSLOT):
        cx.epoch()
        nkt = 8 * (slot + 1)
        with ExitStack() as sA:
            def sba(name, shape, dt=F32):
                return sA.enter_context(nc.sbuf_tensor(f"a{slot}_" + name, list(shape), dt))
            xT_f = sba("xT_f", [128, 8, G])
            xfb = Buf()
            attnT = sba("attnT", [64, NH, G], BF16)
            atb = Buf()
            pooledT = sba("pooledT", [128, 4, G], BF16)
            plb = Buf()
            sA1 = ExitStack()

            def sba1(name, shape, dt=F32):
                return sA1.enter_context(nc.sbuf_tensor(f"a1{slot}_" + name, list(shape), dt))
            wblk = [sba1(f"wblk{i}", [128, 8, 512], BF16) for i in range(2)]
            wblkb = [Buf(), Buf()]
            cq_t = sba1("cq_t", [128, G])
            sq_t = sba1("sq_t", [128, G])
            cqb = Buf()
            qT_f = sba1("qT_f", [128, 4, G])
            qfb = Buf()
            qaug = sba1("qaug", [80, NH, G], BF16)
            qab = Buf()
            t1 = sba1("t1", [128, G])
            t1b = Buf()
            kaug = [sba1(f"kaug{i}", [80, SEQ], BF16) for i in range(2)]
            kab = [Buf(), Buf()]
            vh = [sba1(f"vh{i}", [128, 32, 66], BF16) for i in range(2)]
            vhb = [Buf(), Buf()]
            pT = [sba1(f"pT{i}", [128, G], BF16) for i in range(4)]
            pTb = [Buf(), Buf(), Buf(), Buf()]
            cmk = sba1("cmk", [128, 8, G], BF16)
            cmkb = Buf()
            rden = sba1("rden", [65, G])
            rdb = Buf()
            rden0 = sba1("rden0", [1, G])
            rd0b = Buf()
            bcs = sba1("bcs", [64, G])
            bcsb = Buf()
            gm = sba1("gm", [128, 128])
            gmb = Buf()
            msk = sba1("msk", [128, 3, 128])
            mskb = Buf()
            top8 = sba1("top8", [128, NH, 8])
            top8b = Buf()
            sel = sba1("sel", [128, 128])
            selb = Buf()
            biasw = sba1("biasw", [128, NH, 32])
            bwb = Buf()
            uT = sba1("uT", [128, 4, 16 + G])
            uTb = Buf()
            xh = sba1("xh", [16, D])
            xhb = Buf()
            xhT = sba1("xhT", [128, 8, 16], BF16)
            xhTb = Buf()
            pa = sba1("pa", [128, 16 + G])
            pab = Buf()
            pbt = sba1("pbt", [128, 16 + G])
            pbb = Buf()
            invt = sba1("invt", [128, G])
            invb = Buf()
            diffT = sba1("diffT", [128, G], BF16)
            dfb = Buf()
            wpool_t = sba1("wpool_t", [128, 4, 128], BF16)
            wpb = Buf()
            sga = sba1("sga", [128, G])
            sgab = Buf()

            row0 = slot * G
            load_xT(xown, row0, G, xT_f, xT_b, xfb, xb_buf, xt_tiles, xt_bufs, 0, 1)
            cx.dma("sp", cq_t[:], cosq[:, row0:row0 + G], writes=[cqb], stream="c")
            cx.dma("sp", sq_t[:], sinq[:, row0:row0 + G], writes=[cqb], stream="c")
            cx.dma("sp", cmk[:], cmask[slot % 2, :, :].rearrange("p (j t) -> p j t", j=8), writes=[cmkb], stream="c")
            Vv([], [bwb], lambda: nc.vector.memset(biasw[:], 0.0))
            Vv([], [pab], lambda: nc.vector.memset(pa[:], 0.0))
            Vv([], [pbb], lambda: nc.vector.memset(pbt[:], 0.0))

            load_wblk(wblk[0], wblkb[0], s_win, 0)
            load_wblk(wblk[1], wblkb[1], s_wrot, 0)
            for c in range(4):
                mm_group(ps[2][:, :], [(wblk[0][:, kc, c * 128:(c + 1) * 128], xT_b[:, kc, :]) for kc in range(8)],
                         [wblkb[0], xb_buf], pb[2])
                mm_group(ps[3][:, :], [(wblk[1][:, kc, c * 128:(c + 1) * 128], xT_b[:, kc, :]) for kc in range(8)],
                         [wblkb[1], xb_buf], pb[3])
                Vv([pb[2], cqb], [t1b],
                   lambda: nc.vector.tensor_tensor(out=t1[:], in0=ps[2][:, :], in1=cq_t[:], op=ALU.mult))
                Vv([pb[3], cqb], [qfb],
                   lambda: nc.vector.tensor_tensor(out=qT_f[:, c, :], in0=ps[3][:, :], in1=sq_t[:], op=ALU.mult))
                Vv([t1b], [qfb],
                   lambda: nc.vector.tensor_tensor(out=qT_f[:, c, :], in0=qT_f[:, c, :], in1=t1[:], op=ALU.add))
                Aa([qfb], [qab], lambda: nc.scalar.copy(out=qaug[0:64, 2 * c, :], in_=qT_f[0:64, c, :]))
                Aa([qfb], [qab], lambda: nc.scalar.copy(out=qaug[0:64, 2 * c + 1, :], in_=qT_f[64:128, c, :]))

            for t in range(4):
                tr0 = row0 + t * 128
                cx.dma("sp", msk[:, 0, :], negm[tr0:tr0 + 128, :], writes=[mskb], stream="m")
                cx.dma("sp", msk[:, 1, :], valm[tr0:tr0 + 128, :], writes=[mskb], stream="m")
                cx.dma("sp", msk[:, 2, :], ownm[tr0:tr0 + 128, :], writes=[mskb], stream="m")
                cx.deps("pe", [qfb, kmB], [pb[4]])
                ins = None
                for c in range(4):
                    ins = nc.tensor.matmul(ps[4][:, c * 32:(c + 1) * 32], qT_f[:, c, t * 128:(t + 1) * 128],
                                           kmbd[:, c, :], start=True, stop=True)
                cx.op("pe", ins, [qfb, kmB], [pb[4]])
                Vv([pb[4], mskb], [gmb],
                   lambda: nc.vector.tensor_tensor(out=gm[:], in0=ps[4][:, 0:128], in1=msk[:, 0, :], op=ALU.add))
                for h in range(NH):
                    Vv([gmb], [top8b], lambda: nc.vector.max(out=top8[:, h, :], in_=gm[:, h * 16:(h + 1) * 16]))
                for h in range(NH):
                    Vv([gmb, top8b], [selb],
                       lambda: nc.vector.tensor_scalar(out=sel[:, h * 16:(h + 1) * 16], in0=gm[:, h * 16:(h + 1) * 16],
                                                       scalar1=top8[:, h, 2:3], scalar2=None, op0=ALU.is_ge))
                Vv([selb, mskb], [selb],
                   lambda: nc.vector.tensor_tensor(out=sel[:], in0=sel[:], in1=msk[:, 1, :], op=ALU.mult))
                Vv([selb, mskb], [selb],
                   lambda: nc.vector.tensor_tensor(out=sel[:], in0=sel[:], in1=msk[:, 2, :], op=ALU.add))
                Vv([selb], [bwb],
                   lambda: nc.vector.tensor_scalar(out=biasw[:, :, 0:16],
                                                   in0=sel[:, :].rearrange("p (h n) -> p h n", h=NH),
                                                   scalar1=BIG, scalar2=-BIG, op0=ALU.mult, op1=ALU.add))
                for b2 in range(2):
                    transpose(ps[5][:, b2 * 128:(b2 + 1) * 128],
                              biasw[:, b2 * 4:(b2 + 1) * 4, :].rearrange("p h n -> p (h n)"), ident[:],
                              [bwb, cB], pb[5])
                for h in range(NH):
                    p0 = (h % 4) * 32
                    c0 = (h // 4) * 128
                    Aa([pb[5]], [qab], lambda: nc.scalar.copy(out=qaug[64:80, h, t * 128:(t + 1) * 128],
                                                             in_=ps[5][p0:p0 + 16, c0:c0 + 128]))

            nkeys = nkt * 128
            LOOK = 3
            sbanks = [6, 7, 0, 1]

            def epilogue(h_):
                ob = 4 + h_ % 2
                Vv([pb[ob]], [rdb], lambda: nc.vector.reciprocal(out=rden[64:65, :], in_=ps[ob][64:65, :]))
                Vv([rdb], [rd0b], lambda: nc.vector.tensor_copy(out=rden0[0:1, :], in_=rden[64:65, :]))
                mm_group(ps[2][0:64, :], [(ones1[0:1, 0:64], rden0[0:1, :])], [rd0b, cB], pb[2])
                Vv([pb[2]], [bcsb], lambda: nc.vector.tensor_copy(out=bcs[:], in_=ps[2][0:64, :]))
                Vv([pb[ob], bcsb], [atb],
                   lambda: nc.vector.tensor_tensor(out=attnT[0:64, h_, :], in0=ps[ob][0:64, :], in1=bcs[:], op=ALU.mult))

            pending = None
            for h in range(NH):
                kb_ = h % 2
                ob = 4 + h % 2
                cx.dma("sp", kaug[kb_][0:64, 0:nkeys], s_kT[h, :, 0:nkeys], writes=[kab[kb_]], stream="ka")
                cx.dma("sp", kaug[kb_][64:80, 0:nkeys], blkind[:, 0:nkeys], writes=[kab[kb_]], stream="ka")
                cx.dma("sp", vh[kb_][:, 0:nkt, :], s_v[h, :, 0:nkt, :], writes=[vhb[kb_]], stream="va")

                def issue_S(kt_):
                    bk = sbanks[kt_ % 4]
                    mm_group(ps[bk][:, :], [(kaug[kb_][0:80, kt_ * 128:(kt_ + 1) * 128], qaug[0:80, h, :])],
                             [kab[kb_], qab], pb[bk])

                for kt in range(min(LOOK, nkt)):
                    issue_S(kt)
                for kt in range(nkt):
                    if kt + LOOK < nkt:
                        issue_S(kt + LOOK)
                    sbk = sbanks[kt % 4]
                    pj = kt % 4
                    Aa([pb[sbk]], [pTb[pj]],
                       lambda: nc.scalar.activation(out=pT[pj][:], in_=ps[sbk][:, :], func=AF.Exp, scale=0.125))
                    if kt >= nkt - 8:
                        jj = kt - (nkt - 8)
                        Pp([cmkb], [pTb[pj]],
                           lambda: nc.gpsimd.tensor_tensor(out=pT[pj][:], in0=pT[pj][:], in1=cmk[:, jj, :], op=ALU.mult))
                    cx.deps("pe", [vhb[kb_], pTb[pj]], [pb[ob]] if kt == 0 else [])
                    ins = nc.tensor.matmul(ps[ob][0:65, :], vh[kb_][:, kt, 0:65], pT[pj][:], start=(kt == 0),
                                           stop=(kt == nkt - 1))
                    cx.op("pe", ins, [vhb[kb_], pTb[pj]], [pb[ob]] if kt == nkt - 1 else [])
                if pending is not None:
                    epilogue(pending)
                pending = h
            epilogue(pending)

            cx.dma("sp", xh[:], xhalo[slot * 16:(slot + 1) * 16, :], writes=[xhb], stream="c")
            for kc in range(8):
                transpose(ps[0][:, kc * 16:(kc + 1) * 16], xh[:, kc * 128:(kc + 1) * 128], ident[0:16, 0:16],
                          [xhb, cB], pb[0])
            Vv([pb[0]], [xhTb], lambda: nc.vector.tensor_copy(out=xhT[:, :, :],
                                                             in_=ps[0][:, 0:128].rearrange("p (k t) -> p k t", k=8)))
            load_wblk(wblk[0], wblkb[0], s_win, 1536)
            cx.dma("sp", wpool_t[:], s_wpool[:, :].rearrange("(g c) e -> c g e", g=4), reads=[convs["s_wpool"]], writes=[wpb],
                   stream="w")
            for g4 in range(4):
                mm_group(ps[2][:, :], [(wblk[0][:, kc, g4 * 128:(g4 + 1) * 128], xT_b[:, kc, :]) for kc in range(8)],
                         [wblkb[0], xb_buf], pb[2])
                mm_group(ps[3][:, 0:16], [(wblk[0][:, kc, g4 * 128:(g4 + 1) * 128], xhT[:, kc, :]) for kc in range(8)],
                         [wblkb[0], xhTb], pb[3])
                Aa([pb[2]], [uTb], lambda: nc.scalar.copy(out=uT[:, g4, 16:16 + G], in_=ps[2][:, :]))
                Aa([pb[3]], [uTb], lambda: nc.scalar.copy(out=uT[:, g4, 0:16], in_=ps[3][:, 0:16]))
                cur, curb = uT[:, g4, :], uTb
                L = 16 + G
                for k in range(g4 + 1):
                    sh = 1 << k
                    nxt, nxtb = (pa, pab) if k % 2 == 0 else (pbt, pbb)
                    Vv([curb], [nxtb], lambda: nc.vector.tensor_tensor(out=nxt[:, sh:L], in0=cur[:, sh:L],
                                                                      in1=cur[:, 0:L - sh], op=ALU.add))
                    cur, curb = nxt[:, :], nxtb
                cx.dma("sp", invt[:], invc[slot, :, g4 * G:(g4 + 1) * G], writes=[invb], stream="c")
                Vv([curb, invb], [t1b], lambda: nc.vector.tensor_tensor(out=t1[:], in0=cur[:, 16:L], in1=invt[:],
                                                                       op=ALU.mult))
                Vv([t1b, uTb], [dfb], lambda: nc.vector.tensor_tensor(out=diffT[:], in0=t1[:], in1=uT[:, g4, 16:L],
                                                                     op=ALU.subtract))
                mm_group(ps[5][:, :], [(wpool_t[:, g4, :], diffT[:])], [wpb, dfb], pb[5])
                Vv([pb[5], cB], [plb], lambda: nc.vector.tensor_scalar(out=pooledT[:, g4, :], in0=ps[5][:, :],
                                                                      scalar1=fp[:, 48 + g4:49 + g4], scalar2=None,
                                                                      op0=ALU.mult))
            if slot == NSLOT - 1:
                for g4 in range(4):
                    transpose(ps[0][:, g4 * 128:(g4 + 1) * 128], uT[:, g4, 16 + G - 128:16 + G], ident[:],
                              [uTb, cB], pb[0])
                Aa([pb[0]], [sgab], lambda: nc.scalar.copy(out=sga[:], in_=ps[0][:, :]))
                cx.dma("sp", pool_out[:, :], sga[112:128, :], reads=[sgab], stream="po")

            barrier()
            sA1.close()
            merge_ln1(sba, G, xT_f, xfb, xT_b, xb_buf, attnT, atb, pooledT, plb)
            barrier()

        with ExitStack() as sB:
            def sbb(name, shape, dt=F32):
                return sB.enter_context(nc.sbuf_tensor(f"b{slot}_" + name, list(shape), dt))
            moe_and_out(sbb, slot * G, G, y_out)
            barrier()

    with ExitStack() as sC:
        def sbc(name, shape, dt=F32):
            return sC.enter_context(nc.sbuf_tensor("c_" + name, list(shape), dt))
        merge_ln1(sbc, 4, xsT_f, xsfb, xsT_b, xsbb, attnT_s, atsb, pooledT_s, plsb)
        barrier()
    with ExitStack() as sD:
        def sbd(name, shape, dt=F32):
            return sD.enter_context(nc.sbuf_tensor("d_" + name, list(shape), dt))
        moe_and_out(sbd, 0, 4, ys_out)
        barrier()
    cx.drain("sp")


_PROG = None


def _rope_tables(pos):
    half = HD // 2
    inv_freq = 1.0 / (10000.0 ** (np.arange(0, HD, 2, dtype=np.float32) / HD))
    ang = pos.astype(np.float32)[None, :] * inv_freq[:, None].astype(np.float32)
    cos = np.cos(ang).astype(np.float32)
    sin = np.sin(ang).astype(np.float32)
    cos64 = np.concatenate([cos, cos], 0)
    sin64 = np.concatenate([-sin, sin], 0)
    return np.ascontiguousarray(np.concatenate([cos64, cos64], 0)), np.ascontiguousarray(np.concatenate([sin64, sin64], 0))


def kernel(**inp):
    global _PROG
    x_prompt = np.asarray(inp["x_prompt"], np.float32)
    w_in = np.asarray(inp["w_in"], np.float32)[0]
    wqk = w_in[:, 0:1024].reshape(D, 16, 2, 32)
    w_rot = np.ascontiguousarray(wqk[:, :, ::-1, :].reshape(D, 1024))
    w_r = np.concatenate([np.asarray(inp["w_group_router"], np.float32)[0],
                          np.asarray(inp["w_expert_router"], np.float32)[0].transpose(1, 0, 2).reshape(D, 16)], 1)
    w_r = np.ascontiguousarray(w_r.reshape(8, 128, 20).transpose(1, 0, 2).reshape(128, 160))
    b_r = np.concatenate([np.asarray(inp["b_group_router"], np.float32)[0],
                          np.asarray(inp["b_expert_router"], np.float32)[0].reshape(16)])
    b_r = np.ascontiguousarray(np.broadcast_to(b_r[None, :], (128, 20)))

    def fm(v):
        return np.asarray(v, np.float32).reshape(8, 128).T

    fpar = np.concatenate([fm(inp["ln1_g"][0]), fm(inp["ln1_b"][0]), fm(inp["ln2_g"][0]), fm(inp["ln2_b"][0]),
                           fm(inp["b_gate"][0, 0]), fm(inp["b_gate"][0, 1]),
                           np.asarray(inp["pool_scale"], np.float32)[0].reshape(4, 128).T], 1)
    fpar = np.ascontiguousarray(fpar)
    cosk, sink = _rope_tables(np.arange(SEQ))
    blk = (np.arange(SEQ)[None, :] // 256 == np.arange(16)[:, None]).astype(ml_dtypes.bfloat16)
    ident = np.eye(128, dtype=np.float32)
    sel16 = np.ascontiguousarray(np.repeat(np.eye(16, dtype=np.float32)[:, :, None], 128, axis=2).reshape(16, 16 * 128))
    kk = np.arange(128)[:, None]
    qq = np.arange(G)[None, :]
    mt = []
    for t in range(4):
        kp = 128 * t + kk
        mt.append(1.0 - ((kp // 256 == qq // 256) & (kp > qq)).astype(np.float32))
    ones = np.ones((128, G), np.float32)
    zeros = np.zeros((128, G), np.float32)
    lower = np.concatenate(mt + [zeros] * 4, 1)
    upper = np.concatenate([ones] * 4 + mt, 1)

    x_sample = np.asarray(inp["x_sample"], np.float32)[:, 0, :]
    ck = np.asarray(inp["cache_k"], np.float32)[0].reshape(2560 * 8, 8192)
    cv = np.asarray(inp["cache_v"], np.float32)[0].reshape(2560 * 8, 8192)
    page_table = np.asarray(inp["page_table"], np.int32)
    state_pool = np.asarray(inp["state_pool"], np.float32)[0]
    c8, s8 = _rope_tables(np.array([8192]))
    cs8 = np.ascontiguousarray(np.tile(c8[0:64, 0][None, :], (4, NH)))
    sn8 = np.ascontiguousarray(np.tile(s8[0:64, 0][None, :], (4, NH)))
    pp = np.arange(128)
    selT = np.zeros((4, 256), np.float32)
    pairsel = np.zeros((128, 8), np.float32)
    for t in range(2):
        for p_ in range(128):
            selT[2 * t + p_ // 64, t * 128 + p_] = 1.0
            pairsel[p_, t * 4 + 2 * t + p_ // 64] = 1.0
    in_maps = []
    for c in range(8):
        s, p = c // 2, c % 2
        groups = [0, 3, 4, 7] if p == 0 else [1, 2, 5, 6]
        xs = x_prompt[s]
        xo = np.concatenate([xs[g * G:(g + 1) * G] for g in groups], 0)
        xh = np.zeros((NSLOT * 16, D), np.float32)
        for i, g in enumerate(groups):
            if g > 0:
                xh[i * 16:(i + 1) * 16] = xs[g * G - 16:g * G]
        posq = np.concatenate([np.arange(g * G, (g + 1) * G) for g in groups])
        cq, sq = _rope_tables(posq)
        own = posq // 256
        nb = np.arange(16)[None, :]
        valm = np.ascontiguousarray(np.tile((nb < own[:, None]).astype(np.float32), (1, NH)))
        ownm = np.ascontiguousarray(np.tile((nb == own[:, None]).astype(np.float32), (1, NH)))
        negm = np.ascontiguousarray(np.tile(np.where(nb < own[:, None], 0.0, NEG).astype(np.float32), (1, NH)))
        cm = np.stack([lower if (g % 2 == 0) else upper for g in groups[:2]], 0).astype(ml_dtypes.bfloat16)
        invc = np.zeros((NSLOT, 128, 4 * G), np.float32)
        for i, g in enumerate(groups):
            pos = np.arange(g * G, (g + 1) * G)
            for gi, w in enumerate((2, 4, 8, 16)):
                invc[i, :, gi * G:(gi + 1) * G] = (1.0 / np.minimum(w, pos + 1))[None, :]
        in_maps.append({
            "xseq": np.ascontiguousarray(xs), "xown": np.ascontiguousarray(xo), "xhalo": xh,
            "w_in": w_in, "w_rot": w_rot,
            "w_pool": np.asarray(inp["w_pool"], np.float32)[0].reshape(512, 128),
            "w_a": np.asarray(inp["w_branch_a"], np.float32)[0], "w_b": np.asarray(inp["w_branch_b"], np.float32)[0],
            "w_out": np.asarray(inp["w_out"], np.float32)[0],
            "w_eg": np.asarray(inp["w_e_gate"], np.float32)[0].reshape(NE * D, DE),
            "w_eu": np.asarray(inp["w_e_up"], np.float32)[0].reshape(NE * D, DE),
            "w_ed": np.asarray(inp["w_e_down"], np.float32)[0].reshape(NE * DE, D),
            "w_r": w_r, "b_r": b_r, "fpar": fpar, "cosk": cosk, "sink": sink, "cosq": cq, "sinq": sq,
            "negm": negm, "valm": valm, "ownm": ownm, "cmask": np.ascontiguousarray(cm.reshape(2, 128, 8 * G)),
            "blkind": blk, "invc": invc, "ident": ident, "sel16": sel16,
            "xs4": np.ascontiguousarray(x_sample[4 * c:4 * c + 4]), "ck": ck, "cv": cv,
            "pt": np.ascontiguousarray(page_table[4 * c:4 * c + 4].reshape(2, 128).T),
            "st4": np.ascontiguousarray(state_pool[4 * c:4 * c + 4].reshape(4, 15 * 512)),
            "cs8": cs8, "sn8": sn8, "selT": selT, "pairsel": pairsel,
        })
    if _PROG is None:
        _PROG = build_program()
    res = run_bass_kernel_spmd(_PROG, in_maps, core_ids=list(range(8)))
    R = res.results
    y_p = np.zeros((4, SEQ, D), np.float32)
    k_p = np.zeros((1, 4, SEQ, NH, HD), np.float32)
    v_p = np.zeros((1, 4, SEQ, NH, HD), np.float32)
    pool_p = np.zeros((1, 4, 15, 512), np.float32)
    for c in range(8):
        s, p = c // 2, c % 2
        groups = [0, 3, 4, 7] if p == 0 else [1, 2, 5, 6]
        for i, g in enumerate(groups):
            y_p[s, g * G:(g + 1) * G] = R[c]["y_out"][i * G:(i + 1) * G]
        if p == 0:
            k_p[0, s] = R[c]["k_out"].reshape(SEQ, NH, HD)
            v_p[0, s] = R[c]["v_out"].reshape(SEQ, NH, HD)
            pool_p[0, s] = R[c]["pool_out"][1:16]
    y_s = np.zeros((32, 1, D), np.float32)
    k_s = np.zeros((1, 32, 1, NH, HD), np.float32)
    v_s = np.zeros((1, 32, 1, NH, HD), np.float32)
    pool_s = np.zeros((1, 32, 15, 512), np.float32)
    for c in range(8):
        y_s[4 * c:4 * c + 4, 0] = R[c]["ys_out"]
        k_s[0, 4 * c:4 * c + 4, 0] = R[c]["ks_out"].reshape(4, NH, HD)
        v_s[0, 4 * c:4 * c + 4, 0] = R[c]["vs_out"].reshape(4, NH, HD)
        pool_s[0, 4 * c:4 * c + 4] = R[c]["ps_out"].reshape(4, 15, 512)
    return (y_p, y_s, k_p, v_p, pool_p, k_s, v_s, pool_s)
```
